# Optimizing a Trainium2 kernel written in Bass

```python
import math
import jax
import jax.numpy as jnp
from jax import lax
import numpy as np

D_MODEL = 2048
BATCH = 32
SEQ = 256
DEPTH = 1
DEC_BATCH = 4
DEC_SEQ = 2048
PAST_LEN = 512

GRID_W = 64
SSM_EXPAND = 2
D_SSM = SSM_EXPAND * D_MODEL
SSM_HEADDIM = 64
N_SSM_HEADS = D_SSM // SSM_HEADDIM
N_SSM_GROUPS = 8
D_STATE = 128
D_CONV = 3
CHUNK = 128
CONV_DIM = D_SSM + 2 * N_SSM_GROUPS * D_STATE
N_MLA_HEADS = 16
Q_LORA_RANK = 512
KV_LORA_RANK = 512
QK_NOPE_DIM = 128
QK_ROPE_DIM = 64
V_HEAD_DIM = 128
ROPE_THETA = 10000.0
Q_BLOCK = 128
N_EXPERTS = 32
TOP_K = 4
D_FF = D_MODEL
SWIGLU_LIMIT = 7.0
SWIGLU_ALPHA = 1.702
LN_EPS = 1e-5
RMS_EPS = 1e-6
DEEPNORM_ALPHA = (2 * DEPTH) ** 0.25
DEEPNORM_BETA = (8 * DEPTH) ** -0.25
IN_SPLITS = (D_SSM, CONV_DIM, 2 * N_SSM_HEADS, Q_LORA_RANK, KV_LORA_RANK + QK_ROPE_DIM, 2 * D_MODEL)
D_IN_PROJ = sum(IN_SPLITS)
IN_OFFSETS = tuple(sum(IN_SPLITS[:i + 1]) for i in range(len(IN_SPLITS) - 1))

kernel_name = 'hybrid_ssd_mla_moe_diffusion_step'


def layer_norm(x, w, b):
    xf = x.astype(jnp.float32)
    mu = jnp.mean(xf, -1, keepdims=True)
    var = jnp.mean(jnp.square(xf - mu), -1, keepdims=True)
    return ((xf - mu) * lax.rsqrt(var + LN_EPS) * w + b).astype(x.dtype)


def rms_norm(x, w):
    xf = x.astype(jnp.float32)
    return (xf * lax.rsqrt(jnp.mean(xf * xf, -1, keepdims=True) + RMS_EPS) * w).astype(x.dtype)


def adaln_mod(cond, lp):
    m = jax.nn.silu(cond) @ lp['w_ada'] + lp['b_ada']
    return jnp.split(m[:, None, :], 6, axis=-1)


def axial_rope(n_tok):
    rows = n_tok // GRID_W
    row = jnp.repeat(jnp.arange(rows, dtype=jnp.float32), GRID_W)
    col = jnp.tile(jnp.arange(GRID_W, dtype=jnp.float32), rows)
    n_freq = QK_ROPE_DIM // 4
    inv = ROPE_THETA ** (-jnp.arange(n_freq, dtype=jnp.float32) / n_freq)
    ang = jnp.concatenate([row[:, None] * inv, col[:, None] * inv], -1)
    return jnp.cos(ang), jnp.sin(ang)


def apply_rope(x, cos, sin):
    x1, x2 = jnp.split(x, 2, axis=-1)
    return jnp.concatenate([x1 * cos - x2 * sin, x2 * cos + x1 * sin], -1).astype(x.dtype)


def centred_dwconv(x, w, b):
    pad = (D_CONV - 1) // 2
    y = lax.conv_general_dilated(x, w[:, None, :], window_strides=(1,), padding=[(pad, pad)],
                                 dimension_numbers=('NWC', 'WIO', 'NWC'), feature_group_count=x.shape[-1])
    return y + b


def ssd_scan(x, dt, A, B, C, h0):
    b, L, H, P = x.shape
    G, N = B.shape[2], B.shape[3]
    hg = H // G
    nc = L // CHUNK
    xr = x.reshape(b, nc, CHUNK, G, hg, P)
    dtr = dt.reshape(b, nc, CHUNK, G, hg)
    Br = B.reshape(b, nc, CHUNK, G, N)
    Cr = C.reshape(b, nc, CHUNK, G, N)
    acum = jnp.cumsum(dtr * A.reshape(G, hg), axis=2)
    seg = acum[:, :, :, None] - acum[:, :, None]
    mask = jnp.tril(jnp.ones((CHUNK, CHUNK), dtype=bool))[:, :, None, None]
    lmat = jnp.exp(jnp.where(mask, seg, -jnp.inf))
    cb = jnp.einsum('bcqgn,bckgn->bcqkg', Cr, Br)
    m = cb[..., None] * lmat * dtr[:, :, None]
    y_diag = jnp.einsum('bcqkgh,bckghp->bcqghp', m, xr)
    decay_end = jnp.exp(acum[:, :, -1:] - acum)
    states = jnp.einsum('bckgn,bckghp->bcghpn', Br, xr * (decay_end * dtr)[..., None])
    chunk_decay = jnp.exp(acum[:, :, -1])

    def step(h, inp):
        s_c, d_c = inp
        return h * d_c[..., None, None] + s_c, h

    h_init = h0.astype(jnp.float32).reshape(b, G, hg, P, N)
    h_final, h_prev = lax.scan(step, h_init, (jnp.swapaxes(states, 0, 1), jnp.swapaxes(chunk_decay, 0, 1)))
    h_prev = jnp.swapaxes(h_prev, 0, 1)
    y_off = jnp.einsum('bcqgn,bcghpn->bcqghp', Cr, h_prev) * jnp.exp(acum)[..., None]
    y = (y_diag + y_off).reshape(b, L, H, P)
    return y, h_final.reshape(b, H, P, N)


def ssd_branch(z, xbc, dt_raw, lp, h_init):
    b, L, _ = z.shape
    xbc = jax.nn.silu(centred_dwconv(xbc, lp['conv_w'], lp['conv_b']))
    xs, B, C = jnp.split(xbc, [D_SSM, D_SSM + N_SSM_GROUPS * D_STATE], axis=-1)
    xs = xs.reshape(b, L, N_SSM_HEADS, SSM_HEADDIM)
    B = B.reshape(b, L, N_SSM_GROUPS, D_STATE)
    C = C.reshape(b, L, N_SSM_GROUPS, D_STATE)
    y = xs.astype(jnp.float32) * lp['d_skip'][:, None]
    finals = []
    for d in range(2):
        dt = jax.nn.softplus(dt_raw[..., d * N_SSM_HEADS:(d + 1) * N_SSM_HEADS].astype(jnp.float32) + lp['dt_bias'][d])
        A = -jnp.exp(lp['a_log'][d].astype(jnp.float32))
        if d == 0:
            y_d, h_d = ssd_scan(xs, dt, A, B, C, h_init[:, d])
        else:
            y_d, h_d = ssd_scan(jnp.flip(xs, 1), jnp.flip(dt, 1), A, jnp.flip(B, 1), jnp.flip(C, 1), h_init[:, d])
            y_d = jnp.flip(y_d, 1)
        y = y + y_d
        finals.append(h_d)
    yg = (y.reshape(b, L, D_SSM) * jax.nn.silu(z.astype(jnp.float32))).reshape(b, L, N_SSM_GROUPS, -1)
    yg = yg * lax.rsqrt(jnp.mean(yg * yg, -1, keepdims=True) + RMS_EPS)
    y = yg.reshape(b, L, D_SSM) * lp['ssm_norm_w']
    return y.astype(z.dtype), jnp.stack(finals, axis=1).astype(z.dtype)


def mla_expand_kv(ckv, k_rope, w_kv_b):
    b, L, _ = ckv.shape
    kv = (ckv @ w_kv_b).reshape(b, L, N_MLA_HEADS, QK_NOPE_DIM + V_HEAD_DIM)
    k_nope, v = kv[..., :QK_NOPE_DIM], kv[..., QK_NOPE_DIM:]
    k_r = jnp.broadcast_to(k_rope[:, :, None, :], (b, L, N_MLA_HEADS, QK_ROPE_DIM)).astype(k_nope.dtype)
    return jnp.concatenate([k_nope, k_r], -1), v


def block_attention(q, k, v):
    b, L, h, dq = q.shape
    nb = L // Q_BLOCK
    qb = jnp.swapaxes(q.reshape(b, nb, Q_BLOCK, h, dq), 0, 1)
    scale = dq ** -0.5

    def one(qblk):
        s = jnp.einsum('bqhd,bkhd->bhqk', qblk, k).astype(jnp.float32) * scale
        p = jax.nn.softmax(s, axis=-1).astype(v.dtype)
        return jnp.einsum('bhqk,bkhd->bqhd', p, v)

    o = lax.map(one, qb)
    return jnp.swapaxes(o, 0, 1).reshape(b, L, h, v.shape[-1])


def token_mixer(h, lp, rope, ctx_ckv, ctx_krope, ssm_init):
    b, L, _ = h.shape
    z, xbc, dt_raw, q_a, kv_a, gate_logits = jnp.split(h @ lp['w_in'], IN_OFFSETS, axis=-1)
    y_ssm, ssm_states = ssd_branch(z, xbc, dt_raw, lp, ssm_init)
    q = (rms_norm(q_a, lp['q_a_norm_w']) @ lp['w_q_b']).reshape(b, L, N_MLA_HEADS, QK_NOPE_DIM + QK_ROPE_DIM)
    q_nope, q_rope = q[..., :QK_NOPE_DIM], q[..., QK_NOPE_DIM:]
    ckv = rms_norm(kv_a[..., :KV_LORA_RANK], lp['kv_a_norm_w'])
    k_rope = kv_a[..., KV_LORA_RANK:]
    if rope is not None:
        cos, sin = rope
        q_rope = apply_rope(q_rope, cos[:, None, :], sin[:, None, :])
        k_rope_pos = apply_rope(k_rope, cos, sin)
    else:
        k_rope_pos = k_rope
    q = jnp.concatenate([q_nope, q_rope], -1)
    k, v = mla_expand_kv(ckv, k_rope_pos, lp['w_kv_b'])
    if ctx_ckv is not None:
        k_c, v_c = mla_expand_kv(ctx_ckv, ctx_krope, lp['w_kv_b'])
        k = jnp.concatenate([k, k_c.astype(k.dtype)], axis=1)
        v = jnp.concatenate([v, v_c.astype(v.dtype)], axis=1)
    o = block_attention(q, k, v).reshape(b, L, N_MLA_HEADS * V_HEAD_DIM)
    g_ssm, g_mla = jnp.split(jax.nn.sigmoid(gate_logits), 2, axis=-1)
    merged = g_ssm * (y_ssm @ lp['w_ssm_out']) + g_mla * (o @ lp['w_mla_out'])
    return (merged @ lp['w_o']).astype(h.dtype), ckv, k_rope, ssm_states


def moe_ffn(h, lp):
    b, L, D = h.shape
    t = h.reshape(b * L, D)
    logits = (t @ lp['router_w'] + lp['router_b']).astype(jnp.float32)
    top_v, top_i = lax.top_k(logits, TOP_K)
    wts = jax.nn.softmax(top_v, axis=-1)
    combine = jnp.sum(jax.nn.one_hot(top_i, N_EXPERTS, dtype=jnp.float32) * wts[..., None], axis=1)
    out = jnp.zeros((b * L, D), jnp.float32)
    for e in range(N_EXPERTS):
        gu = t @ lp['w_gu'][e] + lp['b_gu'][e]
        gate, up = gu[..., 0::2], gu[..., 1::2]
        gate = jnp.minimum(gate, SWIGLU_LIMIT)
        up = jnp.clip(up, -SWIGLU_LIMIT, SWIGLU_LIMIT)
        act = gate * jax.nn.sigmoid(SWIGLU_ALPHA * gate) * (up + 1.0)
        out = out + combine[:, e:e + 1] * (act @ lp['w_down'][e] + lp['b_down'][e])
    return out.reshape(b, L, D).astype(h.dtype)


def trunk_layer(x, cond, lp, rope, ctx_ckv, ctx_krope, ssm_init):
    sh1, sc1, g1, sh2, sc2, g2 = adaln_mod(cond, lp)
    mix, ckv, k_rope, states = token_mixer(x * (1.0 + sc1) + sh1, lp, rope, ctx_ckv, ctx_krope, ssm_init)
    x = layer_norm(DEEPNORM_ALPHA * x + g1 * mix, lp['ln1_w'], lp['ln1_b'])
    x = layer_norm(DEEPNORM_ALPHA * x + g2 * moe_ffn(x * (1.0 + sc2) + sh2, lp), lp['ln2_w'], lp['ln2_b'])
    return x, ckv, k_rope, states


def setup_inputs(seed: int = 0) -> dict:
    key = jax.random.key(seed)
    ks = iter(jax.random.split(key, 48))

    def nrm(shape, scale=1.0):
        return jax.random.normal(next(ks), shape, jnp.float32) * scale

    L, D, H, E = DEPTH, D_MODEL, N_SSM_HEADS, N_EXPERTS
    dt0 = jnp.exp(jax.random.uniform(next(ks), (L, 2, H), jnp.float32, math.log(1e-3), math.log(1e-1)))
    dt_bias = dt0 + jnp.log(-jnp.expm1(-dt0))
    a_log = jnp.log(jax.random.uniform(next(ks), (L, 2, H), jnp.float32, 1.0, 16.0))
    return {
        'x_prompt': nrm((BATCH, SEQ, D)),
        'x_sample': nrm((DEC_BATCH, DEC_SEQ, D)),
        'cache_mla_ckv': nrm((DEC_BATCH, DEPTH, PAST_LEN, KV_LORA_RANK)),
        'cache_mla_krope': nrm((DEC_BATCH, DEPTH, PAST_LEN, QK_ROPE_DIM)),
        'state_ssm': nrm((DEC_BATCH, DEPTH, 2, H, SSM_HEADDIM, D_STATE), 0.5),
        'c': nrm((DEC_BATCH, D)),
        'c_ctx': nrm((D,)),
        'w_ada': nrm((L, D, 6 * D), 0.5 * D ** -0.5),
        'b_ada': nrm((L, 6 * D), 0.02),
        'w_in': nrm((L, D, D_IN_PROJ), D ** -0.5),
        'conv_w': nrm((L, D_CONV, CONV_DIM), D_CONV ** -0.5),
        'conv_b': nrm((L, CONV_DIM), 0.02),
        'a_log': a_log,
        'dt_bias': dt_bias,
        'd_skip': 1.0 + nrm((L, H), 0.1),
        'ssm_norm_w': 1.0 + nrm((L, D_SSM), 0.1),
        'w_ssm_out': nrm((L, D_SSM, D), D_SSM ** -0.5),
        'q_a_norm_w': 1.0 + nrm((L, Q_LORA_RANK), 0.1),
        'w_q_b': nrm((L, Q_LORA_RANK, N_MLA_HEADS * (QK_NOPE_DIM + QK_ROPE_DIM)), Q_LORA_RANK ** -0.5),
        'kv_a_norm_w': 1.0 + nrm((L, KV_LORA_RANK), 0.1),
        'w_kv_b': nrm((L, KV_LORA_RANK, N_MLA_HEADS * (QK_NOPE_DIM + V_HEAD_DIM)), KV_LORA_RANK ** -0.5),
        'w_mla_out': nrm((L, N_MLA_HEADS * V_HEAD_DIM, D), (N_MLA_HEADS * V_HEAD_DIM) ** -0.5),
        'w_o': nrm((L, D, D), DEEPNORM_BETA * D ** -0.5),
        'ln1_w': 1.0 + nrm((L, D), 0.1),
        'ln1_b': nrm((L, D), 0.02),
        'router_w': nrm((L, D, E), D ** -0.5),
        'router_b': nrm((L, E), 0.01),
        'w_gu': nrm((L, E, D, 2 * D_FF), D ** -0.5),
        'b_gu': nrm((L, E, 2 * D_FF), 0.01),
        'w_down': nrm((L, E, D_FF, D), DEEPNORM_BETA * D_FF ** -0.5),
        'b_down': nrm((L, E, D), 0.01),
        'ln2_w': 1.0 + nrm((L, D), 0.1),
        'ln2_b': nrm((L, D), 0.02),
    }


def reference(x_prompt, x_sample, cache_mla_ckv, cache_mla_krope, state_ssm, c, c_ctx,
              w_ada, b_ada, w_in, conv_w, conv_b, a_log, dt_bias, d_skip, ssm_norm_w, w_ssm_out,
              q_a_norm_w, w_q_b, kv_a_norm_w, w_kv_b, w_mla_out, w_o, ln1_w, ln1_b,
              router_w, router_b, w_gu, b_gu, w_down, b_down, ln2_w, ln2_b):
    rope = axial_rope(x_sample.shape[1])
    zero_state = jnp.zeros((x_prompt.shape[0], 2, N_SSM_HEADS, SSM_HEADDIM, D_STATE), x_prompt.dtype)
    y_p, y_s = x_prompt, x_sample
    ckv_list, krope_list, state_list = [], [], []
    for l in range(DEPTH):
        lp = {
            'w_ada': w_ada[l], 'b_ada': b_ada[l], 'w_in': w_in[l], 'conv_w': conv_w[l], 'conv_b': conv_b[l],
            'a_log': a_log[l], 'dt_bias': dt_bias[l], 'd_skip': d_skip[l], 'ssm_norm_w': ssm_norm_w[l],
            'w_ssm_out': w_ssm_out[l], 'q_a_norm_w': q_a_norm_w[l], 'w_q_b': w_q_b[l],
            'kv_a_norm_w': kv_a_norm_w[l], 'w_kv_b': w_kv_b[l], 'w_mla_out': w_mla_out[l], 'w_o': w_o[l],
            'ln1_w': ln1_w[l], 'ln1_b': ln1_b[l], 'router_w': router_w[l], 'router_b': router_b[l],
            'w_gu': w_gu[l], 'b_gu': b_gu[l], 'w_down': w_down[l], 'b_down': b_down[l],
            'ln2_w': ln2_w[l], 'ln2_b': ln2_b[l],
        }
        y_p, ckv_l, krope_l, state_l = trunk_layer(y_p, c_ctx[None, :], lp, None, None, None, zero_state)
        ckv_list.append(ckv_l)
        krope_list.append(krope_l)
        state_list.append(state_l)
        y_s, _, _, _ = trunk_layer(y_s, c, lp, rope, cache_mla_ckv[:, l], cache_mla_krope[:, l], state_ssm[:, l])
    new_cache_mla_ckv = jnp.stack(ckv_list, axis=1)
    new_cache_mla_krope = jnp.stack(krope_list, axis=1)
    new_state_ssm = jnp.stack(state_list, axis=1)
    return (y_p, y_s, new_cache_mla_ckv, new_cache_mla_krope, new_state_ssm)
```

```python
import contextlib
import numpy as np
import concourse.bass as bass
import concourse.mybir as mybir
from concourse.bass_utils import run_bass_kernel_spmd

F32 = mybir.dt.float32
BF16 = mybir.dt.bfloat16
AF = mybir.ActivationFunctionType
ALU = mybir.AluOpType

DEBUG = False
STOP_AFTER = None
DEBUG_CORES = None
DEBUG_KEEP = None

T = 2048
D = 2048
NT = 16
KC = 16
ALPHA = 2.0 ** 0.25
SCALE = 192.0 ** -0.5
NKEY = 2560
COMPUTE = ("pe", "act", "dve", "pool")


class Op:
    __slots__ = ("eng", "fn", "deps", "dom", "seq", "waits", "vc", "idx")


class Sched:
    def __init__(self, nc):
        self.nc = nc
        self.ops = []
        self.res_w = {}
        self.res_r = {}
        self.dom_count = {}
        self.dom_ops = {}
        self.bar_idx = None
        self.bar_start = 0
        self.barriers = []

    def add(self, eng, fn, reads=(), writes=(), key=None):
        o = Op()
        o.idx = len(self.ops)
        o.eng = eng
        o.fn = fn
        o.dom = key if key is not None else eng
        assert key is not None or eng in COMPUTE, (eng, key)
        extra = [r for r in reads if r[:2] == "ps" and r[2:].isdigit()]
        if extra:
            writes = list(writes) + extra
        deps = set()
        if self.bar_idx is not None:
            deps.add(self.bar_idx)
        for r in reads:
            w = self.res_w.get(r)
            if w is not None:
                deps.add(w)
        for w_ in writes:
            w = self.res_w.get(w_)
            if w is not None:
                deps.add(w)
            for r in self.res_r.get(w_, ()):
                deps.add(r)
        for r in reads:
            self.res_r.setdefault(r, []).append(o.idx)
        for w_ in writes:
            self.res_w[w_] = o.idx
            self.res_r[w_] = []
        deps.discard(o.idx)
        o.deps = deps
        c = self.dom_count.get(o.dom, 0) + 1
        self.dom_count[o.dom] = c
        o.seq = c
        self.dom_ops.setdefault(o.dom, []).append(o.idx)
        self.ops.append(o)
        return o

    def barrier(self, fn):
        o = self.add("dve", fn)
        last = {}
        for i in range(self.bar_start, o.idx):
            p = self.ops[i]
            last[p.dom] = i
        o.deps = set(last.values())
        if self.bar_idx is not None:
            o.deps.add(self.bar_idx)
        self.bar_idx = o.idx
        self.bar_start = o.idx
        self.barriers.append(o.idx)
        self.res_w = {}
        self.res_r = {}

    def finalize(self, final_wait_prefix="out_"):
        ops = self.ops
        know = {e: {} for e in ("pe", "act", "dve", "pool", "sp")}
        needed = set()
        for o in ops:
            k = know[o.eng]
            waits = {}
            for d in o.deps:
                dop = ops[d]
                if dop.dom == "pe" and o.eng == "pe":
                    continue
                if k.get(dop.dom, 0) >= dop.seq:
                    continue
                if waits.get(dop.dom, 0) < dop.seq:
                    waits[dop.dom] = dop.seq
            real = {}
            for dom, seq in waits.items():
                if k.get(dom, 0) >= seq:
                    continue
                k = dict(k)
                dop = ops[self.dom_ops[dom][seq - 1]]
                for kd, kv in dop.vc.items():
                    if k.get(kd, 0) < kv:
                        k[kd] = kv
                k[dom] = max(k.get(dom, 0), seq)
                needed.add((dom, seq))
                real[dom] = seq
            know[o.eng] = k
            o.waits = real
            o.vc = k
        self.final_waits = []
        for dom, c in self.dom_count.items():
            if isinstance(dom, str) and dom.startswith(final_wait_prefix):
                needed.add((dom, c))
                self.final_waits.append((dom, c))
        self.sig_val = {}
        for dom, lst in self.dom_ops.items():
            n = 0
            for s in range(1, len(lst) + 1):
                if (dom, s) in needed or dom not in COMPUTE:
                    n += 1
                    self.sig_val[(dom, s)] = n

    def emit(self):
        nc = self.nc
        doms = sorted({d for (d, _) in self.sig_val})
        import bisect
        first = {}
        last = {}
        for o in self.ops:
            if o.dom not in COMPUTE:
                first.setdefault(o.dom, o.idx)
                last[o.dom] = o.idx
        phys = {}
        base = {}
        pool_ = []
        nphys = 0
        for d in sorted(first, key=lambda k: first[k]):
            chosen = None
            for ent in pool_:
                bi = bisect.bisect_right(self.barriers, ent[0])
                if bi < len(self.barriers) and self.barriers[bi] <= first[d]:
                    chosen = ent
                    break
            if chosen is None:
                chosen = [0, 0, nphys]
                nphys += 1
                pool_.append(chosen)
            phys[d] = chosen[2]
            base[d] = chosen[1]
            chosen[0] = last[d]
            chosen[1] += self.dom_count[d]
        self.nphys = nphys
        with contextlib.ExitStack() as st:
            psem = [st.enter_context(nc.semaphore("sd%d" % i)) for i in range(nphys)]
            sems = {}
            for d in doms:
                if d in COMPUTE:
                    sems[d] = st.enter_context(nc.semaphore("s_" + str(d)))
                else:
                    sems[d] = psem[phys[d]]
            block = st.enter_context(nc.Block())
            by_eng = {e: [] for e in ("pe", "act", "dve", "pool", "sp")}
            for o in self.ops:
                by_eng[o.eng].append(o)

            def is_dma(dom):
                return dom not in COMPUTE

            def run(engine, lst, final=False):
                for o in lst:
                    for dom, seq in o.waits.items():
                        v = self.sig_val[(dom, seq)]
                        engine.wait_ge(sems[dom], (v + base[dom]) * 16 if is_dma(dom) else v)
                    ins = o.fn(engine)
                    if (o.dom, o.seq) in self.sig_val:
                        ins.then_inc(sems[o.dom], 16 if is_dma(o.dom) else 1)
                if final:
                    for dom, c in self.final_waits:
                        v = self.sig_val[(dom, c)]
                        engine.wait_ge(sems[dom], (v + base[dom]) * 16 if is_dma(dom) else v)

            @block.tensor
            def _(e):
                run(e, by_eng["pe"])

            @block.scalar
            def _(e):
                run(e, by_eng["act"])

            @block.vector
            def _(e):
                run(e, by_eng["dve"])

            @block.gpsimd
            def _(e):
                run(e, by_eng["pool"])

            @block.sync
            def _(e):
                run(e, by_eng["sp"], final=True)


class Arena:
    def __init__(self, t, nwords):
        self.t = t
        self.n = nwords
        self.off = 0

    def f32(self, *shape):
        n = int(np.prod(shape[1:]))
        n = (n + 7) // 8 * 8
        assert self.off + n <= self.n, ("SBUF arena overflow", self.off, n, self.n)
        ap = self.t[0:shape[0], self.off:self.off + int(np.prod(shape[1:]))]
        self.off += n
        return self._shape(ap, shape)

    def bf16(self, *shape):
        ne = int(np.prod(shape[1:]))
        n = (ne + 1) // 2
        n = (n + 7) // 8 * 8
        assert self.off + n <= self.n, ("SBUF arena overflow", self.off, n, self.n)
        ap = self.t[0:shape[0], self.off:self.off + (ne + 1) // 2].bitcast(BF16)
        if ne % 2:
            ap = ap[:, 0:ne]
        self.off += n
        return self._shape(ap, shape)

    @staticmethod
    def _shape(ap, shape):
        if len(shape) == 2:
            return ap
        if len(shape) == 3:
            return ap.rearrange("p (a b) -> p a b", b=shape[2])
        if len(shape) == 4:
            return ap.rearrange("p (a b c) -> p a b c", b=shape[2], c=shape[3])
        raise ValueError(shape)


def build_program():
    nc = bass.Bass("TRN2", target_bir_lowering=False)
    S = Sched(nc)

    def done():
        S.finalize()
        S.emit()
        return nc, S

    def din(name, shape, dt=F32):
        return nc.dram_tensor(name, list(shape), dt, kind="ExternalInput").ap()

    def dout(name, shape, dt=F32):
        return nc.dram_tensor(name, list(shape), dt, kind="ExternalOutput").ap()

    def dscr(name, shape, dt=F32):
        kind = "ExternalOutput" if (DEBUG and (DEBUG_KEEP is None or name in DEBUG_KEEP)) else "Internal"
        return nc.dram_tensor(name, list(shape), dt, kind=kind).ap()

    x_d = din("x", [T, D])
    cond_d = din("cond", [D])
    w_ada_d = din("w_ada", [D, 6 * D])
    b_ada_d = din("b_ada", [6 * D])
    w_in_d = din("w_in", [D, 15552])
    conv_w_d = din("conv_w", [3, 6144])
    conv_b_d = din("conv_b", [6144])
    a_log_d = din("a_log", [128])
    dt_bias_d = din("dt_bias", [128])
    d_skip_d = din("d_skip", [64])
    ssm_norm_w_d = din("ssm_norm_w", [4096])
    w_ssm_out_d = din("w_ssm_out", [4096, D])
    q_a_norm_w_d = din("q_a_norm_w", [512])
    w_q_b_d = din("w_q_b", [512, 3072])
    kv_a_norm_w_d = din("kv_a_norm_w", [512])
    w_kv_b_d = din("w_kv_b", [512, 4096])
    w_mla_out_d = din("w_mla_out", [D, D])
    w_o_d = din("w_o", [D, D])
    ln1_w_d = din("ln1_w", [D])
    ln1_b_d = din("ln1_b", [D])
    router_w_d = din("router_w", [D, 32])
    router_b_d = din("router_b", [32])
    NEW = 32 if STOP_AFTER is None else 1
    w_gu_d = din("w_gu", [NEW, D, 4096])
    b_gu_d = din("b_gu", [32, 4096])
    w_down_d = din("w_down", [NEW, D, D])
    b_down_d = din("b_down", [32, D])
    ln2_w_d = din("ln2_w", [D])
    ln2_b_d = din("ln2_b", [D])
    ctx_ckv_d = din("ctx_ckv", [512, 512])
    ctx_kr_d = din("ctx_kr", [512, 64])
    h0_d = din("h0", [2, 64, 64, 128])
    carry_d = din("carry", [128])
    cos2_d = din("cos2", [64, T])
    sin2_d = din("sin2", [64, T])
    khot_d = din("khot", [9, NKEY])
    qpen_d = din("qpen", [9, T])
    ident_d = din("ident", [128, 128])
    uf_d = din("uf", [128, 128])
    ub_d = din("ub", [128, 128])
    ones_d = din("ones", [128, 128])

    y_d = dout("y", [T, D])
    ckv_out_d = dout("ckv_out", [T, 512])
    kr_out_d = dout("kr_out", [T, 64])
    st_out_d = dout("st_out", [8, 2, 64, 64, 128])

    g1_s = dscr("g1_s", [D])
    xs_tm_s = dscr("xs_tm_s", [T, 4096], BF16)
    b_tm_s = dscr("b_tm_s", [T, 1024], BF16)
    bct_s = dscr("bct_s", [2048, T], BF16)
    zs_s = dscr("zs_s", [T, 4096], BF16)
    dtq_s = dscr("dtq_s", [5, T, 128])
    qnT_s = dscr("qnT_s", [512, T], BF16)
    ckvT_s = dscr("ckvT_s", [512, T], BF16)
    krT_s = dscr("krT_s", [64, T], BF16)
    gT_s = dscr("gT_s", [4096, T], BF16)
    yT_s = dscr("yT_s", [4096, T], BF16)
    oT_s = dscr("oT_s", [2048, T], BF16)
    x1_s = dscr("x1_s", [T, D])

    NW = (nc.sbuf_bytes_remaining - 6144) // 4
    NW = NW // 8 * 8
    arena_t = nc.alloc_sbuf_tensor("arena", [128, NW], F32)
    PERS_W = 1024
    pers = Arena(arena_t, PERS_W)
    ps = [nc.alloc_psum_tensor("ps%d" % i, [128, 512], F32) for i in range(8)]
    psb = [p[:].bitcast(BF16) for p in ps]

    class StageArena(Arena):
        def __init__(self):
            self.t = arena_t
            self.n = NW
            self.off = PERS_W

    def MM(out, lhsT, rhs, start=True, stop=True, r=(), w=()):
        S.add("pe", lambda e: e.matmul(out, lhsT=lhsT, rhs=rhs, start=start, stop=stop), r, w)

    def TR(out, in_, idn, r=(), w=()):
        S.add("pe", lambda e: e.transpose(out, in_, idn), r, w)

    def ACT(out, in_, func, r=(), w=(), bias=None, scale=None, accum=None):
        kw = {}
        if bias is not None:
            kw["bias"] = bias
        if scale is not None:
            kw["scale"] = scale
        if accum is not None:
            kw["accum_out"] = accum
        S.add("act", lambda e: e.activation(out=out, in_=in_, func=func, **kw), r, w)

    def TT(eng, out, in0, in1, op, r=(), w=()):
        S.add(eng, lambda e: e.tensor_tensor(out=out, in0=in0, in1=in1, op=op), r, w)

    def TS(eng, out, in0, s1, op0, s2=None, op1=None, r=(), w=(), accum=None):
        kw = {}
        if op1 is not None:
            kw["op1"] = op1
        if accum is not None:
            kw["accum_out"] = accum
        S.add(eng, lambda e: e.tensor_scalar(out=out, in0=in0, scalar1=s1, scalar2=s2, op0=op0, **kw), r, w)

    def STT(eng, out, in0, scalar, in1, op0, op1, r=(), w=(), accum=None):
        kw = {}
        if accum is not None:
            kw["accum_out"] = accum
        S.add(eng, lambda e: e.scalar_tensor_tensor(out=out, in0=in0, scalar=scalar, in1=in1, op0=op0, op1=op1, **kw), r, w)

    def CP(eng, out, in_, r=(), w=()):
        if eng == "act":
            S.add("act", lambda e: e.copy(out=out, in_=in_), r, w)
        else:
            S.add(eng, lambda e: e.tensor_copy(out=out, in_=in_), r, w)

    def RECIP(out, in_, r=(), w=()):
        S.add("dve", lambda e: e.reciprocal(out=out, in_=in_), r, w)

    def DMA(q, out, in_, key, r=(), w=(), slow=False):
        if slow:
            S.add(q, lambda e: e.dma_start(out=out, in_=in_, allow_slow_non_contiguous=True), r, w, key=key)
        else:
            S.add(q, lambda e: e.dma_start(out=out, in_=in_), r, w, key=key)

    def MEMSET(eng, ap, val, r=(), w=()):
        S.add(eng, lambda e: e.memset(ap, val), r, w)

    bar_t = pers.f32(128, 8)

    def BARRIER():
        S.barrier(lambda e: e.memset(bar_t, 0.0))

    ident = pers.f32(128, 128)
    identb = pers.bf16(128, 128)
    onesf = pers.f32(128, 128)
    onesb = pers.bf16(128, 128)
    uf = pers.f32(128, 128)
    ub = pers.f32(128, 128)
    mod = pers.f32(128, 96)
    sc1p = pers.f32(128, 16)
    sc2p = pers.f32(128, 16)
    carry = pers.f32(128, 1)
    cm1 = pers.f32(128, 1)
    DMA("sp", ident, ident_d, "c_ident", w=["ident"])
    DMA("pool", identb, ident_d, "c_identb", w=["identb"])
    DMA("sp", onesf, ones_d, "c_ones", w=["onesf"])
    DMA("pool", onesb, ones_d, "c_onesb", w=["onesb"])
    DMA("sp", uf, uf_d, "c_uf", w=["uf"])
    DMA("sp", ub, ub_d, "c_ub", w=["ub"])
    DMA("sp", carry, carry_d.rearrange("(p o) -> p o", o=1), "c_carry", w=["carry"], slow=True)
    TS("dve", cm1, carry, -1.0, ALU.add, r=["carry"], w=["cm1"])

    A = StageArena()
    condc = A.f32(128, 16)
    silc = A.f32(128, 16)
    bada = A.f32(128, 96)
    wsl = [A.f32(128, 16, 1024) for _ in range(2)]
    DMA("sp", condc, cond_d.rearrange("(c p) -> p c", p=128), "s0_cond", w=["condc"], slow=True)
    DMA("sp", bada, b_ada_d.rearrange("(j p) -> p j", p=128), "s0_bada", w=["bada"], slow=True)
    ACT(silc, condc, AF.Silu, r=["condc"], w=["silc"])
    w_ada_v = w_ada_d.rearrange("(k p) n -> p k n", p=128)
    for blk in range(12):
        s = blk % 2
        DMA("sp" if blk % 2 == 0 else "act", wsl[s], w_ada_v[:, :, blk * 1024:(blk + 1) * 1024], "s0_w%d" % s, w=["wada%d" % s])
        for mm in range(8):
            col = blk * 8 + mm
            for k in range(KC):
                MM(ps[0][:, col:col + 1], wsl[s][:, k, mm * 128:(mm + 1) * 128], silc[:, k:k + 1],
                   start=(k == 0), stop=(k == KC - 1), r=["wada%d" % s, "silc"], w=["ps0"])
    TT("dve", mod, ps[0][:, 0:96], bada, ALU.add, r=["ps0", "bada"], w=["mod"])
    TS("dve", sc1p, mod[:, 16:32], 1.0, ALU.add, r=["mod"], w=["sc1p"])
    TS("dve", sc2p, mod[:, 64:80], 1.0, ALU.add, r=["mod"], w=["sc2p"])
    DMA("sp", g1_s.rearrange("(c p) -> p c", p=128), mod[:, 32:48], "s0_g1", r=["mod"], slow=True)
    sh1 = mod[:, 0:16]
    sh2 = mod[:, 48:64]
    g2c = mod[:, 80:96]
    BARRIER()
    if STOP_AFTER == '0':
        return done()

    A = StageArena()
    hT = A.bf16(128, 16, T)
    mark_h = A.off
    xsl = [A.f32(128, D) for _ in range(2)]
    for i in range(NT):
        s = i % 2
        DMA("sp", xsl[s], x_d[i * 128:(i + 1) * 128, :], "s1_x%d" % s, w=["xs%d" % s])
        for cq in range(4):
            b = cq
            for cc in range(4):
                c = cq * 4 + cc
                TR(ps[b][:, cc * 128:(cc + 1) * 128], xsl[s][:, c * 128:(c + 1) * 128], ident,
                   r=["xs%d" % s, "ident"], w=["ps%d" % b])
            for cc in range(4):
                c = cq * 4 + cc
                o_ = hT[:, c, i * 128:(i + 1) * 128]
                i_ = ps[b][:, cc * 128:(cc + 1) * 128]
                if cc % 2 == 0:
                    ACT(o_, i_, AF.Identity, r=["ps%d" % b, "sc1p", "mod"], w=["hT"], scale=sc1p[:, c:c + 1], bias=sh1[:, c:c + 1])
                else:
                    TS("dve", o_, i_, sc1p[:, c:c + 1], ALU.mult, sh1[:, c:c + 1], ALU.add, r=["ps%d" % b, "sc1p", "mod"], w=["hT"])
    BARRIER()
    if STOP_AFTER == '1':
        return done()

    A.off = mark_h
    w_in_v = w_in_d.rearrange("(k p) n -> p k n", p=128)
    NSL = 3
    wsl = [A.bf16(128, 16, 512) for _ in range(NSL)]
    cw = A.f32(128, 48, 3)
    cb = A.f32(128, 48)
    w0c = A.f32(128, 48)
    w2c = A.f32(128, 48)
    qnw = A.f32(128, 4)
    kvw = A.f32(128, 4)
    mark_u = A.off
    cos2 = A.f32(64, T)
    sin2 = A.f32(64, T)
    for j in range(3):
        DMA("sp", cw[:, :, j], conv_w_d[j].rearrange("(c p) -> p c", p=128), "sA_cw%d" % j, w=["cw%d" % j], slow=True)
    DMA("sp", cb, conv_b_d.rearrange("(c p) -> p c", p=128), "sA_cb", w=["cb"], slow=True)
    DMA("sp", qnw, q_a_norm_w_d.rearrange("(c p) -> p c", p=128), "sA_qnw", w=["qnw"], slow=True)
    DMA("sp", kvw, kv_a_norm_w_d.rearrange("(c p) -> p c", p=128), "sA_kvw", w=["kvw"], slow=True)
    DMA("sp", cos2, cos2_d, "sA_cos", w=["cos2"])
    DMA("sp", sin2, sin2_d, "sA_sin", w=["sin2"])
    TS("dve", w0c, cw[:, :, 0], cm1[:, 0:1], ALU.mult, r=["cw0", "cm1"], w=["w0c"])
    TS("dve", w2c, cw[:, :, 2], cm1[:, 0:1], ALU.mult, r=["cw2", "cm1"], w=["w2c"])

    groups = []
    groups.append(("qa", 10368, 0))
    groups.append(("kva", 10880, 0))
    for g in range(12):
        groups.append(("xbc", 4096 + g * 512, g))
    for g in range(8):
        groups.append(("gate", 11456 + g * 512, g))
    for g in range(8):
        groups.append(("z", g * 512, g))

    def issue_w(n):
        if n < len(groups):
            s = n % NSL
            c0 = groups[n][1]
            DMA("pool", wsl[s], w_in_v[:, :, c0:c0 + 512], "sA_w%d" % s, w=["wA%d" % s])

    sq = A.f32(128, 4, 512)
    rt = A.f32(128, 512)
    rstd = A.f32(128, 512)
    nT = A.bf16(128, 4, T)
    ckn32 = A.f32(128, 4, 512)
    otile = [A.f32(128, 512) for _ in range(2)]
    wkr = A.bf16(128, 16, 64)
    wkrs = A.bf16(128, 16, 64)
    kr32 = A.f32(64, 512)
    krt1 = A.f32(64, 512)
    krt2 = A.f32(64, 512)
    krT = A.bf16(64, T)
    kro = [A.f32(128, 4, 64) for _ in range(2)]
    DMA("pool", wkr, w_in_v[:, :, 11392:11456], "sA_wkr", w=["wkr"])
    CP("dve", wkrs[:, :, 0:32], wkr[:, :, 32:64], r=["wkr"], w=["wkrs"])
    CP("dve", wkrs[:, :, 32:64], wkr[:, :, 0:32], r=["wkr"], w=["wkrs"])

    for n in range(NSL - 1):
        issue_w(n)
    bankrr = [0]

    def next_bank():
        b = bankrr[0] % 4
        bankrr[0] += 1
        return b

    chunk_ctr = [0]
    for n, (kind, c0, g) in enumerate(groups):
        issue_w(n + NSL - 1)
        s = n % NSL
        wres = "wA%d" % s
        if n == 2:
            BARRIER()
            if STOP_AFTER in ("Aqa", "Aqa_a", "Aqa_b", "Aqa_c", "Aqa_d"):
                return done()
            A.off = mark_u
            pre = [A.f32(128, T + 2) for _ in range(2)]
            acc = A.f32(128, T)
            post = [A.bf16(128, T) for _ in range(2)]
            tm = [A.bf16(128, 16, 128) for _ in range(2)]
            zt = [A.bf16(128, 512) for _ in range(4)]
            for s_ in range(2):
                MEMSET("dve", pre[s_][:, 0:1], 0.0, w=["pre%d" % s_])
                MEMSET("dve", pre[s_][:, T + 1:T + 2], 0.0, w=["pre%d" % s_])
        if (STOP_AFTER == "Ap" and n == 0) or (STOP_AFTER == "Aqa1" and n == 1):
            BARRIER()
            return done()
        if STOP_AFTER == "Ax1" and n == 3:
            BARRIER()
            return done()
        if kind == "xbc":
            for j in range(4):
                cc = g * 4 + j
                pslot = chunk_ctr[0] % 2
                chunk_ctr[0] += 1
                P_ = pre[pslot]
                for tb in range(4):
                    b = next_bank()
                    for k in range(KC):
                        MM(ps[b][:, :], wsl[s][:, k, j * 128:(j + 1) * 128], hT[:, k, tb * 512:(tb + 1) * 512],
                           start=(k == 0), stop=(k == KC - 1), r=[wres, "hT"], w=["ps%d" % b])
                    CP("act", P_[:, 1 + tb * 512:1 + (tb + 1) * 512], ps[b][:, :], r=["ps%d" % b], w=["pre%d" % pslot])
                pr = ["pre%d" % pslot]
                TS("dve", acc, P_[:, 0:T], cw[:, cc, 0:1], ALU.mult, r=pr + ["cw0"], w=["acc"])
                STT("dve", acc, P_[:, 1:T + 1], cw[:, cc, 1:2], acc, ALU.mult, ALU.add, r=pr + ["cw1", "acc"], w=["acc"])
                STT("dve", acc, P_[:, 2:T + 2], cw[:, cc, 2:3], acc, ALU.mult, ALU.add, r=pr + ["cw2", "acc"], w=["acc"])
                accv = acc.rearrange("p (s t) -> p s t", t=256)
                xv = P_[:, 1:T + 1].rearrange("p (s t) -> p s t", t=256)
                STT("dve", accv[:, 1:8, 0:1], xv[:, 0:7, 255:256], w0c[:, cc:cc + 1], accv[:, 1:8, 0:1], ALU.mult, ALU.add,
                    r=pr + ["w0c", "acc"], w=["acc"])
                STT("dve", accv[:, 0:7, 255:256], xv[:, 1:8, 0:1], w2c[:, cc:cc + 1], accv[:, 0:7, 255:256], ALU.mult, ALU.add,
                    r=pr + ["w2c", "acc"], w=["acc"])
                ACT(post[pslot], acc, AF.Silu, r=["acc", "cb"], w=["post%d" % pslot], bias=cb[:, cc:cc + 1])
                if cc < 40:
                    for half in range(2):
                        b = 4 + half
                        for i in range(8):
                            tix = half * 8 + i
                            TR(psb[b][:, i * 128:(i + 1) * 128], post[pslot][:, tix * 128:(tix + 1) * 128], identb,
                               r=["post%d" % pslot, "identb"], w=["ps%d" % b])
                        CP("dve", tm[pslot][:, half * 8:(half + 1) * 8, :], psb[b][:, :].rearrange("p (a b) -> p a b", b=128),
                           r=["ps%d" % b], w=["tm%d" % pslot])
                    if cc < 32:
                        dst = xs_tm_s.rearrange("(i p) c -> p i c", p=128)[:, :, cc * 128:(cc + 1) * 128]
                    else:
                        dst = b_tm_s.rearrange("(i p) c -> p i c", p=128)[:, :, (cc - 32) * 128:(cc - 31) * 128]
                    DMA("sp", dst, tm[pslot], "sA_tm%d" % pslot, r=["tm%d" % pslot])
                if cc >= 32:
                    DMA("sp", bct_s[(cc - 32) * 128:(cc - 31) * 128, :], post[pslot], "sA_post%d" % pslot, r=["post%d" % pslot])
        elif kind in ("qa", "kva"):
            nw = qnw if kind == "qa" else kvw
            for tb in range(4):
                tsl = slice(tb * 512, (tb + 1) * 512)
                for c in range(4):
                    for k in range(KC):
                        MM(ps[c][:, :], wsl[s][:, k, c * 128:(c + 1) * 128], hT[:, k, tsl],
                           start=(k == 0), stop=(k == KC - 1), r=[wres, "hT"], w=["ps%d" % c])
                for c in range(4):
                    ACT(sq[:, c, :], ps[c][:, :], AF.Square, r=["ps%d" % c], w=["sq"])
                for c in range(4):
                    MM(ps[6][:, :], onesf, sq[:, c, :], start=(c == 0), stop=(c == 3), r=["sq", "onesf"], w=["ps6"])
                ACT(rt, ps[6][:, :], AF.Sqrt, r=["ps6"], w=["rt"], scale=1.0 / 512.0, bias=1e-6)
                RECIP(rstd, rt, r=["rt"], w=["rstd"])
                if kind == "qa":
                    for c in range(4):
                        STT("dve", nT[:, c, tsl], ps[c][:, :], nw[:, c:c + 1], rstd, ALU.mult, ALU.mult,
                            r=["ps%d" % c, "qnw", "rstd"], w=["nT"])
                else:
                    for c in range(4):
                        STT("dve", ckn32[:, c, :], ps[c][:, :], nw[:, c:c + 1], rstd, ALU.mult, ALU.mult,
                            r=["ps%d" % c, "kvw", "rstd"], w=["ckn32"])
                    CP("pool", nT[:, :, tsl], ckn32, r=["ckn32"], w=["nT"])
                    for i4 in range(4 if STOP_AFTER != "Aqa_b" else 0):
                        os_ = (tb * 4 + i4) % 2
                        for c in range(4):
                            TR(ps[4][:, c * 128:(c + 1) * 128], ckn32[:, c, i4 * 128:(i4 + 1) * 128], ident,
                               r=["ckn32", "ident"], w=["ps4"])
                        CP("act", otile[os_], ps[4][:, :], r=["ps4"], w=["otile%d" % os_])
                        row = (tb * 4 + i4) * 128
                        DMA("sp", ckv_out_d[row:row + 128, :], otile[os_], "out_ckv%d" % os_, r=["otile%d" % os_])
                    if STOP_AFTER == "Aqa_a":
                        continue
                    for k in range(KC):
                        MM(ps[5][0:64, :], wkr[:, k, :], hT[:, k, tsl], start=(k == 0), stop=(k == KC - 1), r=["wkr", "hT"], w=["ps5"])
                    for k in range(KC):
                        MM(ps[7][0:64, :], wkrs[:, k, :], hT[:, k, tsl], start=(k == 0), stop=(k == KC - 1), r=["wkrs", "hT"], w=["ps7"])
                    if STOP_AFTER == "Aqa_d":
                        continue
                    CP("act", kr32, ps[5][0:64, :], r=["ps5"], w=["kr32"])
                    TT("dve", krt1, ps[5][0:64, :], cos2[:, tsl], ALU.mult, r=["ps5", "cos2"], w=["krt1"])
                    TT("dve", krt2, ps[7][0:64, :], sin2[:, tsl], ALU.mult, r=["ps7", "sin2"], w=["krt2"])
                    TT("dve", krT[:, tsl], krt1, krt2, ALU.add, r=["krt1", "krt2"], w=["krT"])
                    ks_ = tb % 2
                    if STOP_AFTER == "Aqa_c":
                        continue
                    for i4 in range(4):
                        TR(ps[6][:, i4 * 64:(i4 + 1) * 64], kr32[:, i4 * 128:(i4 + 1) * 128], ident[0:64, 0:64],
                           r=["kr32", "ident"], w=["ps6"])
                    CP("act", kro[ks_], ps[6][:, 0:256].rearrange("p (a b) -> p a b", b=64), r=["ps6"], w=["kro%d" % ks_])
                    DMA("sp", kr_out_d[tb * 512:(tb + 1) * 512, :].rearrange("(a p) c -> p a c", p=128), kro[ks_],
                        "out_kr%d" % ks_, r=["kro%d" % ks_])
            if kind == "qa":
                DMA("sp", qnT_s.rearrange("(c p) t -> p c t", p=128), nT, "sA_nT", r=["nT"])
            else:
                DMA("sp", ckvT_s.rearrange("(c p) t -> p c t", p=128), nT, "sA_nT", r=["nT"])
                DMA("sp", krT_s, krT, "sA_krT", r=["krT"])
        elif kind == "gate":
            for j in range(4):
                cc = g * 4 + j
                pslot = chunk_ctr[0] % 2
                chunk_ctr[0] += 1
                for tb in range(4):
                    b = next_bank()
                    for k in range(KC):
                        MM(ps[b][:, :], wsl[s][:, k, j * 128:(j + 1) * 128], hT[:, k, tb * 512:(tb + 1) * 512],
                           start=(k == 0), stop=(k == KC - 1), r=[wres, "hT"], w=["ps%d" % b])
                    ACT(post[pslot][:, tb * 512:(tb + 1) * 512], ps[b][:, :], AF.Sigmoid, r=["ps%d" % b], w=["post%d" % pslot])
                DMA("sp", gT_s[cc * 128:(cc + 1) * 128, :], post[pslot], "sA_post%d" % pslot, r=["post%d" % pslot])
        else:
            for i in range(NT):
                b = next_bank()
                zs_ = i % 4
                for k in range(KC):
                    MM(ps[b][:, :], hT[:, k, i * 128:(i + 1) * 128], wsl[s][:, k, :],
                       start=(k == 0), stop=(k == KC - 1), r=[wres, "hT"], w=["ps%d" % b])
                ACT(zt[zs_], ps[b][:, :], AF.Silu, r=["ps%d" % b], w=["zt%d" % zs_])
                DMA("sp", zs_s[i * 128:(i + 1) * 128, g * 512:(g + 1) * 512], zt[zs_], "sA_zt%d" % zs_, r=["zt%d" % zs_])
    BARRIER()
    if STOP_AFTER == 'A':
        return done()

    A.off = mark_h
    wdt = A.bf16(128, 16, 128)
    dtraw = A.f32(128, 16, 128)
    dte = A.f32(128, 16, 128)
    dtv = A.f32(128, 16, 128)
    lndt = A.f32(128, 16, 128)
    qa_ = A.f32(128, 16, 128)
    qbexp = A.f32(128, 16, 128)
    qeac = A.f32(128, 16, 128)
    qwdec = A.f32(128, 16, 128)
    qcdec = A.f32(128, 16, 128)
    dtb_bc = A.f32(128, 128)
    alog_bc = A.f32(128, 128)
    Abc = A.f32(128, 128)
    acs = [A.f32(128, 128) for _ in range(2)]
    tmpd = [A.f32(128, 128) for _ in range(2)]
    DMA("pool", wdt, w_in_v[:, :, 10240:10368], "sA3_w", w=["wdt"])
    DMA("sp", dtb_bc, dt_bias_d.partition_broadcast(128), "sA3_dtb", w=["dtb"])
    DMA("sp", alog_bc, a_log_d.partition_broadcast(128), "sA3_alog", w=["alog"])
    ACT(Abc, alog_bc, AF.Exp, r=["alog"], w=["Abc"])
    TS("dve", Abc, Abc, -1.0, ALU.mult, r=["Abc"], w=["Abc"])
    for i in range(NT):
        b = i // 4 % 2
        for k in range(KC):
            MM(ps[b][:, (i % 4) * 128:(i % 4 + 1) * 128], hT[:, k, i * 128:(i + 1) * 128], wdt[:, k, :],
               start=(k == 0), stop=(k == KC - 1), r=["wdt", "hT"], w=["ps%d" % b])
        if i % 4 == 3:
            i0 = i - 3
            TT("dve", dtraw[:, i0:i0 + 4, :], ps[b][:, :].rearrange("p (a b) -> p a b", b=128),
               dtb_bc.unsqueeze(1).to_broadcast([128, 4, 128]), ALU.add, r=["ps%d" % b, "dtb"], w=["dtraw"])
    ACT(dte, dtraw, AF.Exp, r=["dtraw"], w=["dte"])
    ACT(dtv, dte, AF.Ln, r=["dte"], w=["dtv"], bias=1.0, scale=1.0)
    ACT(lndt, dtv, AF.Ln, r=["dtv"], w=["lndt"])
    TT("dve", qa_, dtv, Abc.unsqueeze(1).to_broadcast([128, 16, 128]), ALU.mult, r=["dtv", "Abc"], w=["qa"])
    for c in range(NT):
        s = c % 2
        MM(ps[2 + s][:, 0:64], uf, qa_[:, c, 0:64], r=["uf", "qa"], w=["ps%d" % (2 + s)])
        MM(ps[2 + s][:, 64:128], ub, qa_[:, c, 64:128], r=["ub", "qa"], w=["ps%d" % (2 + s)])
        MM(ps[4 + s][:, 0:128], onesf, qa_[:, c, :], r=["onesf", "qa"], w=["ps%d" % (4 + s)])
        CP("act", acs[s], ps[2 + s][:, 0:128], r=["ps%d" % (2 + s)], w=["acs%d" % s])
        TT("dve", qbexp[:, c, :], lndt[:, c, :], acs[s], ALU.subtract, r=["lndt", "acs%d" % s], w=["qbexp"])
        ACT(qeac[:, c, :], ps[2 + s][:, 0:128], AF.Exp, r=["ps%d" % (2 + s)], w=["qeac"])
        ACT(qcdec[:, c, :], ps[4 + s][:, 0:128], AF.Exp, r=["ps%d" % (4 + s)], w=["qcdec"])
        TT("dve", tmpd[s], ps[4 + s][:, 0:128], acs[s], ALU.subtract, r=["ps%d" % (4 + s), "acs%d" % s], w=["tmpd%d" % s])
        ACT(tmpd[s], tmpd[s], AF.Exp, r=["tmpd%d" % s], w=["tmpd%d" % s])
        TT("dve", qwdec[:, c, :], tmpd[s], dtv[:, c, :], ALU.mult, r=["tmpd%d" % s, "dtv"], w=["qwdec"])
    for qi, (qt, nm) in enumerate(((qa_, "qa"), (qbexp, "qbexp"), (qeac, "qeac"), (qwdec, "qwdec"), (qcdec, "qcdec"))):
        DMA("sp", dtq_s[qi].rearrange("(c p) n -> p c n", p=128), qt, "sA3_q%d" % qi, r=[nm])
    BARRIER()
    if STOP_AFTER == 'A3':
        return done()

    A = StageArena()
    dq = [A.f32(128, 16, 128) for _ in range(5)]
    q_a, q_bexp, q_eac, q_wdec, q_cdec = dq
    for qi in range(5):
        DMA("sp", dq[qi], dtq_s[qi].rearrange("(c p) n -> p c n", p=128), "sB_q%d" % qi, w=["dq"])
    D_bc = A.f32(128, 64)
    DMA("sp", D_bc, d_skip_d.partition_broadcast(128), "sB_D", w=["D_bc"])
    xtm = [A.bf16(128, 16, 512) for _ in range(2)]
    btm = [A.bf16(128, 16, 128) for _ in range(2)]
    BTt = [A.bf16(128, T) for _ in range(2)]
    CTt = [A.bf16(128, T) for _ in range(2)]
    normw = [A.f32(128, 512) for _ in range(2)]
    yacc = A.f32(128, 16, 512)
    S32 = [A.f32(128, 512) for _ in range(2)]
    Sbf = [A.bf16(128, 512) for _ in range(2)]
    cbm = [A.bf16(128, 128) for _ in range(2)]
    Lsb = [A.bf16(128, 8, 128) for _ in range(2)]
    mT = [A.bf16(128, 8, 128) for _ in range(2)]
    yoff = [A.f32(128, 512) for _ in range(2)]
    xw = [A.bf16(128, 512) for _ in range(2)]
    ztB = [A.bf16(128, 512) for _ in range(2)]
    yg = [A.f32(128, 512) for _ in range(2)]
    ygsq = A.f32(128, 512)
    yn = [A.bf16(128, 512) for _ in range(2)]
    ssq = [A.f32(128, 1) for _ in range(2)]
    srt = [A.f32(128, 1) for _ in range(2)]
    srs = [A.f32(128, 1) for _ in range(2)]
    yTsb = A.bf16(128, 4, T)
    stout = [A.f32(128, 4, 128) for _ in range(2)]
    h0t = [A.f32(128, 4, 128) for _ in range(2)]
    xs_tm_v = xs_tm_s.rearrange("(i p) c -> p i c", p=128)
    b_tm_v = b_tm_s.rearrange("(i p) c -> p i c", p=128)

    def loadB(g):
        s = g % 2
        DMA("sp", xtm[s], xs_tm_v[:, :, g * 512:(g + 1) * 512], "sB_x%d" % s, w=["xtm%d" % s])
        DMA("sp", btm[s], b_tm_v[:, :, g * 128:(g + 1) * 128], "sB_b%d" % s, w=["btm%d" % s])
        DMA("sp", BTt[s], bct_s[g * 128:(g + 1) * 128, :], "sB_BT%d" % s, w=["BT%d" % s])
        DMA("sp", CTt[s], bct_s[1024 + g * 128:1024 + (g + 1) * 128, :], "sB_CT%d" % s, w=["CT%d" % s])
        DMA("sp", normw[s], ssm_norm_w_d[g * 512:(g + 1) * 512].partition_broadcast(128), "sB_nw%d" % s, w=["normw%d" % s])

    loadB(0)
    it = [0]
    for g in range(8):
        gs = g % 2
        if g + 1 < 8:
            loadB(g + 1)
        X = xtm[gs]
        xr = "xtm%d" % gs
        for c in range(NT):
            TT("pool", yacc[:, c, :].rearrange("p (h q) -> p h q", q=64), X[:, c, :].rearrange("p (h q) -> p h q", q=64),
               D_bc[:, g * 8:(g + 1) * 8].unsqueeze(2).to_broadcast([128, 8, 64]), ALU.mult, r=[xr, "D_bc"], w=["yacc%d" % c])
        for d in range(2):
            U = uf if d == 0 else ub
            ures = "uf" if d == 0 else "ub"
            col0 = d * 64 + g * 8
            order = list(range(NT)) if d == 0 else list(range(NT - 1, -1, -1))
            for c in order:
                first = (c == 0) if d == 0 else (c == NT - 1)
                seg_start = (c % 2 == 0) if d == 0 else (c % 2 == 1)
                seg_end = (c % 2 == 1) if d == 0 else (c % 2 == 0)
                sres = "S32_%d" % d
                bres = "Sbf_%d" % d
                csl = slice(c * 128, (c + 1) * 128)
                if first:
                    hs = (g * 2 + d) % 2
                    DMA("sp", h0t[hs], h0_d[d, g * 8:(g + 1) * 8].rearrange("(jj h2) q n -> (h2 q) jj n", h2=2),
                        "sB_h0%d" % hs, w=["h0t%d" % hs])
                    for jj in range(4):
                        TR(ps[6][:, jj * 128:(jj + 1) * 128], h0t[hs][:, jj, :], ident, r=["h0t%d" % hs, "ident"], w=["ps6"])
                    CP("dve", S32[d], ps[6][:, :], r=["ps6"], w=[sres])
                    CP("act", Sbf[d], ps[6][:, :], r=["ps6"], w=[bres])
                elif seg_start:
                    TS("dve", S32[d], S32[d], carry[:, 0:1], ALU.mult, r=[sres, "carry"], w=[sres])
                    CP("act", Sbf[d], S32[d], r=[sres], w=[bres])
                k2 = it[0] % 2
                it[0] += 1
                MM(ps[0][:, 0:128], BTt[gs][:, csl], CTt[gs][:, csl], r=["BT%d" % gs, "CT%d" % gs], w=["ps0"])
                TT("dve", cbm[k2], ps[0][:, 0:128], U, ALU.mult, r=["ps0", ures], w=["cbm%d" % k2])
                for half in range(2):
                    for jj in range(4):
                        j = half * 4 + jj
                        MM(ps[1 + half][:, jj * 128:(jj + 1) * 128], q_a[:, c, col0 + j:col0 + j + 1].to_broadcast([128, 128]), U,
                           r=["dq", ures], w=["ps%d" % (1 + half)])
                    for jj in range(4):
                        j = half * 4 + jj
                        ACT(Lsb[k2][:, j, :], ps[1 + half][:, jj * 128:(jj + 1) * 128], AF.Exp,
                            r=["ps%d" % (1 + half), "dq"], w=["L%d_%d" % (k2, half)], bias=q_bexp[:, c, col0 + j:col0 + j + 1])
                    STT("dve", mT[k2][:, half * 4:(half + 1) * 4, :], Lsb[k2][:, half * 4:(half + 1) * 4, :], 1e30,
                        cbm[k2].unsqueeze(1).to_broadcast([128, 4, 128]), ALU.min, ALU.mult,
                        r=["L%d_%d" % (k2, half), "cbm%d" % k2], w=["mT%d_%d" % (k2, half)])
                MM(ps[4][:, :], CTt[gs][:, csl], Sbf[d], r=["CT%d" % gs, bres], w=["ps4"])
                for j in range(8):
                    MM(ps[3][:, j * 64:(j + 1) * 64], mT[k2][:, j, :], X[:, c, j * 64:(j + 1) * 64],
                       r=["mT%d_%d" % (k2, j // 4), xr], w=["ps3"])
                TT("dve", yoff[k2].rearrange("p (h q) -> p h q", q=64), ps[4][:, :].rearrange("p (h q) -> p h q", q=64),
                   q_eac[:, c, col0:col0 + 8].unsqueeze(2).to_broadcast([128, 8, 64]), ALU.mult, r=["ps4", "dq"], w=["yoff%d" % k2])
                TT("dve", yoff[k2], yoff[k2], ps[3][:, :], ALU.add, r=["yoff%d" % k2, "ps3"], w=["yoff%d" % k2])
                TT("pool", yacc[:, c, :], yacc[:, c, :], yoff[k2], ALU.add, r=["yacc%d" % c, "yoff%d" % k2], w=["yacc%d" % c])
                TT("pool", xw[k2].rearrange("p (h q) -> p h q", q=64), X[:, c, :].rearrange("p (h q) -> p h q", q=64),
                   q_wdec[:, c, col0:col0 + 8].unsqueeze(2).to_broadcast([128, 8, 64]), ALU.mult, r=[xr, "dq"], w=["xw%d" % k2])
                MM(ps[5][:, :], btm[gs][:, c, :], xw[k2], r=["btm%d" % gs, "xw%d" % k2], w=["ps5"])
                TT("dve", S32[d].rearrange("p (h q) -> p h q", q=64), S32[d].rearrange("p (h q) -> p h q", q=64),
                   q_cdec[:, c, col0:col0 + 8].unsqueeze(2).to_broadcast([128, 8, 64]), ALU.mult, r=[sres, "dq"], w=[sres])
                TT("dve", S32[d], S32[d], ps[5][:, :], ALU.add, r=[sres, "ps5"], w=[sres])
                CP("act", Sbf[d], S32[d], r=[sres], w=[bres])
                if seg_end:
                    so = (c // 2 + d) % 2
                    for jj in range(4):
                        TR(ps[6][:, jj * 128:(jj + 1) * 128], S32[d][:, jj * 128:(jj + 1) * 128], ident, r=[sres, "ident"], w=["ps6"])
                    CP("act", stout[so], ps[6][:, :].rearrange("p (a b) -> p a b", b=128), r=["ps6"], w=["stout%d" % so])
                    DMA("sp", st_out_d[c // 2, d, g * 8:(g + 1) * 8].rearrange("(jj h2) q n -> (h2 q) jj n", h2=2), stout[so],
                        "out_st%d" % so, r=["stout%d" % so])
        for c in range(NT):
            k2 = c % 2
            DMA("sp", ztB[k2], zs_s[c * 128:(c + 1) * 128, g * 512:(g + 1) * 512], "sB_zt%d" % k2, w=["ztB%d" % k2])
            TT("dve", yg[k2], yacc[:, c, :], ztB[k2], ALU.mult, r=["yacc%d" % c, "ztB%d" % k2], w=["yg%d" % k2])
            ACT(ygsq, yg[k2], AF.Square, r=["yg%d" % k2], w=["ygsq", "ssq%d" % k2], accum=ssq[k2])
            ACT(srt[k2], ssq[k2], AF.Sqrt, r=["ssq%d" % k2], w=["srt%d" % k2], scale=1.0 / 512.0, bias=1e-6)
            RECIP(srs[k2], srt[k2], r=["srt%d" % k2], w=["srs%d" % k2])
            STT("dve", yn[k2], yg[k2], srs[k2][:, 0:1], normw[gs], ALU.mult, ALU.mult, r=["yg%d" % k2, "srs%d" % k2, "normw%d" % gs], w=["yn%d" % k2])
            for cc in range(4):
                TR(psb[7][:, cc * 128:(cc + 1) * 128], yn[k2][:, cc * 128:(cc + 1) * 128], identb, r=["yn%d" % k2, "identb"], w=["ps7"])
            CP("act", yTsb[:, :, c * 128:(c + 1) * 128], psb[7][:, 0:512].rearrange("p (a b) -> p a b", b=128), r=["ps7"], w=["yTsb"])
        DMA("sp", yT_s.rearrange("(cc p) t -> p cc t", p=128)[:, g * 4:(g + 1) * 4, :], yTsb, "sB_yT", r=["yTsb"])
    BARRIER()
    if STOP_AFTER == 'B':
        return done()

    A = StageArena()
    qnT = A.bf16(128, 4, T)
    ckvT = A.bf16(128, 4, NKEY)
    kra = A.bf16(73, NKEY)
    wq = A.bf16(128, 4, 3072)
    wkv = A.bf16(128, 4, 4096)
    cos2 = A.f32(64, T)
    sin2 = A.f32(64, T)
    cxl = [A.f32(128, 512) for _ in range(2)]
    cxk = A.f32(128, 4, 64)
    wqs = [A.bf16(128, 4, 64) for _ in range(2)]
    qn_h = [A.bf16(128, T) for _ in range(2)]
    qra = [A.bf16(73, T) for _ in range(2)]
    kn_h = [A.bf16(128, NKEY) for _ in range(2)]
    v_h = [A.bf16(128, 20, 128) for _ in range(2)]
    PT = [A.bf16(128, 512) for _ in range(4)]
    rden = A.f32(128, 512)
    oTh = [A.bf16(128, T) for _ in range(2)]
    rp1 = A.f32(64, 512)
    rp2 = A.f32(64, 512)
    DMA("sp", qnT, qnT_s.rearrange("(c p) t -> p c t", p=128), "sC_qnT", w=["qnT"])
    DMA("sp", ckvT[:, :, 0:T], ckvT_s.rearrange("(c p) t -> p c t", p=128), "sC_ckvT", w=["ckvT_own"])
    DMA("sp", kra[0:64, 0:T], krT_s, "sC_kr", w=["kra_own"])
    DMA("pool", kra[64:73, :], khot_d, "sC_khot", w=["kra_hot"])
    for s in range(2):
        DMA("pool", qra[s][64:73, :], qpen_d, "sC_qpen%d" % s, w=["qra_pen%d" % s])
    DMA("pool", wq[:, :, 0:1536], w_q_b_d.rearrange("(k p) n -> p k n", p=128)[:, :, 0:1536], "sC_wq0", w=["wq0"])
    DMA("pool", wq[:, :, 1536:3072], w_q_b_d.rearrange("(k p) n -> p k n", p=128)[:, :, 1536:3072], "sC_wq1", w=["wq1"])
    DMA("pool", wkv[:, :, 0:2048], w_kv_b_d.rearrange("(k p) n -> p k n", p=128)[:, :, 0:2048], "sC_wkv0", w=["wkv0"])
    DMA("pool", wkv[:, :, 2048:4096], w_kv_b_d.rearrange("(k p) n -> p k n", p=128)[:, :, 2048:4096], "sC_wkv1", w=["wkv1"])
    DMA("sp", cos2, cos2_d, "sC_cos", w=["cos2"])
    DMA("sp", sin2, sin2_d, "sC_sin", w=["sin2"])
    for kt in range(4):
        s = kt % 2
        DMA("sp", cxl[s], ctx_ckv_d[kt * 128:(kt + 1) * 128, :], "sC_cx%d" % s, w=["cxl%d" % s])
        for c in range(4):
            TR(ps[4][:, c * 128:(c + 1) * 128], cxl[s][:, c * 128:(c + 1) * 128], ident, r=["cxl%d" % s, "ident"], w=["ps4"])
        CP("dve", ckvT[:, :, T + kt * 128:T + (kt + 1) * 128], ps[4][:, :].rearrange("p (a b) -> p a b", b=128), r=["ps4"], w=["ckvT_ctx"])
    DMA("sp", cxk, ctx_kr_d.rearrange("(a p) c -> p a c", p=128), "sC_cxk", w=["cxk"])
    for kt in range(4):
        TR(ps[5][0:64, kt * 128:(kt + 1) * 128], cxk[:, kt, :], ident, r=["cxk", "ident"], w=["ps5"])
    CP("dve", kra[0:64, T:NKEY], ps[5][0:64, :], r=["ps5"], w=["kra_ctx"])
    ckr = ["ckvT_own", "ckvT_ctx"]
    krr = ["kra_own", "kra_hot", "kra_ctx"]
    for h in range(16):
        hs = h % 2
        wqr = "wq%d" % (h // 8)
        wkr_ = "wkv%d" % (h // 8)
        qc0 = h * 192
        kc0 = h * 256
        CP("pool", wqs[hs][:, :, 0:32], wq[:, :, qc0 + 160:qc0 + 192], r=[wqr], w=["wqs%d" % hs])
        CP("pool", wqs[hs][:, :, 32:64], wq[:, :, qc0 + 128:qc0 + 160], r=[wqr], w=["wqs%d" % hs])
        for tb in range(4):
            tsl = slice(tb * 512, (tb + 1) * 512)
            for k in range(4):
                MM(ps[4][:, :], wq[:, k, qc0:qc0 + 128], qnT[:, k, tsl], start=(k == 0), stop=(k == 3), r=[wqr, "qnT"], w=["ps4"])
            CP("act", qn_h[hs][:, tsl], ps[4][:, :], r=["ps4"], w=["qn_h%d" % hs])
            for k in range(4):
                MM(ps[5][0:64, :], wq[:, k, qc0 + 128:qc0 + 192], qnT[:, k, tsl], start=(k == 0), stop=(k == 3), r=[wqr, "qnT"], w=["ps5"])
            for k in range(4):
                MM(ps[6][0:64, :], wqs[hs][:, k, :], qnT[:, k, tsl], start=(k == 0), stop=(k == 3), r=["wqs%d" % hs, "qnT"], w=["ps6"])
            TT("dve", rp1, ps[5][0:64, :], cos2[:, tsl], ALU.mult, r=["ps5", "cos2"], w=["rp1"])
            TT("dve", rp2, ps[6][0:64, :], sin2[:, tsl], ALU.mult, r=["ps6", "sin2"], w=["rp2"])
            TT("dve", qra[hs][0:64, tsl], rp1, rp2, ALU.add, r=["rp1", "rp2"], w=["qra%d" % hs])
        for kb in range(5):
            ksl = slice(kb * 512, (kb + 1) * 512)
            for k in range(4):
                MM(ps[7][:, :], wkv[:, k, kc0:kc0 + 128], ckvT[:, k, ksl], start=(k == 0), stop=(k == 3), r=[wkr_] + ckr, w=["ps7"])
            CP("act", kn_h[hs][:, ksl], ps[7][:, :], r=["ps7"], w=["kn_h%d" % hs])
        for kq in range(5):
            for kk in range(4):
                kt = kq * 4 + kk
                for k in range(4):
                    MM(ps[4][:, kk * 128:(kk + 1) * 128], ckvT[:, k, kt * 128:(kt + 1) * 128], wkv[:, k, kc0 + 128:kc0 + 256],
                       start=(k == 0), stop=(k == 3), r=[wkr_] + ckr, w=["ps4"])
            CP("dve", v_h[hs][:, kq * 4:(kq + 1) * 4, :], ps[4][:, :].rearrange("p (a b) -> p a b", b=128), r=["ps4"], w=["v_h%d" % hs])
        pti = 0
        for qb in range(4):
            qsl = slice(qb * 512, (qb + 1) * 512)
            for kt in range(20):
                sb = kt % 2
                p_ = pti % 4
                pti += 1
                MM(ps[sb][:, :], kn_h[hs][:, kt * 128:(kt + 1) * 128], qn_h[hs][:, qsl], start=True, stop=False,
                   r=["kn_h%d" % hs, "qn_h%d" % hs], w=["ps%d" % sb])
                MM(ps[sb][:, :], kra[0:73, kt * 128:(kt + 1) * 128], qra[hs][0:73, qsl], start=False, stop=True,
                   r=krr + ["qra%d" % hs, "qra_pen%d" % hs], w=["ps%d" % sb])
                ACT(PT[p_], ps[sb][:, :], AF.Exp, r=["ps%d" % sb], w=["PT%d" % p_], scale=SCALE)
                MM(ps[2][:, :], v_h[hs][:, kt, :], PT[p_], start=(kt == 0), stop=(kt == 19), r=["v_h%d" % hs, "PT%d" % p_], w=["ps2"])
                MM(ps[3][:, :], onesb, PT[p_], start=(kt == 0), stop=(kt == 19), r=["onesb", "PT%d" % p_], w=["ps3"])
            RECIP(rden, ps[3][:, :], r=["ps3"], w=["rden"])
            TT("dve", oTh[hs][:, qsl], ps[2][:, :], rden, ALU.mult, r=["ps2", "rden"], w=["oTh%d" % hs])
        DMA("sp", oT_s[h * 128:(h + 1) * 128, :], oTh[hs], "sC_oT%d" % hs, r=["oTh%d" % hs])
    BARRIER()
    if STOP_AFTER == 'C':
        return done()

    A = StageArena()
    g1bc = A.f32(128, D)
    lnw = A.f32(128, D)
    lnb = A.f32(128, D)
    DMA("sp", g1bc, g1_s.partition_broadcast(128), "sD_g1", w=["g1bc"])
    DMA("sp", lnw, ln1_w_d.partition_broadcast(128), "sD_lnw", w=["lnw"])
    DMA("sp", lnb, ln1_b_d.partition_broadcast(128), "sD_lnb", w=["lnb"])
    yTb = A.bf16(128, 32, 512)
    oTb = A.bf16(128, 16, 512)
    gsl = [A.bf16(128, 2, 2, 512) for _ in range(2)]
    mrg = A.bf16(128, 16, 512)
    NSD = 2
    wD = [A.bf16(128, 32, 256) for _ in range(NSD)]
    t1 = [A.f32(128, 512) for _ in range(2)]
    t2 = [A.f32(128, 512) for _ in range(2)]
    vt = A.f32(128, 4, D)
    xin = [A.f32(128, D)]
    junk = A.f32(128, D)
    st1 = [A.f32(128, 4) for _ in range(2)]
    w_ssm_v = w_ssm_out_d.rearrange("(k p) n -> p k n", p=128)
    w_mla_v = w_mla_out_d.rearrange("(k p) n -> p k n", p=128)
    w_o_v = w_o_d.rearrange("(k p) n -> p k n", p=128)
    dgroups = []
    for tb in range(4):
        for m2 in range(8):
            dgroups.append(("ssm", m2, tb))
            dgroups.append(("mla", m2, tb))
        for fb in range(4):
            dgroups.append(("wo", fb, tb))

    def issue_d(n):
        if n < len(dgroups):
            s = n % NSD
            kind, m, _ = dgroups[n]
            if kind == "ssm":
                DMA("pool", wD[s], w_ssm_v[:, :, m * 256:(m + 1) * 256], "sD_w%d" % s, w=["wD%d" % s])
            elif kind == "mla":
                DMA("pool", wD[s][:, 0:16, :], w_mla_v[:, :, m * 256:(m + 1) * 256], "sD_w%d" % s, w=["wD%d" % s])
            else:
                DMA("pool", wD[s].rearrange("p a b -> p (a b)").rearrange("p (a b) -> p a b", b=512), w_o_v[:, :, m * 512:(m + 1) * 512],
                    "sD_w%d" % s, w=["wD%d" % s])

    for n in range(NSD - 1):
        issue_d(n)
    tctr = 0
    for n, (kind, m, tb) in enumerate(dgroups):
        issue_d(n + NSD - 1)
        s = n % NSD
        wres = "wD%d" % s
        tsl = slice(tb * 512, (tb + 1) * 512)
        if kind == "ssm" and m == 0:
            DMA("sp", yTb, yT_s.rearrange("(c p) t -> p c t", p=128)[:, :, tsl], "sD_yT", w=["yTb"])
            DMA("sp", oTb, oT_s.rearrange("(c p) t -> p c t", p=128)[:, :, tsl], "sD_oT", w=["oTb"])
        if kind == "ssm":
            gq = m % 2
            gT_v = gT_s.rearrange("(c p) t -> p c t", p=128)
            DMA("sp", gsl[gq][:, 0, :, :], gT_v[:, m * 2:m * 2 + 2, tsl], "sD_gs%d" % gq, w=["gsl%d" % gq])
            DMA("sp", gsl[gq][:, 1, :, :], gT_v[:, 16 + m * 2:16 + m * 2 + 2, tsl], "sD_gm%d" % gq, w=["gsl%d" % gq])
            for mm in range(2):
                mc = m * 2 + mm
                b = mm
                for k in range(32):
                    MM(ps[b][:, :], wD[s][:, k, mm * 128:(mm + 1) * 128], yTb[:, k, :], start=(k == 0), stop=(k == 31),
                       r=[wres, "yTb"], w=["ps%d" % b])
                TT("dve", t1[mm], ps[b][:, :], gsl[gq][:, 0, mm, :], ALU.mult, r=["ps%d" % b, "gsl%d" % gq], w=["t1_%d" % mm])
        elif kind == "mla":
            gq = m % 2
            for mm in range(2):
                mc = m * 2 + mm
                b = 2 + mm
                for k in range(16):
                    MM(ps[b][:, :], wD[s][:, k, mm * 128:(mm + 1) * 128], oTb[:, k, :], start=(k == 0), stop=(k == 15),
                       r=[wres, "oTb"], w=["ps%d" % b])
                TT("dve", t2[mm], ps[b][:, :], gsl[gq][:, 1, mm, :], ALU.mult, r=["ps%d" % b, "gsl%d" % gq], w=["t2_%d" % mm])
                TT("pool", mrg[:, mc, :], t1[mm], t2[mm], ALU.add, r=["t1_%d" % mm, "t2_%d" % mm], w=["mrg"])
        else:
            fb = m
            fsl = slice(fb * 512, (fb + 1) * 512)
            wv = wD[s].rearrange("p a b -> p (a b)").rearrange("p (a b) -> p a b", b=512)
            for i4 in range(4):
                i = tb * 4 + i4
                b = 4 + (i4 % 2)
                if fb == 0:
                    DMA("sp", xin[0], x_d[i * 128:(i + 1) * 128, :], "sD_x0", w=["xin0"])
                for k in range(16):
                    MM(ps[b][:, :], mrg[:, k, i4 * 128:(i4 + 1) * 128], wv[:, k, :], start=(k == 0), stop=(k == 15),
                       r=["mrg", wres], w=["ps%d" % b])
                tq = tctr % 2
                tctr += 1
                TT("dve", t1[tq], ps[b][:, :], g1bc[:, fsl], ALU.mult, r=["ps%d" % b, "g1bc"], w=["t1_%d" % tq])
                if fb == 0:
                    TS("pool", vt[:, i4, :], xin[0], ALU_ALPHA, ALU.mult, r=["xin0"], w=["vt%d" % i4])
                TT("pool", vt[:, i4, fsl], vt[:, i4, fsl], t1[tq], ALU.add, r=["vt%d" % i4, "t1_%d" % tq], w=["vt%d" % i4])
            if fb == 3:
                for i4 in range(4):
                    i = tb * 4 + i4
                    q2 = i4 % 2
                    V = vt[:, i4, :]
                    st = st1[q2]
                    TS("dve", junk, V, 1.0, ALU.mult, 0.0, ALU.add, r=["vt%d" % i4], w=["junk", "st%d" % q2], accum=st[:, 0:1])
                    TS("dve", st[:, 1:2], st[:, 0:1], -1.0 / D, ALU.mult, r=["st%d" % q2], w=["st%d" % q2])
                    ACT(junk, V, AF.Square, r=["vt%d" % i4, "st%d" % q2], w=["junk", "st%d" % q2], bias=st[:, 1:2], accum=st[:, 2:3])
                    ACT(st[:, 3:4], st[:, 2:3], AF.Sqrt, r=["st%d" % q2], w=["st%d" % q2], scale=1.0 / D, bias=1e-5)
                    RECIP(st[:, 3:4], st[:, 3:4], r=["st%d" % q2], w=["st%d" % q2])
                    TS("dve", V, V, st[:, 1:2], ALU.add, st[:, 3:4], ALU.mult, r=["vt%d" % i4, "st%d" % q2], w=["vt%d" % i4])
                    TT("dve", V, V, lnw, ALU.mult, r=["vt%d" % i4, "lnw"], w=["vt%d" % i4])
                    TT("pool", V, V, lnb, ALU.add, r=["vt%d" % i4, "lnb"], w=["vt%d" % i4])
                    DMA("sp", x1_s[i * 128:(i + 1) * 128, :], V, "sD_x1_%d" % i4, r=["vt%d" % i4])
    BARRIER()
    if STOP_AFTER == 'D':
        return done()

    A = StageArena()
    h2T = A.bf16(128, 16, 1024)
    accT = A.f32(128, 16, 1024)
    bg = A.f32(128, 32, 16, 2)
    bd = A.f32(128, 32, 16)
    comb = A.f32(128, 8, 32)
    rw = A.f32(128, 16, 32)
    rb_bc = A.f32(128, 32)
    NSE = 3
    wE = [A.bf16(128, 16, 256) for _ in range(NSE)]
    lg = A.f32(128, 32)
    m8 = A.f32(128, 8)
    nmx = A.f32(128, 1)
    msk = A.f32(128, 32)
    ex = A.f32(128, 32)
    ssum = A.f32(128, 1)
    mark_e = A.off
    h32 = A.f32(128, 16, 128)
    x1l = [A.f32(128, D) for _ in range(2)]
    A.off = mark_e
    actT = A.bf16(128, 16, 1024)
    combs = A.f32(128, 1024)
    gsb = [A.f32(128, 512) for _ in range(2)]
    sgb = [A.f32(128, 512) for _ in range(2)]
    usb = [A.f32(128, 512) for _ in range(2)]
    tcb = [A.f32(128, 512) for _ in range(2)]
    A.off = mark_e
    lnw2 = A.f32(128, D)
    lnb2 = A.f32(128, D)
    v2 = [A.f32(128, D) for _ in range(2)]
    x1e = [A.f32(128, D)]
    junk2 = A.f32(128, D)
    st2 = [A.f32(128, 4) for _ in range(2)]
    for e4 in range(4):
        DMA("sp", bg[:, e4 * 8:(e4 + 1) * 8, :, :], b_gu_d[e4 * 8:(e4 + 1) * 8, :].rearrange("e (c p two) -> p e c two", p=128, two=2),
            "sE_bg%d" % e4, w=["bg"], slow=True)
        DMA("sp", bd[:, e4 * 8:(e4 + 1) * 8, :], b_down_d[e4 * 8:(e4 + 1) * 8, :].rearrange("e (c p) -> p e c", p=128),
            "sE_bd%d" % e4, w=["bd"], slow=True)
    DMA("sp", rw, router_w_d.rearrange("(k p) e -> p k e", p=128), "sE_rw", w=["rw"])
    DMA("sp", rb_bc, router_b_d.partition_broadcast(128), "sE_rb", w=["rb"])
    w_gu_v = w_gu_d.rearrange("e (k p) n -> e p k n", p=128)
    w_dn_v = w_down_d.rearrange("e (k p) n -> e p k n", p=128)
    egroups = []
    for blk in range(2):
        for e in range(32):
            for m in range(16):
                egroups.append(("gu", blk, e, m))
            for j2 in range(8):
                egroups.append(("dn", blk, e, j2))

    def issue_e(n):
        if n < len(egroups):
            s = n % NSE
            kind, _, e, m = egroups[n]
            if kind == "gu":
                DMA("pool", wE[s], w_gu_v[e][:, :, m * 256:(m + 1) * 256], "sE_w%d" % s, w=["wE%d" % s])
            else:
                DMA("pool", wE[s], w_dn_v[e][:, :, m * 256:(m + 1) * 256], "sE_w%d" % s, w=["wE%d" % s])

    n = 0
    for blk in range(2):
        for i8 in range(8):
            i = blk * 8 + i8
            xs_ = i % 2
            DMA("sp", x1l[xs_], x1_s[i * 128:(i + 1) * 128, :], "sE_x1_%d" % xs_, w=["x1l%d" % xs_])
            for cq in range(4):
                b = 4 + cq % 2
                for cc in range(4):
                    c = cq * 4 + cc
                    TR(ps[b][:, cc * 128:(cc + 1) * 128], x1l[xs_][:, c * 128:(c + 1) * 128], ident, r=["x1l%d" % xs_, "ident"], w=["ps%d" % b])
                for cc in range(4):
                    c = cq * 4 + cc
                    if cc % 2 == 0:
                        ACT(h32[:, c, :], ps[b][:, cc * 128:(cc + 1) * 128], AF.Identity, r=["ps%d" % b, "sc2p", "mod"], w=["h32_%d" % c],
                            scale=sc2p[:, c:c + 1], bias=sh2[:, c:c + 1])
                    else:
                        TS("dve", h32[:, c, :], ps[b][:, cc * 128:(cc + 1) * 128], sc2p[:, c:c + 1], ALU.mult, sh2[:, c:c + 1], ALU.add,
                           r=["ps%d" % b, "sc2p", "mod"], w=["h32_%d" % c])
            CP("pool", h2T[:, :, i8 * 128:(i8 + 1) * 128], h32, r=["h32_%d" % c for c in range(16)], w=["h2T"])
            for k in range(16):
                MM(ps[6][:, 0:32], h32[:, k, :], rw[:, k, :], start=(k == 0), stop=(k == 15), r=["h32_%d" % k, "rw"], w=["ps6"])
            TT("dve", lg, ps[6][:, 0:32], rb_bc, ALU.add, r=["ps6", "rb"], w=["lg"])
            S.add("dve", lambda e: e.max(out=m8, in_=lg), ["lg"], ["m8"])
            TS("dve", msk, lg, m8[:, 3:4], ALU.is_ge, r=["lg", "m8"], w=["msk"])
            TS("dve", nmx, m8[:, 0:1], -1.0, ALU.mult, r=["m8"], w=["nmx"])
            ACT(ex, lg, AF.Exp, r=["lg", "nmx"], w=["ex"], bias=nmx[:, 0:1])
            STT("dve", ex, ex, 1.0, msk, ALU.mult, ALU.mult, r=["ex", "msk"], w=["ex", "ssum"], accum=ssum[:, 0:1])
            RECIP(ssum, ssum, r=["ssum"], w=["ssum"])
            TS("dve", comb[:, i8, :], ex, ssum[:, 0:1], ALU.mult, r=["ex", "ssum"], w=["comb"])
        BARRIER()
        if blk == 0:
            for q in range(NSE - 1):
                issue_e(q)
        for e in range(32):
            for i8 in range(8):
                b = 6 + (i8 // 4)
                MM(ps[b][:, (i8 % 4) * 128:(i8 % 4 + 1) * 128], comb[:, i8, e:e + 1].to_broadcast([128, 128]), ident,
                   r=["comb", "ident"], w=["ps%d" % b])
            CP("act", combs[:, 0:512], ps[6][:, :], r=["ps6"], w=["combs"])
            CP("act", combs[:, 512:1024], ps[7][:, :], r=["ps7"], w=["combs"])
            for m in range(16):
                issue_e(n + NSE - 1)
                s = n % NSE
                n += 1
                wres = "wE%d" % s
                for t2_ in range(2):
                    tsl = slice(t2_ * 512, (t2_ + 1) * 512)
                    q2 = (m * 2 + t2_) % 2
                    for k in range(16):
                        MM(ps[0 + t2_][:, :], wE[s][:, k, 0:256:2], h2T[:, k, tsl], start=(k == 0), stop=(k == 15), r=[wres, "h2T"], w=["ps%d" % t2_])
                    for k in range(16):
                        MM(ps[2 + t2_][:, :], wE[s][:, k, 1:256:2], h2T[:, k, tsl], start=(k == 0), stop=(k == 15), r=[wres, "h2T"], w=["ps%d" % (2 + t2_)])
                    TS("dve", gsb[q2], ps[0 + t2_][:, :], bg[:, e, m, 0:1], ALU.add, 7.0, ALU.min, r=["ps%d" % t2_, "bg"], w=["gsb%d" % q2])
                    ACT(sgb[q2], gsb[q2], AF.Sigmoid, r=["gsb%d" % q2], w=["sgb%d" % q2], scale=1.702)
                    TS("dve", usb[q2], ps[2 + t2_][:, :], bg[:, e, m, 1:2], ALU.add, 7.0, ALU.min, r=["ps%d" % (2 + t2_), "bg"], w=["usb%d" % q2])
                    TS("dve", usb[q2], usb[q2], -7.0, ALU.max, 1.0, ALU.add, r=["usb%d" % q2], w=["usb%d" % q2])
                    TT("dve", gsb[q2], gsb[q2], sgb[q2], ALU.mult, r=["gsb%d" % q2, "sgb%d" % q2], w=["gsb%d" % q2])
                    TT("dve", actT[:, m, tsl], gsb[q2], usb[q2], ALU.mult, r=["gsb%d" % q2, "usb%d" % q2], w=["actT%d" % m])
            ar = ["actT%d" % m for m in range(16)]
            for j2 in range(8):
                issue_e(n + NSE - 1)
                s = n % NSE
                n += 1
                wres = "wE%d" % s
                for jj in range(2):
                    j = j2 * 2 + jj
                    for t2_ in range(2):
                        tsl = slice(t2_ * 512, (t2_ + 1) * 512)
                        b = 4 + (jj * 2 + t2_) % 2
                        q2 = (jj * 2 + t2_) % 2
                        for k in range(16):
                            MM(ps[b][:, :], wE[s][:, k, jj * 128:(jj + 1) * 128], actT[:, k, tsl], start=(k == 0), stop=(k == 15),
                               r=[wres] + ar, w=["ps%d" % b])
                        if e == 0:
                            STT("dve", accT[:, j, tsl], ps[b][:, :], bd[:, e, j:j + 1], combs[:, tsl], ALU.add, ALU.mult,
                                r=["ps%d" % b, "bd", "combs"], w=["accT%d" % j])
                        else:
                            STT("dve", tcb[q2], ps[b][:, :], bd[:, e, j:j + 1], combs[:, tsl], ALU.add, ALU.mult,
                                r=["ps%d" % b, "bd", "combs"], w=["tcb%d" % q2])
                            TT("pool", accT[:, j, tsl], accT[:, j, tsl], tcb[q2], ALU.add, r=["accT%d" % j, "tcb%d" % q2], w=["accT%d" % j])
        accr = ["accT%d" % j for j in range(16)]
        for j in range(16):
            TS("dve", accT[:, j, :], accT[:, j, :], g2c[:, j:j + 1], ALU.mult, r=["accT%d" % j, "mod"], w=["accT%d" % j])
        BARRIER()
        alias_r = []
        DMA("sp", lnw2, ln2_w_d.partition_broadcast(128), "sE_lnw", r=[], w=["lnw2"])
        DMA("sp", lnb2, ln2_b_d.partition_broadcast(128), "sE_lnb", r=[], w=["lnb2"])
        for i8 in range(8):
            i = blk * 8 + i8
            q2 = i8 % 2
            DMA("sp", x1e[0], x1_s[i * 128:(i + 1) * 128, :], "sE_x1e0", w=["x1e0"])
            for cq in range(4):
                b = cq % 2
                for cc in range(4):
                    c = cq * 4 + cc
                    TR(ps[b][:, cc * 128:(cc + 1) * 128], accT[:, c, i8 * 128:(i8 + 1) * 128], ident, r=["accT%d" % c, "ident"], w=["ps%d" % b])
                STT("dve", v2[q2][:, cq * 512:(cq + 1) * 512], x1e[0][:, cq * 512:(cq + 1) * 512], ALU_ALPHA, ps[b][:, :], ALU.mult, ALU.add,
                    r=["x1e0", "ps%d" % b], w=["v2_%d" % q2])
            V = v2[q2]
            st = st2[q2]
            vr = "v2_%d" % q2
            sr = "st2_%d" % q2
            TS("dve", junk2, V, 1.0, ALU.mult, 0.0, ALU.add, r=[vr], w=["junk2", sr], accum=st[:, 0:1])
            TS("dve", st[:, 1:2], st[:, 0:1], -1.0 / D, ALU.mult, r=[sr], w=[sr])
            ACT(junk2, V, AF.Square, r=[vr, sr], w=["junk2", sr], bias=st[:, 1:2], accum=st[:, 2:3])
            ACT(st[:, 3:4], st[:, 2:3], AF.Sqrt, r=[sr], w=[sr], scale=1.0 / D, bias=1e-5)
            RECIP(st[:, 3:4], st[:, 3:4], r=[sr], w=[sr])
            TS("dve", V, V, st[:, 1:2], ALU.add, st[:, 3:4], ALU.mult, r=[vr, sr], w=[vr])
            TT("dve", V, V, lnw2, ALU.mult, r=[vr, "lnw2"], w=[vr])
            TT("pool", V, V, lnb2, ALU.add, r=[vr, "lnb2"], w=[vr])
            DMA("sp", y_d[i * 128:(i + 1) * 128, :], V, "out_y%d" % q2, r=[vr])
        BARRIER()

    return done()


ALU_ALPHA = ALPHA

_CACHE = {}


def _rope_tables():
    n_tok = 2048
    rows = n_tok // 64
    row = np.repeat(np.arange(rows, dtype=np.float32), 64)
    col = np.tile(np.arange(64, dtype=np.float32), rows)
    n_freq = 16
    inv = (np.float32(10000.0) ** (-np.arange(n_freq, dtype=np.float32) / n_freq)).astype(np.float32)
    ang = np.concatenate([row[:, None] * inv, col[:, None] * inv], -1).astype(np.float32)
    cos = np.cos(ang).astype(np.float32)
    sin = np.sin(ang).astype(np.float32)
    cos2 = np.concatenate([cos, cos], 1).T.copy()
    sin2 = np.concatenate([-sin, sin], 1).T.copy()
    return cos2, sin2


def kernel(x_prompt, x_sample, cache_mla_ckv, cache_mla_krope, state_ssm, c, c_ctx,
           w_ada, b_ada, w_in, conv_w, conv_b, a_log, dt_bias, d_skip, ssm_norm_w, w_ssm_out,
           q_a_norm_w, w_q_b, kv_a_norm_w, w_kv_b, w_mla_out, w_o, ln1_w, ln1_b,
           router_w, router_b, w_gu, b_gu, w_down, b_down, ln2_w, ln2_b):
    f = lambda a: np.ascontiguousarray(np.asarray(a, dtype=np.float32))
    if "nc" not in _CACHE:
        _CACHE["nc"] = build_program()
    nc, _ = _CACHE["nc"]
    shared = {
        "w_ada": f(w_ada[0]), "b_ada": f(b_ada[0]), "w_in": f(w_in[0]), "conv_w": f(conv_w[0]), "conv_b": f(conv_b[0]),
        "a_log": f(a_log[0]).reshape(128), "dt_bias": f(dt_bias[0]).reshape(128), "d_skip": f(d_skip[0]),
        "ssm_norm_w": f(ssm_norm_w[0]), "w_ssm_out": f(w_ssm_out[0]), "q_a_norm_w": f(q_a_norm_w[0]), "w_q_b": f(w_q_b[0]),
        "kv_a_norm_w": f(kv_a_norm_w[0]), "w_kv_b": f(w_kv_b[0]), "w_mla_out": f(w_mla_out[0]), "w_o": f(w_o[0]),
        "ln1_w": f(ln1_w[0]), "ln1_b": f(ln1_b[0]), "router_w": f(router_w[0]), "router_b": f(router_b[0]),
        "w_gu": f(w_gu[0]) if STOP_AFTER is None else f(w_gu[0][:1]), "b_gu": f(b_gu[0]),
        "w_down": f(w_down[0]) if STOP_AFTER is None else f(w_down[0][:1]), "b_down": f(b_down[0]),
        "ln2_w": f(ln2_w[0]), "ln2_b": f(ln2_b[0]),
        "ident": np.eye(128, dtype=np.float32),
        "uf": np.triu(np.ones((128, 128), np.float32)),
        "ub": np.tril(np.ones((128, 128), np.float32)),
        "ones": np.ones((128, 128), np.float32),
    }
    khot = np.zeros((9, NKEY), np.float32)
    for t in range(T):
        khot[t // 256, t] = 1.0
    khot[8, T:] = 1.0
    shared["khot"] = khot
    cos2, sin2 = _rope_tables()
    pen_prompt = np.full((9, T), -16384.0, np.float32)
    for t in range(T):
        pen_prompt[t // 256, t] = 0.0
    in_maps = []
    for core in range(8):
        m = dict(shared)
        if core < 4:
            b = core
            m["x"] = f(x_sample[b])
            m["cond"] = f(c[b])
            m["ctx_ckv"] = f(cache_mla_ckv[b, 0])
            m["ctx_kr"] = f(cache_mla_krope[b, 0])
            m["h0"] = f(state_ssm[b, 0])
            m["carry"] = np.ones(128, np.float32)
            m["cos2"] = cos2
            m["sin2"] = sin2
            m["qpen"] = np.zeros((9, T), np.float32)
        else:
            s0 = (core - 4) * 8
            m["x"] = f(x_prompt[s0:s0 + 8]).reshape(T, D)
            m["cond"] = f(c_ctx)
            m["ctx_ckv"] = np.zeros((512, 512), np.float32)
            m["ctx_kr"] = np.zeros((512, 64), np.float32)
            m["h0"] = np.zeros((2, 64, 64, 128), np.float32)
            m["carry"] = np.zeros(128, np.float32)
            m["cos2"] = np.ones((64, T), np.float32)
            m["sin2"] = np.zeros((64, T), np.float32)
            m["qpen"] = pen_prompt
        in_maps.append(m)
    if DEBUG_CORES is not None:
        sub = [in_maps[i] for i in DEBUG_CORES]
        res = run_bass_kernel_spmd(nc, sub, core_ids=list(range(len(sub))))
        _CACHE["last"] = {c: res.results[j] for j, c in enumerate(DEBUG_CORES)}
        return None
    res = run_bass_kernel_spmd(nc, in_maps, core_ids=list(range(8)))
    R = res.results
    _CACHE["last"] = R
    y_s = np.stack([R[i]["y"] for i in range(4)], 0)
    y_p = np.concatenate([R[i]["y"].reshape(8, 256, D) for i in range(4, 8)], 0)
    ckv = np.concatenate([R[i]["ckv_out"].reshape(8, 1, 256, 512) for i in range(4, 8)], 0)
    kr = np.concatenate([R[i]["kr_out"].reshape(8, 1, 256, 64) for i in range(4, 8)], 0)
    st = np.concatenate([R[i]["st_out"].reshape(8, 1, 2, 64, 64, 128) for i in range(4, 8)], 0)
    return (y_p.astype(np.float32), y_s.astype(np.float32), ckv.astype(np.float32), kr.astype(np.float32), st.astype(np.float32))
```

```python
import contextlib
import numpy as np
import concourse.bass as bass
import concourse.mybir as mybir
from concourse.bass_utils import run_bass_kernel_spmd

F32 = mybir.dt.float32
BF16 = mybir.dt.bfloat16
AF = mybir.ActivationFunctionType
ALU = mybir.AluOpType

DEBUG = False
STOP_AFTER = None
DEBUG_CORES = None
DEBUG_KEEP = None

T = 2048
D = 2048
NT = 16
KC = 16
ALPHA = 2.0 ** 0.25
SCALE = 192.0 ** -0.5
NKEY = 2560
COMPUTE = ("pe", "act", "dve", "pool")


class Op:
    __slots__ = ("eng", "fn", "deps", "dom", "seq", "waits", "vc", "idx")


class Sched:
    def __init__(self, nc):
        self.nc = nc
        self.ops = []
        self.res_w = {}
        self.res_r = {}
        self.dom_count = {}
        self.dom_ops = {}
        self.bar_idx = None
        self.bar_start = 0
        self.barriers = []

    def add(self, eng, fn, reads=(), writes=(), key=None):
        o = Op()
        o.idx = len(self.ops)
        o.eng = eng
        o.fn = fn
        o.dom = key if key is not None else eng
        assert key is not None or eng in COMPUTE, (eng, key)
        extra = [r for r in reads if r[:2] == "ps" and r[2:].isdigit()]
        if extra:
            writes = list(writes) + extra
        deps = set()
        if self.bar_idx is not None:
            deps.add(self.bar_idx)
        for r in reads:
            w = self.res_w.get(r)
            if w is not None:
                deps.add(w)
        for w_ in writes:
            w = self.res_w.get(w_)
            if w is not None:
                deps.add(w)
            for r in self.res_r.get(w_, ()):
                deps.add(r)
        for r in reads:
            self.res_r.setdefault(r, []).append(o.idx)
        for w_ in writes:
            self.res_w[w_] = o.idx
            self.res_r[w_] = []
        deps.discard(o.idx)
        o.deps = deps
        c = self.dom_count.get(o.dom, 0) + 1
        self.dom_count[o.dom] = c
        o.seq = c
        self.dom_ops.setdefault(o.dom, []).append(o.idx)
        self.ops.append(o)
        return o

    def barrier(self, fn):
        o = self.add("dve", fn)
        last = {}
        for i in range(self.bar_start, o.idx):
            p = self.ops[i]
            last[p.dom] = i
        o.deps = set(last.values())
        if self.bar_idx is not None:
            o.deps.add(self.bar_idx)
        self.bar_idx = o.idx
        self.bar_start = o.idx
        self.barriers.append(o.idx)
        self.res_w = {}
        self.res_r = {}

    def finalize(self, final_wait_prefix="out_"):
        ops = self.ops
        know = {e: {} for e in ("pe", "act", "dve", "pool", "sp")}
        needed = set()
        for o in ops:
            k = know[o.eng]
            waits = {}
            for d in o.deps:
                dop = ops[d]
                if dop.dom == "pe" and o.eng == "pe":
                    continue
                if k.get(dop.dom, 0) >= dop.seq:
                    continue
                if waits.get(dop.dom, 0) < dop.seq:
                    waits[dop.dom] = dop.seq
            real = {}
            for dom, seq in waits.items():
                if k.get(dom, 0) >= seq:
                    continue
                k = dict(k)
                dop = ops[self.dom_ops[dom][seq - 1]]
                for kd, kv in dop.vc.items():
                    if k.get(kd, 0) < kv:
                        k[kd] = kv
                k[dom] = max(k.get(dom, 0), seq)
                needed.add((dom, seq))
                real[dom] = seq
            know[o.eng] = k
            o.waits = real
            o.vc = k
        self.final_waits = []
        for dom, c in self.dom_count.items():
            if isinstance(dom, str) and dom.startswith(final_wait_prefix):
                needed.add((dom, c))
                self.final_waits.append((dom, c))
        self.sig_val = {}
        for dom, lst in self.dom_ops.items():
            n = 0
            for s in range(1, len(lst) + 1):
                if (dom, s) in needed or dom not in COMPUTE:
                    n += 1
                    self.sig_val[(dom, s)] = n

    def emit(self):
        nc = self.nc
        doms = sorted({d for (d, _) in self.sig_val})
        import bisect
        first = {}
        last = {}
        for o in self.ops:
            if o.dom not in COMPUTE:
                first.setdefault(o.dom, o.idx)
                last[o.dom] = o.idx
        phys = {}
        base = {}
        pool_ = []
        nphys = 0
        for d in sorted(first, key=lambda k: first[k]):
            chosen = None
            for ent in pool_:
                bi = bisect.bisect_right(self.barriers, ent[0])
                if bi < len(self.barriers) and self.barriers[bi] <= first[d]:
                    chosen = ent
                    break
            if chosen is None:
                chosen = [0, 0, nphys]
                nphys += 1
                pool_.append(chosen)
            phys[d] = chosen[2]
            base[d] = chosen[1]
            chosen[0] = last[d]
            chosen[1] += self.dom_count[d]
        self.nphys = nphys
        with contextlib.ExitStack() as st:
            psem = [st.enter_context(nc.semaphore("sd%d" % i)) for i in range(nphys)]
            sems = {}
            for d in doms:
                if d in COMPUTE:
                    sems[d] = st.enter_context(nc.semaphore("s_" + str(d)))
                else:
                    sems[d] = psem[phys[d]]
            block = st.enter_context(nc.Block())
            by_eng = {e: [] for e in ("pe", "act", "dve", "pool", "sp")}
            for o in self.ops:
                by_eng[o.eng].append(o)

            def is_dma(dom):
                return dom not in COMPUTE

            def run(engine, lst, final=False):
                for o in lst:
                    for dom, seq in o.waits.items():
                        v = self.sig_val[(dom, seq)]
                        engine.wait_ge(sems[dom], (v + base[dom]) * 16 if is_dma(dom) else v)
                    ins = o.fn(engine)
                    if (o.dom, o.seq) in self.sig_val:
                        ins.then_inc(sems[o.dom], 16 if is_dma(o.dom) else 1)
                if final:
                    for dom, c in self.final_waits:
                        v = self.sig_val[(dom, c)]
                        engine.wait_ge(sems[dom], (v + base[dom]) * 16 if is_dma(dom) else v)

            @block.tensor
            def _(e):
                run(e, by_eng["pe"])

            @block.scalar
            def _(e):
                run(e, by_eng["act"])

            @block.vector
            def _(e):
                run(e, by_eng["dve"])

            @block.gpsimd
            def _(e):
                run(e, by_eng["pool"])

            @block.sync
            def _(e):
                run(e, by_eng["sp"], final=True)


class Arena:
    def __init__(self, t, nwords):
        self.t = t
        self.n = nwords
        self.off = 0

    def f32(self, *shape):
        n = int(np.prod(shape[1:]))
        n = (n + 7) // 8 * 8
        assert self.off + n <= self.n, ("SBUF arena overflow", self.off, n, self.n)
        ap = self.t[0:shape[0], self.off:self.off + int(np.prod(shape[1:]))]
        self.off += n
        return self._shape(ap, shape)

    def bf16(self, *shape):
        ne = int(np.prod(shape[1:]))
        n = (ne + 1) // 2
        n = (n + 7) // 8 * 8
        assert self.off + n <= self.n, ("SBUF arena overflow", self.off, n, self.n)
        ap = self.t[0:shape[0], self.off:self.off + (ne + 1) // 2].bitcast(BF16)
        if ne % 2:
            ap = ap[:, 0:ne]
        self.off += n
        return self._shape(ap, shape)

    @staticmethod
    def _shape(ap, shape):
        if len(shape) == 2:
            return ap
        if len(shape) == 3:
            return ap.rearrange("p (a b) -> p a b", b=shape[2])
        if len(shape) == 4:
            return ap.rearrange("p (a b c) -> p a b c", b=shape[2], c=shape[3])
        raise ValueError(shape)


def build_program():
    nc = bass.Bass("TRN2", target_bir_lowering=False)
    S = Sched(nc)

    def done():
        S.finalize()
        S.emit()
        return nc, S

    def din(name, shape, dt=F32):
        return nc.dram_tensor(name, list(shape), dt, kind="ExternalInput").ap()

    def dout(name, shape, dt=F32):
        return nc.dram_tensor(name, list(shape), dt, kind="ExternalOutput").ap()

    def dscr(name, shape, dt=F32):
        kind = "ExternalOutput" if (DEBUG and (DEBUG_KEEP is None or name in DEBUG_KEEP)) else "Internal"
        return nc.dram_tensor(name, list(shape), dt, kind=kind).ap()

    x_d = din("x", [T, D])
    cond_d = din("cond", [D])
    w_ada_d = din("w_ada", [D, 6 * D])
    b_ada_d = din("b_ada", [6 * D])
    w_in_d = din("w_in", [D, 15552])
    conv_w_d = din("conv_w", [3, 6144])
    conv_b_d = din("conv_b", [6144])
    a_log_d = din("a_log", [128])
    dt_bias_d = din("dt_bias", [128])
    d_skip_d = din("d_skip", [64])
    ssm_norm_w_d = din("ssm_norm_w", [4096])
    w_ssm_out_d = din("w_ssm_out", [4096, D])
    q_a_norm_w_d = din("q_a_norm_w", [512])
    w_q_b_d = din("w_q_b", [512, 3072])
    kv_a_norm_w_d = din("kv_a_norm_w", [512])
    w_kv_b_d = din("w_kv_b", [512, 4096])
    w_mla_out_d = din("w_mla_out", [D, D])
    w_o_d = din("w_o", [D, D])
    ln1_w_d = din("ln1_w", [D])
    ln1_b_d = din("ln1_b", [D])
    router_w_d = din("router_w", [D, 32])
    router_b_d = din("router_b", [32])
    NEW = 32 if STOP_AFTER is None else 1
    w_gu_d = din("w_gu", [NEW, D, 4096])
    b_gu_d = din("b_gu", [32, 4096])
    w_down_d = din("w_down", [NEW, D, D])
    b_down_d = din("b_down", [32, D])
    ln2_w_d = din("ln2_w", [D])
    ln2_b_d = din("ln2_b", [D])
    ctx_ckv_d = din("ctx_ckv", [512, 512])
    ctx_kr_d = din("ctx_kr", [512, 64])
    h0_d = din("h0", [2, 64, 64, 128])
    carry_d = din("carry", [128])
    cos2_d = din("cos2", [64, T])
    sin2_d = din("sin2", [64, T])
    khot_d = din("khot", [9, NKEY])
    qpen_d = din("qpen", [9, T])
    ident_d = din("ident", [128, 128])
    uf_d = din("uf", [128, 128])
    ub_d = din("ub", [128, 128])
    ones_d = din("ones", [128, 128])

    y_d = dout("y", [T, D])
    ckv_out_d = dout("ckv_out", [T, 512])
    kr_out_d = dout("kr_out", [T, 64])
    st_out_d = dout("st_out", [8, 2, 64, 64, 128])

    g1_s = dscr("g1_s", [D])
    xs_tm_s = dscr("xs_tm_s", [T, 4096], BF16)
    b_tm_s = dscr("b_tm_s", [T, 1024], BF16)
    bct_s = dscr("bct_s", [2048, T], BF16)
    zs_s = dscr("zs_s", [T, 4096], BF16)
    dtq_s = dscr("dtq_s", [5, T, 128])
    qnT_s = dscr("qnT_s", [512, T], BF16)
    ckvT_s = dscr("ckvT_s", [512, T], BF16)
    krT_s = dscr("krT_s", [64, T], BF16)
    gT_s = dscr("gT_s", [4096, T], BF16)
    yT_s = dscr("yT_s", [4096, T], BF16)
    oT_s = dscr("oT_s", [2048, T], BF16)
    x1_s = dscr("x1_s", [T, D])

    NW = (nc.sbuf_bytes_remaining - 6144) // 4
    NW = NW // 8 * 8
    arena_t = nc.alloc_sbuf_tensor("arena", [128, NW], F32)
    PERS_W = 1024
    pers = Arena(arena_t, PERS_W)
    ps = [nc.alloc_psum_tensor("ps%d" % i, [128, 512], F32) for i in range(8)]
    psb = [p[:].bitcast(BF16) for p in ps]

    class StageArena(Arena):
        def __init__(self):
            self.t = arena_t
            self.n = NW
            self.off = PERS_W

    def MM(out, lhsT, rhs, start=True, stop=True, r=(), w=()):
        S.add("pe", lambda e: e.matmul(out, lhsT=lhsT, rhs=rhs, start=start, stop=stop), r, w)

    def TR(out, in_, idn, r=(), w=()):
        S.add("pe", lambda e: e.transpose(out, in_, idn), r, w)

    def ACT(out, in_, func, r=(), w=(), bias=None, scale=None, accum=None):
        kw = {}
        if bias is not None:
            kw["bias"] = bias
        if scale is not None:
            kw["scale"] = scale
        if accum is not None:
            kw["accum_out"] = accum
        S.add("act", lambda e: e.activation(out=out, in_=in_, func=func, **kw), r, w)

    def TT(eng, out, in0, in1, op, r=(), w=()):
        S.add(eng, lambda e: e.tensor_tensor(out=out, in0=in0, in1=in1, op=op), r, w)

    def TS(eng, out, in0, s1, op0, s2=None, op1=None, r=(), w=(), accum=None):
        kw = {}
        if op1 is not None:
            kw["op1"] = op1
        if accum is not None:
            kw["accum_out"] = accum
        S.add(eng, lambda e: e.tensor_scalar(out=out, in0=in0, scalar1=s1, scalar2=s2, op0=op0, **kw), r, w)

    def STT(eng, out, in0, scalar, in1, op0, op1, r=(), w=(), accum=None):
        kw = {}
        if accum is not None:
            kw["accum_out"] = accum
        S.add(eng, lambda e: e.scalar_tensor_tensor(out=out, in0=in0, scalar=scalar, in1=in1, op0=op0, op1=op1, **kw), r, w)

    def CP(eng, out, in_, r=(), w=()):
        if eng == "act":
            S.add("act", lambda e: e.copy(out=out, in_=in_), r, w)
        else:
            S.add(eng, lambda e: e.tensor_copy(out=out, in_=in_), r, w)

    def RECIP(out, in_, r=(), w=()):
        S.add("dve", lambda e: e.reciprocal(out=out, in_=in_), r, w)

    def DMA(q, out, in_, key, r=(), w=(), slow=False):
        if slow:
            S.add(q, lambda e: e.dma_start(out=out, in_=in_, allow_slow_non_contiguous=True), r, w, key=key)
        else:
            S.add(q, lambda e: e.dma_start(out=out, in_=in_), r, w, key=key)

    def MEMSET(eng, ap, val, r=(), w=()):
        S.add(eng, lambda e: e.memset(ap, val), r, w)

    bar_t = pers.f32(128, 8)

    def BARRIER():
        S.barrier(lambda e: e.memset(bar_t, 0.0))

    ident = pers.f32(128, 128)
    identb = pers.bf16(128, 128)
    onesf = pers.f32(128, 128)
    onesb = pers.bf16(128, 128)
    uf = pers.f32(128, 128)
    ub = pers.f32(128, 128)
    mod = pers.f32(128, 96)
    sc1p = pers.f32(128, 16)
    sc2p = pers.f32(128, 16)
    carry = pers.f32(128, 1)
    cm1 = pers.f32(128, 1)
    DMA("sp", ident, ident_d, "c_ident", w=["ident"])
    DMA("pool", identb, ident_d, "c_identb", w=["identb"])
    DMA("sp", onesf, ones_d, "c_ones", w=["onesf"])
    DMA("pool", onesb, ones_d, "c_onesb", w=["onesb"])
    DMA("sp", uf, uf_d, "c_uf", w=["uf"])
    DMA("sp", ub, ub_d, "c_ub", w=["ub"])
    DMA("sp", carry, carry_d.rearrange("(p o) -> p o", o=1), "c_carry", w=["carry"], slow=True)
    TS("dve", cm1, carry, -1.0, ALU.add, r=["carry"], w=["cm1"])

    A = StageArena()
    condc = A.f32(128, 16)
    silc = A.f32(128, 16)
    bada = A.f32(128, 96)
    wsl = [A.f32(128, 16, 1024) for _ in range(2)]
    DMA("sp", condc, cond_d.rearrange("(c p) -> p c", p=128), "s0_cond", w=["condc"], slow=True)
    DMA("sp", bada, b_ada_d.rearrange("(j p) -> p j", p=128), "s0_bada", w=["bada"], slow=True)
    ACT(silc, condc, AF.Silu, r=["condc"], w=["silc"])
    w_ada_v = w_ada_d.rearrange("(k p) n -> p k n", p=128)
    for blk in range(12):
        s = blk % 2
        DMA("sp" if blk % 2 == 0 else "act", wsl[s], w_ada_v[:, :, blk * 1024:(blk + 1) * 1024], "s0_w%d" % s, w=["wada%d" % s])
        for mm in range(8):
            col = blk * 8 + mm
            for k in range(KC):
                MM(ps[0][:, col:col + 1], wsl[s][:, k, mm * 128:(mm + 1) * 128], silc[:, k:k + 1],
                   start=(k == 0), stop=(k == KC - 1), r=["wada%d" % s, "silc"], w=["ps0"])
    TT("dve", mod, ps[0][:, 0:96], bada, ALU.add, r=["ps0", "bada"], w=["mod"])
    TS("dve", sc1p, mod[:, 16:32], 1.0, ALU.add, r=["mod"], w=["sc1p"])
    TS("dve", sc2p, mod[:, 64:80], 1.0, ALU.add, r=["mod"], w=["sc2p"])
    DMA("sp", g1_s.rearrange("(c p) -> p c", p=128), mod[:, 32:48], "s0_g1", r=["mod"], slow=True)
    sh1 = mod[:, 0:16]
    sh2 = mod[:, 48:64]
    g2c = mod[:, 80:96]
    BARRIER()
    if STOP_AFTER == '0':
        return done()

    A = StageArena()
    hT = A.bf16(128, 16, T)
    mark_h = A.off
    xsl = [A.f32(128, D) for _ in range(2)]
    for i in range(NT):
        s = i % 2
        DMA("sp", xsl[s], x_d[i * 128:(i + 1) * 128, :], "s1_x%d" % s, w=["xs%d" % s])
        for cq in range(4):
            b = cq
            for cc in range(4):
                c = cq * 4 + cc
                TR(ps[b][:, cc * 128:(cc + 1) * 128], xsl[s][:, c * 128:(c + 1) * 128], ident,
                   r=["xs%d" % s, "ident"], w=["ps%d" % b])
            for cc in range(4):
                c = cq * 4 + cc
                o_ = hT[:, c, i * 128:(i + 1) * 128]
                i_ = ps[b][:, cc * 128:(cc + 1) * 128]
                if cc % 2 == 0:
                    ACT(o_, i_, AF.Identity, r=["ps%d" % b, "sc1p", "mod"], w=["hT"], scale=sc1p[:, c:c + 1], bias=sh1[:, c:c + 1])
                else:
                    TS("dve", o_, i_, sc1p[:, c:c + 1], ALU.mult, sh1[:, c:c + 1], ALU.add, r=["ps%d" % b, "sc1p", "mod"], w=["hT"])
    BARRIER()
    if STOP_AFTER == '1':
        return done()

    A.off = mark_h
    w_in_v = w_in_d.rearrange("(k p) n -> p k n", p=128)
    NSL = 3
    wsl = [A.bf16(128, 16, 512) for _ in range(NSL)]
    cw = A.f32(128, 48, 3)
    cb = A.f32(128, 48)
    w0c = A.f32(128, 48)
    w2c = A.f32(128, 48)
    qnw = A.f32(128, 4)
    kvw = A.f32(128, 4)
    mark_u = A.off
    cos2 = A.f32(64, T)
    sin2 = A.f32(64, T)
    for j in range(3):
        DMA("sp", cw[:, :, j], conv_w_d[j].rearrange("(c p) -> p c", p=128), "sA_cw%d" % j, w=["cw%d" % j], slow=True)
    DMA("sp", cb, conv_b_d.rearrange("(c p) -> p c", p=128), "sA_cb", w=["cb"], slow=True)
    DMA("sp", qnw, q_a_norm_w_d.rearrange("(c p) -> p c", p=128), "sA_qnw", w=["qnw"], slow=True)
    DMA("sp", kvw, kv_a_norm_w_d.rearrange("(c p) -> p c", p=128), "sA_kvw", w=["kvw"], slow=True)
    DMA("sp", cos2, cos2_d, "sA_cos", w=["cos2"])
    DMA("sp", sin2, sin2_d, "sA_sin", w=["sin2"])
    TS("dve", w0c, cw[:, :, 0], cm1[:, 0:1], ALU.mult, r=["cw0", "cm1"], w=["w0c"])
    TS("dve", w2c, cw[:, :, 2], cm1[:, 0:1], ALU.mult, r=["cw2", "cm1"], w=["w2c"])

    groups = []
    groups.append(("qa", 10368, 0))
    groups.append(("kva", 10880, 0))
    for g in range(12):
        groups.append(("xbc", 4096 + g * 512, g))
    for g in range(8):
        groups.append(("gate", 11456 + g * 512, g))
    for g in range(8):
        groups.append(("z", g * 512, g))

    def issue_w(n):
        if n < len(groups):
            s = n % NSL
            c0 = groups[n][1]
            DMA("pool", wsl[s], w_in_v[:, :, c0:c0 + 512], "sA_w%d" % s, w=["wA%d" % s])

    sq = A.f32(128, 4, 512)
    rt = A.f32(128, 512)
    rstd = A.f32(128, 512)
    nT = A.bf16(128, 4, T)
    ckn32 = A.f32(128, 4, 512)
    otile = [A.f32(128, 512) for _ in range(2)]
    wkr = A.bf16(128, 16, 64)
    wkrs = A.bf16(128, 16, 64)
    kr32 = A.f32(64, 512)
    krt1 = A.f32(64, 512)
    krt2 = A.f32(64, 512)
    krT = A.bf16(64, T)
    kro = [A.f32(128, 4, 64) for _ in range(2)]
    DMA("pool", wkr, w_in_v[:, :, 11392:11456], "sA_wkr", w=["wkr"])
    CP("dve", wkrs[:, :, 0:32], wkr[:, :, 32:64], r=["wkr"], w=["wkrs"])
    CP("dve", wkrs[:, :, 32:64], wkr[:, :, 0:32], r=["wkr"], w=["wkrs"])

    for n in range(NSL - 1):
        issue_w(n)
    bankrr = [0]

    def next_bank():
        b = bankrr[0] % 4
        bankrr[0] += 1
        return b

    chunk_ctr = [0]
    for n, (kind, c0, g) in enumerate(groups):
        issue_w(n + NSL - 1)
        s = n % NSL
        wres = "wA%d" % s
        if n == 2:
            BARRIER()
            if STOP_AFTER in ("Aqa", "Aqa_a", "Aqa_b", "Aqa_c", "Aqa_d"):
                return done()
            A.off = mark_u
            pre = [A.f32(128, T + 2) for _ in range(2)]
            acc = A.f32(128, T)
            post = [A.bf16(128, T) for _ in range(2)]
            tm = [A.bf16(128, 16, 128) for _ in range(2)]
            zt = [A.bf16(128, 512) for _ in range(4)]
            for s_ in range(2):
                MEMSET("dve", pre[s_][:, 0:1], 0.0, w=["pre%d" % s_])
                MEMSET("dve", pre[s_][:, T + 1:T + 2], 0.0, w=["pre%d" % s_])
        if (STOP_AFTER == "Ap" and n == 0) or (STOP_AFTER == "Aqa1" and n == 1):
            BARRIER()
            return done()
        if STOP_AFTER == "Ax1" and n == 3:
            BARRIER()
            return done()
        if kind == "xbc":
            for j in range(4):
                cc = g * 4 + j
                pslot = chunk_ctr[0] % 2
                chunk_ctr[0] += 1
                P_ = pre[pslot]
                for tb in range(4):
                    b = next_bank()
                    for k in range(KC):
                        MM(ps[b][:, :], wsl[s][:, k, j * 128:(j + 1) * 128], hT[:, k, tb * 512:(tb + 1) * 512],
                           start=(k == 0), stop=(k == KC - 1), r=[wres, "hT"], w=["ps%d" % b])
                    CP("act", P_[:, 1 + tb * 512:1 + (tb + 1) * 512], ps[b][:, :], r=["ps%d" % b], w=["pre%d" % pslot])
                pr = ["pre%d" % pslot]
                TS("dve", acc, P_[:, 0:T], cw[:, cc, 0:1], ALU.mult, r=pr + ["cw0"], w=["acc"])
                STT("dve", acc, P_[:, 1:T + 1], cw[:, cc, 1:2], acc, ALU.mult, ALU.add, r=pr + ["cw1", "acc"], w=["acc"])
                STT("dve", acc, P_[:, 2:T + 2], cw[:, cc, 2:3], acc, ALU.mult, ALU.add, r=pr + ["cw2", "acc"], w=["acc"])
                accv = acc.rearrange("p (s t) -> p s t", t=256)
                xv = P_[:, 1:T + 1].rearrange("p (s t) -> p s t", t=256)
                STT("dve", accv[:, 1:8, 0:1], xv[:, 0:7, 255:256], w0c[:, cc:cc + 1], accv[:, 1:8, 0:1], ALU.mult, ALU.add,
                    r=pr + ["w0c", "acc"], w=["acc"])
                STT("dve", accv[:, 0:7, 255:256], xv[:, 1:8, 0:1], w2c[:, cc:cc + 1], accv[:, 0:7, 255:256], ALU.mult, ALU.add,
                    r=pr + ["w2c", "acc"], w=["acc"])
                ACT(post[pslot], acc, AF.Silu, r=["acc", "cb"], w=["post%d" % pslot], bias=cb[:, cc:cc + 1])
                if cc < 40:
                    for half in range(2):
                        b = 4 + half
                        for i in range(8):
                            tix = half * 8 + i
                            TR(psb[b][:, i * 128:(i + 1) * 128], post[pslot][:, tix * 128:(tix + 1) * 128], identb,
                               r=["post%d" % pslot, "identb"], w=["ps%d" % b])
                        CP("dve", tm[pslot][:, half * 8:(half + 1) * 8, :], psb[b][:, :].rearrange("p (a b) -> p a b", b=128),
                           r=["ps%d" % b], w=["tm%d" % pslot])
                    if cc < 32:
                        dst = xs_tm_s.rearrange("(i p) c -> p i c", p=128)[:, :, cc * 128:(cc + 1) * 128]
                    else:
                        dst = b_tm_s.rearrange("(i p) c -> p i c", p=128)[:, :, (cc - 32) * 128:(cc - 31) * 128]
                    DMA("sp", dst, tm[pslot], "sA_tm%d" % pslot, r=["tm%d" % pslot])
                if cc >= 32:
                    DMA("sp", bct_s[(cc - 32) * 128:(cc - 31) * 128, :], post[pslot], "sA_post%d" % pslot, r=["post%d" % pslot])
        elif kind in ("qa", "kva"):
            nw = qnw if kind == "qa" else kvw
            for tb in range(4):
                tsl = slice(tb * 512, (tb + 1) * 512)
                for c in range(4):
                    for k in range(KC):
                        MM(ps[c][:, :], wsl[s][:, k, c * 128:(c + 1) * 128], hT[:, k, tsl],
                           start=(k == 0), stop=(k == KC - 1), r=[wres, "hT"], w=["ps%d" % c])
                for c in range(4):
                    ACT(sq[:, c, :], ps[c][:, :], AF.Square, r=["ps%d" % c], w=["sq"])
                for c in range(4):
                    MM(ps[6][:, :], onesf, sq[:, c, :], start=(c == 0), stop=(c == 3), r=["sq", "onesf"], w=["ps6"])
                ACT(rt, ps[6][:, :], AF.Sqrt, r=["ps6"], w=["rt"], scale=1.0 / 512.0, bias=1e-6)
                RECIP(rstd, rt, r=["rt"], w=["rstd"])
                if kind == "qa":
                    for c in range(4):
                        STT("dve", nT[:, c, tsl], ps[c][:, :], nw[:, c:c + 1], rstd, ALU.mult, ALU.mult,
                            r=["ps%d" % c, "qnw", "rstd"], w=["nT"])
                else:
                    for c in range(4):
                        STT("dve", ckn32[:, c, :], ps[c][:, :], nw[:, c:c + 1], rstd, ALU.mult, ALU.mult,
                            r=["ps%d" % c, "kvw", "rstd"], w=["ckn32"])
                    CP("pool", nT[:, :, tsl], ckn32, r=["ckn32"], w=["nT"])
                    for i4 in range(4 if STOP_AFTER != "Aqa_b" else 0):
                        os_ = (tb * 4 + i4) % 2
                        for c in range(4):
                            TR(ps[4][:, c * 128:(c + 1) * 128], ckn32[:, c, i4 * 128:(i4 + 1) * 128], ident,
                               r=["ckn32", "ident"], w=["ps4"])
                        CP("act", otile[os_], ps[4][:, :], r=["ps4"], w=["otile%d" % os_])
                        row = (tb * 4 + i4) * 128
                        DMA("sp", ckv_out_d[row:row + 128, :], otile[os_], "out_ckv%d" % os_, r=["otile%d" % os_])
                    if STOP_AFTER == "Aqa_a":
                        continue
                    for k in range(KC):
                        MM(ps[5][0:64, :], wkr[:, k, :], hT[:, k, tsl], start=(k == 0), stop=(k == KC - 1), r=["wkr", "hT"], w=["ps5"])
                    for k in range(KC):
                        MM(ps[7][0:64, :], wkrs[:, k, :], hT[:, k, tsl], start=(k == 0), stop=(k == KC - 1), r=["wkrs", "hT"], w=["ps7"])
                    if STOP_AFTER == "Aqa_d":
                        continue
                    CP("act", kr32, ps[5][0:64, :], r=["ps5"], w=["kr32"])
                    TT("dve", krt1, ps[5][0:64, :], cos2[:, tsl], ALU.mult, r=["ps5", "cos2"], w=["krt1"])
                    TT("dve", krt2, ps[7][0:64, :], sin2[:, tsl], ALU.mult, r=["ps7", "sin2"], w=["krt2"])
                    TT("dve", krT[:, tsl], krt1, krt2, ALU.add, r=["krt1", "krt2"], w=["krT"])
                    ks_ = tb % 2
                    if STOP_AFTER == "Aqa_c":
                        continue
                    for i4 in range(4):
                        TR(ps[6][:, i4 * 64:(i4 + 1) * 64], kr32[:, i4 * 128:(i4 + 1) * 128], ident[0:64, 0:64],
                           r=["kr32", "ident"], w=["ps6"])
                    CP("act", kro[ks_], ps[6][:, 0:256].rearrange("p (a b) -> p a b", b=64), r=["ps6"], w=["kro%d" % ks_])
                    DMA("sp", kr_out_d[tb * 512:(tb + 1) * 512, :].rearrange("(a p) c -> p a c", p=128), kro[ks_],
                        "out_kr%d" % ks_, r=["kro%d" % ks_])
            if kind == "qa":
                DMA("sp", qnT_s.rearrange("(c p) t -> p c t", p=128), nT, "sA_nT", r=["nT"])
            else:
                DMA("sp", ckvT_s.rearrange("(c p) t -> p c t", p=128), nT, "sA_nT", r=["nT"])
                DMA("sp", krT_s, krT, "sA_krT", r=["krT"])
        elif kind == "gate":
            for j in range(4):
                cc = g * 4 + j
                pslot = chunk_ctr[0] % 2
                chunk_ctr[0] += 1
                for tb in range(4):
                    b = next_bank()
                    for k in range(KC):
                        MM(ps[b][:, :], wsl[s][:, k, j * 128:(j + 1) * 128], hT[:, k, tb * 512:(tb + 1) * 512],
                           start=(k == 0), stop=(k == KC - 1), r=[wres, "hT"], w=["ps%d" % b])
                    ACT(post[pslot][:, tb * 512:(tb + 1) * 512], ps[b][:, :], AF.Sigmoid, r=["ps%d" % b], w=["post%d" % pslot])
                DMA("sp", gT_s[cc * 128:(cc + 1) * 128, :], post[pslot], "sA_post%d" % pslot, r=["post%d" % pslot])
        else:
            for i in range(NT):
                b = next_bank()
                zs_ = i % 4
                for k in range(KC):
                    MM(ps[b][:, :], hT[:, k, i * 128:(i + 1) * 128], wsl[s][:, k, :],
                       start=(k == 0), stop=(k == KC - 1), r=[wres, "hT"], w=["ps%d" % b])
                ACT(zt[zs_], ps[b][:, :], AF.Silu, r=["ps%d" % b], w=["zt%d" % zs_])
                DMA("sp", zs_s[i * 128:(i + 1) * 128, g * 512:(g + 1) * 512], zt[zs_], "sA_zt%d" % zs_, r=["zt%d" % zs_])
    BARRIER()
    if STOP_AFTER == 'A':
        return done()

    A.off = mark_h
    wdt = A.bf16(128, 16, 128)
    dtraw = A.f32(128, 16, 128)
    dte = A.f32(128, 16, 128)
    dtv = A.f32(128, 16, 128)
    lndt = A.f32(128, 16, 128)
    qa_ = A.f32(128, 16, 128)
    qbexp = A.f32(128, 16, 128)
    qeac = A.f32(128, 16, 128)
    qwdec = A.f32(128, 16, 128)
    qcdec = A.f32(128, 16, 128)
    dtb_bc = A.f32(128, 128)
    alog_bc = A.f32(128, 128)
    Abc = A.f32(128, 128)
    acs = [A.f32(128, 128) for _ in range(2)]
    tmpd = [A.f32(128, 128) for _ in range(2)]
    DMA("pool", wdt, w_in_v[:, :, 10240:10368], "sA3_w", w=["wdt"])
    DMA("sp", dtb_bc, dt_bias_d.partition_broadcast(128), "sA3_dtb", w=["dtb"])
    DMA("sp", alog_bc, a_log_d.partition_broadcast(128), "sA3_alog", w=["alog"])
    ACT(Abc, alog_bc, AF.Exp, r=["alog"], w=["Abc"])
    TS("dve", Abc, Abc, -1.0, ALU.mult, r=["Abc"], w=["Abc"])
    for i in range(NT):
        b = i // 4 % 2
        for k in range(KC):
            MM(ps[b][:, (i % 4) * 128:(i % 4 + 1) * 128], hT[:, k, i * 128:(i + 1) * 128], wdt[:, k, :],
               start=(k == 0), stop=(k == KC - 1), r=["wdt", "hT"], w=["ps%d" % b])
        if i % 4 == 3:
            i0 = i - 3
            TT("dve", dtraw[:, i0:i0 + 4, :], ps[b][:, :].rearrange("p (a b) -> p a b", b=128),
               dtb_bc.unsqueeze(1).to_broadcast([128, 4, 128]), ALU.add, r=["ps%d" % b, "dtb"], w=["dtraw"])
    ACT(dte, dtraw, AF.Exp, r=["dtraw"], w=["dte"])
    ACT(dtv, dte, AF.Ln, r=["dte"], w=["dtv"], bias=1.0, scale=1.0)
    ACT(lndt, dtv, AF.Ln, r=["dtv"], w=["lndt"])
    TT("dve", qa_, dtv, Abc.unsqueeze(1).to_broadcast([128, 16, 128]), ALU.mult, r=["dtv", "Abc"], w=["qa"])
    for c in range(NT):
        s = c % 2
        MM(ps[2 + s][:, 0:64], uf, qa_[:, c, 0:64], r=["uf", "qa"], w=["ps%d" % (2 + s)])
        MM(ps[2 + s][:, 64:128], ub, qa_[:, c, 64:128], r=["ub", "qa"], w=["ps%d" % (2 + s)])
        MM(ps[4 + s][:, 0:128], onesf, qa_[:, c, :], r=["onesf", "qa"], w=["ps%d" % (4 + s)])
        CP("act", acs[s], ps[2 + s][:, 0:128], r=["ps%d" % (2 + s)], w=["acs%d" % s])
        TT("dve", qbexp[:, c, :], lndt[:, c, :], acs[s], ALU.subtract, r=["lndt", "acs%d" % s], w=["qbexp"])
        ACT(qeac[:, c, :], ps[2 + s][:, 0:128], AF.Exp, r=["ps%d" % (2 + s)], w=["qeac"])
        ACT(qcdec[:, c, :], ps[4 + s][:, 0:128], AF.Exp, r=["ps%d" % (4 + s)], w=["qcdec"])
        TT("dve", tmpd[s], ps[4 + s][:, 0:128], acs[s], ALU.subtract, r=["ps%d" % (4 + s), "acs%d" % s], w=["tmpd%d" % s])
        ACT(tmpd[s], tmpd[s], AF.Exp, r=["tmpd%d" % s], w=["tmpd%d" % s])
        TT("dve", qwdec[:, c, :], tmpd[s], dtv[:, c, :], ALU.mult, r=["tmpd%d" % s, "dtv"], w=["qwdec"])
    for qi, (qt, nm) in enumerate(((qa_, "qa"), (qbexp, "qbexp"), (qeac, "qeac"), (qwdec, "qwdec"), (qcdec, "qcdec"))):
        DMA("sp", dtq_s[qi].rearrange("(c p) n -> p c n", p=128), qt, "sA3_q%d" % qi, r=[nm])
    BARRIER()
    if STOP_AFTER == 'A3':
        return done()

    A = StageArena()
    dq = [A.f32(128, 16, 128) for _ in range(5)]
    q_a, q_bexp, q_eac, q_wdec, q_cdec = dq
    for qi in range(5):
        DMA("sp", dq[qi], dtq_s[qi].rearrange("(c p) n -> p c n", p=128), "sB_q%d" % qi, w=["dq"])
    D_bc = A.f32(128, 64)
    DMA("sp", D_bc, d_skip_d.partition_broadcast(128), "sB_D", w=["D_bc"])
    xtm = [A.bf16(128, 16, 512) for _ in range(2)]
    btm = [A.bf16(128, 16, 128) for _ in range(2)]
    BTt = [A.bf16(128, T) for _ in range(2)]
    CTt = [A.bf16(128, T) for _ in range(2)]
    normw = [A.f32(128, 512) for _ in range(2)]
    yacc = A.f32(128, 16, 512)
    S32 = [A.f32(128, 512) for _ in range(2)]
    Sbf = [A.bf16(128, 512) for _ in range(2)]
    cbm = [A.bf16(128, 128) for _ in range(2)]
    Lsb = [A.bf16(128, 8, 128) for _ in range(2)]
    mT = [A.bf16(128, 8, 128) for _ in range(2)]
    yoff = [A.f32(128, 512) for _ in range(2)]
    xw = [A.bf16(128, 512) for _ in range(2)]
    ztB = [A.bf16(128, 512) for _ in range(2)]
    yg = [A.f32(128, 512) for _ in range(2)]
    ygsq = A.f32(128, 512)
    yn = [A.bf16(128, 512) for _ in range(2)]
    ssq = [A.f32(128, 1) for _ in range(2)]
    srt = [A.f32(128, 1) for _ in range(2)]
    srs = [A.f32(128, 1) for _ in range(2)]
    yTsb = A.bf16(128, 4, T)
    stout = [A.f32(128, 4, 128) for _ in range(2)]
    h0t = [A.f32(128, 4, 128) for _ in range(2)]
    xs_tm_v = xs_tm_s.rearrange("(i p) c -> p i c", p=128)
    b_tm_v = b_tm_s.rearrange("(i p) c -> p i c", p=128)

    def loadB(g):
        s = g % 2
        DMA("sp", xtm[s], xs_tm_v[:, :, g * 512:(g + 1) * 512], "sB_x%d" % s, w=["xtm%d" % s])
        DMA("sp", btm[s], b_tm_v[:, :, g * 128:(g + 1) * 128], "sB_b%d" % s, w=["btm%d" % s])
        DMA("sp", BTt[s], bct_s[g * 128:(g + 1) * 128, :], "sB_BT%d" % s, w=["BT%d" % s])
        DMA("sp", CTt[s], bct_s[1024 + g * 128:1024 + (g + 1) * 128, :], "sB_CT%d" % s, w=["CT%d" % s])
        DMA("sp", normw[s], ssm_norm_w_d[g * 512:(g + 1) * 512].partition_broadcast(128), "sB_nw%d" % s, w=["normw%d" % s])

    loadB(0)
    it = [0]
    for g in range(8):
        gs = g % 2
        if g + 1 < 8:
            loadB(g + 1)
        X = xtm[gs]
        xr = "xtm%d" % gs
        for c in range(NT):
            TT("pool", yacc[:, c, :].rearrange("p (h q) -> p h q", q=64), X[:, c, :].rearrange("p (h q) -> p h q", q=64),
               D_bc[:, g * 8:(g + 1) * 8].unsqueeze(2).to_broadcast([128, 8, 64]), ALU.mult, r=[xr, "D_bc"], w=["yacc%d" % c])
        iters = [(d, c) for d in range(2) for c in (range(NT) if d == 0 else range(NT - 1, -1, -1))]

        def front(idx):
            d, c = iters[idx]
            k2 = idx % 2
            U = uf if d == 0 else ub
            ures = "uf" if d == 0 else "ub"
            col0 = d * 64 + g * 8
            csl = slice(c * 128, (c + 1) * 128)
            MM(ps[0][:, 0:128], BTt[gs][:, csl], CTt[gs][:, csl], r=["BT%d" % gs, "CT%d" % gs], w=["ps0"])
            TT("dve", cbm[k2], ps[0][:, 0:128], U, ALU.mult, r=["ps0", ures], w=["cbm%d" % k2])
            for half in range(2):
                for jj in range(4):
                    j = half * 4 + jj
                    MM(ps[1 + half][:, jj * 128:(jj + 1) * 128], q_a[:, c, col0 + j:col0 + j + 1].to_broadcast([128, 128]), U,
                       r=["dq", ures], w=["ps%d" % (1 + half)])
                for jj in range(4):
                    j = half * 4 + jj
                    ACT(Lsb[k2][:, j, :], ps[1 + half][:, jj * 128:(jj + 1) * 128], AF.Exp,
                        r=["ps%d" % (1 + half), "dq"], w=["L%d_%d" % (k2, half)], bias=q_bexp[:, c, col0 + j:col0 + j + 1])
                STT("dve", mT[k2][:, half * 4:(half + 1) * 4, :], Lsb[k2][:, half * 4:(half + 1) * 4, :], 1e30,
                    cbm[k2].unsqueeze(1).to_broadcast([128, 4, 128]), ALU.min, ALU.mult,
                    r=["L%d_%d" % (k2, half), "cbm%d" % k2], w=["mT%d_%d" % (k2, half)])

        def back(idx):
            d, c = iters[idx]
            k2 = idx % 2
            col0 = d * 64 + g * 8
            first = (c == 0) if d == 0 else (c == NT - 1)
            seg_start = (c % 2 == 0) if d == 0 else (c % 2 == 1)
            seg_end = (c % 2 == 1) if d == 0 else (c % 2 == 0)
            sres = "S32_%d" % d
            bres = "Sbf_%d" % d
            csl = slice(c * 128, (c + 1) * 128)
            if first:
                hs = (g * 2 + d) % 2
                DMA("sp", h0t[hs], h0_d[d, g * 8:(g + 1) * 8].rearrange("(jj h2) q n -> (h2 q) jj n", h2=2),
                    "sB_h0%d" % hs, w=["h0t%d" % hs])
                for jj in range(4):
                    TR(ps[6][:, jj * 128:(jj + 1) * 128], h0t[hs][:, jj, :], ident, r=["h0t%d" % hs, "ident"], w=["ps6"])
                CP("dve", S32[d], ps[6][:, :], r=["ps6"], w=[sres])
                CP("act", Sbf[d], ps[6][:, :], r=["ps6"], w=[bres])
            elif seg_start:
                TS("dve", S32[d], S32[d], carry[:, 0:1], ALU.mult, r=[sres, "carry"], w=[sres])
                CP("act", Sbf[d], S32[d], r=[sres], w=[bres])
            MM(ps[4][:, :], CTt[gs][:, csl], Sbf[d], r=["CT%d" % gs, bres], w=["ps4"])
            for j in range(8):
                MM(ps[3][:, j * 64:(j + 1) * 64], mT[k2][:, j, :], X[:, c, j * 64:(j + 1) * 64],
                   r=["mT%d_%d" % (k2, j // 4), xr], w=["ps3"])
            TT("pool", xw[k2].rearrange("p (h q) -> p h q", q=64), X[:, c, :].rearrange("p (h q) -> p h q", q=64),
               q_wdec[:, c, col0:col0 + 8].unsqueeze(2).to_broadcast([128, 8, 64]), ALU.mult, r=[xr, "dq"], w=["xw%d" % k2])
            MM(ps[5][:, :], btm[gs][:, c, :], xw[k2], r=["btm%d" % gs, "xw%d" % k2], w=["ps5"])
            TT("dve", yoff[k2].rearrange("p (h q) -> p h q", q=64), ps[4][:, :].rearrange("p (h q) -> p h q", q=64),
               q_eac[:, c, col0:col0 + 8].unsqueeze(2).to_broadcast([128, 8, 64]), ALU.mult, r=["ps4", "dq"], w=["yoff%d" % k2])
            TT("dve", S32[d].rearrange("p (h q) -> p h q", q=64), S32[d].rearrange("p (h q) -> p h q", q=64),
               q_cdec[:, c, col0:col0 + 8].unsqueeze(2).to_broadcast([128, 8, 64]), ALU.mult, r=[sres, "dq"], w=[sres])
            TT("dve", S32[d], S32[d], ps[5][:, :], ALU.add, r=[sres, "ps5"], w=[sres])
            CP("act", Sbf[d], S32[d], r=[sres], w=[bres])
            TT("dve", yoff[k2], yoff[k2], ps[3][:, :], ALU.add, r=["yoff%d" % k2, "ps3"], w=["yoff%d" % k2])
            TT("pool", yacc[:, c, :], yacc[:, c, :], yoff[k2], ALU.add, r=["yacc%d" % c, "yoff%d" % k2], w=["yacc%d" % c])
            if seg_end:
                so = (c // 2 + d) % 2
                for jj in range(4):
                    TR(ps[6][:, jj * 128:(jj + 1) * 128], S32[d][:, jj * 128:(jj + 1) * 128], ident, r=[sres, "ident"], w=["ps6"])
                CP("act", stout[so], ps[6][:, :].rearrange("p (a b) -> p a b", b=128), r=["ps6"], w=["stout%d" % so])
                DMA("sp", st_out_d[c // 2, d, g * 8:(g + 1) * 8].rearrange("(jj h2) q n -> (h2 q) jj n", h2=2), stout[so],
                    "out_st%d" % so, r=["stout%d" % so])

        front(0)
        for idx in range(len(iters)):
            if idx + 1 < len(iters):
                front(idx + 1)
            back(idx)
        for c in range(NT):
            k2 = c % 2
            DMA("sp", ztB[k2], zs_s[c * 128:(c + 1) * 128, g * 512:(g + 1) * 512], "sB_zt%d" % k2, w=["ztB%d" % k2])
            TT("dve", yg[k2], yacc[:, c, :], ztB[k2], ALU.mult, r=["yacc%d" % c, "ztB%d" % k2], w=["yg%d" % k2])
            ACT(ygsq, yg[k2], AF.Square, r=["yg%d" % k2], w=["ygsq", "ssq%d" % k2], accum=ssq[k2])
            ACT(srt[k2], ssq[k2], AF.Sqrt, r=["ssq%d" % k2], w=["srt%d" % k2], scale=1.0 / 512.0, bias=1e-6)
            RECIP(srs[k2], srt[k2], r=["srt%d" % k2], w=["srs%d" % k2])
            STT("dve", yn[k2], yg[k2], srs[k2][:, 0:1], normw[gs], ALU.mult, ALU.mult, r=["yg%d" % k2, "srs%d" % k2, "normw%d" % gs], w=["yn%d" % k2])
            for cc in range(4):
                TR(psb[7][:, cc * 128:(cc + 1) * 128], yn[k2][:, cc * 128:(cc + 1) * 128], identb, r=["yn%d" % k2, "identb"], w=["ps7"])
            CP("act", yTsb[:, :, c * 128:(c + 1) * 128], psb[7][:, 0:512].rearrange("p (a b) -> p a b", b=128), r=["ps7"], w=["yTsb"])
        DMA("sp", yT_s.rearrange("(cc p) t -> p cc t", p=128)[:, g * 4:(g + 1) * 4, :], yTsb, "sB_yT", r=["yTsb"])
    BARRIER()
    if STOP_AFTER == 'B':
        return done()

    A = StageArena()
    qnT = A.bf16(128, 4, T)
    ckvT = A.bf16(128, 4, NKEY)
    kra = A.bf16(73, NKEY)
    wq = A.bf16(128, 4, 3072)
    wkv = A.bf16(128, 4, 4096)
    cos2 = A.f32(64, T)
    sin2 = A.f32(64, T)
    cxl = [A.f32(128, 512) for _ in range(2)]
    cxk = A.f32(128, 4, 64)
    wqs = [A.bf16(128, 4, 64) for _ in range(2)]
    qn_h = [A.bf16(128, T) for _ in range(2)]
    qra = [A.bf16(73, T) for _ in range(2)]
    kn_h = [A.bf16(128, NKEY) for _ in range(2)]
    v_h = [A.bf16(128, 20, 128) for _ in range(2)]
    PT = [A.bf16(128, 512) for _ in range(4)]
    rden = A.f32(128, 512)
    oTh = [A.bf16(128, T) for _ in range(2)]
    rp1 = A.f32(64, 512)
    rp2 = A.f32(64, 512)
    DMA("sp", qnT, qnT_s.rearrange("(c p) t -> p c t", p=128), "sC_qnT", w=["qnT"])
    DMA("sp", ckvT[:, :, 0:T], ckvT_s.rearrange("(c p) t -> p c t", p=128), "sC_ckvT", w=["ckvT_own"])
    DMA("sp", kra[0:64, 0:T], krT_s, "sC_kr", w=["kra_own"])
    DMA("pool", kra[64:73, :], khot_d, "sC_khot", w=["kra_hot"])
    for s in range(2):
        DMA("pool", qra[s][64:73, :], qpen_d, "sC_qpen%d" % s, w=["qra_pen%d" % s])
    DMA("pool", wq[:, :, 0:1536], w_q_b_d.rearrange("(k p) n -> p k n", p=128)[:, :, 0:1536], "sC_wq0", w=["wq0"])
    DMA("pool", wq[:, :, 1536:3072], w_q_b_d.rearrange("(k p) n -> p k n", p=128)[:, :, 1536:3072], "sC_wq1", w=["wq1"])
    DMA("pool", wkv[:, :, 0:2048], w_kv_b_d.rearrange("(k p) n -> p k n", p=128)[:, :, 0:2048], "sC_wkv0", w=["wkv0"])
    DMA("pool", wkv[:, :, 2048:4096], w_kv_b_d.rearrange("(k p) n -> p k n", p=128)[:, :, 2048:4096], "sC_wkv1", w=["wkv1"])
    DMA("sp", cos2, cos2_d, "sC_cos", w=["cos2"])
    DMA("sp", sin2, sin2_d, "sC_sin", w=["sin2"])
    for kt in range(4):
        s = kt % 2
        DMA("sp", cxl[s], ctx_ckv_d[kt * 128:(kt + 1) * 128, :], "sC_cx%d" % s, w=["cxl%d" % s])
        for c in range(4):
            TR(ps[4][:, c * 128:(c + 1) * 128], cxl[s][:, c * 128:(c + 1) * 128], ident, r=["cxl%d" % s, "ident"], w=["ps4"])
        CP("dve", ckvT[:, :, T + kt * 128:T + (kt + 1) * 128], ps[4][:, :].rearrange("p (a b) -> p a b", b=128), r=["ps4"], w=["ckvT_ctx"])
    DMA("sp", cxk, ctx_kr_d.rearrange("(a p) c -> p a c", p=128), "sC_cxk", w=["cxk"])
    for kt in range(4):
        TR(ps[5][0:64, kt * 128:(kt + 1) * 128], cxk[:, kt, :], ident, r=["cxk", "ident"], w=["ps5"])
    CP("dve", kra[0:64, T:NKEY], ps[5][0:64, :], r=["ps5"], w=["kra_ctx"])
    ckr = ["ckvT_own", "ckvT_ctx"]
    krr = ["kra_own", "kra_hot", "kra_ctx"]
    for h in range(16):
        hs = h % 2
        wqr = "wq%d" % (h // 8)
        wkr_ = "wkv%d" % (h // 8)
        qc0 = h * 192
        kc0 = h * 256
        CP("pool", wqs[hs][:, :, 0:32], wq[:, :, qc0 + 160:qc0 + 192], r=[wqr], w=["wqs%d" % hs])
        CP("pool", wqs[hs][:, :, 32:64], wq[:, :, qc0 + 128:qc0 + 160], r=[wqr], w=["wqs%d" % hs])
        for tb in range(4):
            tsl = slice(tb * 512, (tb + 1) * 512)
            for k in range(4):
                MM(ps[4][:, :], wq[:, k, qc0:qc0 + 128], qnT[:, k, tsl], start=(k == 0), stop=(k == 3), r=[wqr, "qnT"], w=["ps4"])
            CP("act", qn_h[hs][:, tsl], ps[4][:, :], r=["ps4"], w=["qn_h%d" % hs])
            for k in range(4):
                MM(ps[5][0:64, :], wq[:, k, qc0 + 128:qc0 + 192], qnT[:, k, tsl], start=(k == 0), stop=(k == 3), r=[wqr, "qnT"], w=["ps5"])
            for k in range(4):
                MM(ps[6][0:64, :], wqs[hs][:, k, :], qnT[:, k, tsl], start=(k == 0), stop=(k == 3), r=["wqs%d" % hs, "qnT"], w=["ps6"])
            TT("dve", rp1, ps[5][0:64, :], cos2[:, tsl], ALU.mult, r=["ps5", "cos2"], w=["rp1"])
            TT("dve", rp2, ps[6][0:64, :], sin2[:, tsl], ALU.mult, r=["ps6", "sin2"], w=["rp2"])
            TT("dve", qra[hs][0:64, tsl], rp1, rp2, ALU.add, r=["rp1", "rp2"], w=["qra%d" % hs])
        for kb in range(5):
            ksl = slice(kb * 512, (kb + 1) * 512)
            for k in range(4):
                MM(ps[7][:, :], wkv[:, k, kc0:kc0 + 128], ckvT[:, k, ksl], start=(k == 0), stop=(k == 3), r=[wkr_] + ckr, w=["ps7"])
            CP("act", kn_h[hs][:, ksl], ps[7][:, :], r=["ps7"], w=["kn_h%d" % hs])
        for kq in range(5):
            for kk in range(4):
                kt = kq * 4 + kk
                for k in range(4):
                    MM(ps[4][:, kk * 128:(kk + 1) * 128], ckvT[:, k, kt * 128:(kt + 1) * 128], wkv[:, k, kc0 + 128:kc0 + 256],
                       start=(k == 0), stop=(k == 3), r=[wkr_] + ckr, w=["ps4"])
            CP("dve", v_h[hs][:, kq * 4:(kq + 1) * 4, :], ps[4][:, :].rearrange("p (a b) -> p a b", b=128), r=["ps4"], w=["v_h%d" % hs])
        pti = 0
        for qb in range(4):
            qsl = slice(qb * 512, (qb + 1) * 512)
            prev = None
            for kt in range(21):
                if kt < 20:
                    sb = kt % 2
                    p_ = pti % 4
                    pti += 1
                    MM(ps[sb][:, :], kn_h[hs][:, kt * 128:(kt + 1) * 128], qn_h[hs][:, qsl], start=True, stop=False,
                       r=["kn_h%d" % hs, "qn_h%d" % hs], w=["ps%d" % sb])
                    MM(ps[sb][:, :], kra[0:73, kt * 128:(kt + 1) * 128], qra[hs][0:73, qsl], start=False, stop=True,
                       r=krr + ["qra%d" % hs, "qra_pen%d" % hs], w=["ps%d" % sb])
                    ACT(PT[p_], ps[sb][:, :], AF.Exp, r=["ps%d" % sb], w=["PT%d" % p_], scale=SCALE)
                if prev is not None:
                    pk, pp = prev
                    MM(ps[2][:, :], v_h[hs][:, pk, :], PT[pp], start=(pk == 0), stop=(pk == 19), r=["v_h%d" % hs, "PT%d" % pp], w=["ps2"])
                    MM(ps[3][:, :], onesb, PT[pp], start=(pk == 0), stop=(pk == 19), r=["onesb", "PT%d" % pp], w=["ps3"])
                prev = (kt, p_) if kt < 20 else None
            RECIP(rden, ps[3][:, :], r=["ps3"], w=["rden"])
            TT("dve", oTh[hs][:, qsl], ps[2][:, :], rden, ALU.mult, r=["ps2", "rden"], w=["oTh%d" % hs])
        DMA("sp", oT_s[h * 128:(h + 1) * 128, :], oTh[hs], "sC_oT%d" % hs, r=["oTh%d" % hs])
    BARRIER()
    if STOP_AFTER == 'C':
        return done()

    A = StageArena()
    g1bc = A.f32(128, D)
    lnw = A.f32(128, D)
    lnb = A.f32(128, D)
    DMA("sp", g1bc, g1_s.partition_broadcast(128), "sD_g1", w=["g1bc"])
    DMA("sp", lnw, ln1_w_d.partition_broadcast(128), "sD_lnw", w=["lnw"])
    DMA("sp", lnb, ln1_b_d.partition_broadcast(128), "sD_lnb", w=["lnb"])
    yTb = A.bf16(128, 32, 512)
    oTb = A.bf16(128, 16, 512)
    gsl = [A.bf16(128, 2, 2, 512) for _ in range(2)]
    mrg = A.bf16(128, 16, 512)
    NSD = 2
    wD = [A.bf16(128, 32, 256) for _ in range(NSD)]
    t1 = [A.f32(128, 512) for _ in range(2)]
    t2 = [A.f32(128, 512) for _ in range(2)]
    vt = A.f32(128, 4, D)
    xin = [A.f32(128, D)]
    junk = A.f32(128, D)
    st1 = [A.f32(128, 4) for _ in range(2)]
    w_ssm_v = w_ssm_out_d.rearrange("(k p) n -> p k n", p=128)
    w_mla_v = w_mla_out_d.rearrange("(k p) n -> p k n", p=128)
    w_o_v = w_o_d.rearrange("(k p) n -> p k n", p=128)
    dgroups = []
    for tb in range(4):
        for m2 in range(8):
            dgroups.append(("ssm", m2, tb))
            dgroups.append(("mla", m2, tb))
        for fb in range(4):
            dgroups.append(("wo", fb, tb))

    def issue_d(n):
        if n < len(dgroups):
            s = n % NSD
            kind, m, _ = dgroups[n]
            if kind == "ssm":
                DMA("pool", wD[s], w_ssm_v[:, :, m * 256:(m + 1) * 256], "sD_w%d" % s, w=["wD%d" % s])
            elif kind == "mla":
                DMA("pool", wD[s][:, 0:16, :], w_mla_v[:, :, m * 256:(m + 1) * 256], "sD_w%d" % s, w=["wD%d" % s])
            else:
                DMA("pool", wD[s].rearrange("p a b -> p (a b)").rearrange("p (a b) -> p a b", b=512), w_o_v[:, :, m * 512:(m + 1) * 512],
                    "sD_w%d" % s, w=["wD%d" % s])

    for n in range(NSD - 1):
        issue_d(n)
    tctr = 0
    for n, (kind, m, tb) in enumerate(dgroups):
        issue_d(n + NSD - 1)
        s = n % NSD
        wres = "wD%d" % s
        tsl = slice(tb * 512, (tb + 1) * 512)
        if kind == "ssm" and m == 0:
            DMA("sp", yTb, yT_s.rearrange("(c p) t -> p c t", p=128)[:, :, tsl], "sD_yT", w=["yTb"])
            DMA("sp", oTb, oT_s.rearrange("(c p) t -> p c t", p=128)[:, :, tsl], "sD_oT", w=["oTb"])
        if kind == "ssm":
            gq = m % 2
            gT_v = gT_s.rearrange("(c p) t -> p c t", p=128)
            DMA("sp", gsl[gq][:, 0, :, :], gT_v[:, m * 2:m * 2 + 2, tsl], "sD_gs%d" % gq, w=["gsl%d" % gq])
            DMA("sp", gsl[gq][:, 1, :, :], gT_v[:, 16 + m * 2:16 + m * 2 + 2, tsl], "sD_gm%d" % gq, w=["gsl%d" % gq])
            for mm in range(2):
                mc = m * 2 + mm
                b = mm
                for k in range(32):
                    MM(ps[b][:, :], wD[s][:, k, mm * 128:(mm + 1) * 128], yTb[:, k, :], start=(k == 0), stop=(k == 31),
                       r=[wres, "yTb"], w=["ps%d" % b])
                TT("dve", t1[mm], ps[b][:, :], gsl[gq][:, 0, mm, :], ALU.mult, r=["ps%d" % b, "gsl%d" % gq], w=["t1_%d" % mm])
        elif kind == "mla":
            gq = m % 2
            for mm in range(2):
                mc = m * 2 + mm
                b = 2 + mm
                for k in range(16):
                    MM(ps[b][:, :], wD[s][:, k, mm * 128:(mm + 1) * 128], oTb[:, k, :], start=(k == 0), stop=(k == 15),
                       r=[wres, "oTb"], w=["ps%d" % b])
                TT("dve", t2[mm], ps[b][:, :], gsl[gq][:, 1, mm, :], ALU.mult, r=["ps%d" % b, "gsl%d" % gq], w=["t2_%d" % mm])
                TT("pool", mrg[:, mc, :], t1[mm], t2[mm], ALU.add, r=["t1_%d" % mm, "t2_%d" % mm], w=["mrg"])
        else:
            fb = m
            fsl = slice(fb * 512, (fb + 1) * 512)
            wv = wD[s].rearrange("p a b -> p (a b)").rearrange("p (a b) -> p a b", b=512)
            for i4 in range(4):
                i = tb * 4 + i4
                b = 4 + (i4 % 2)
                if fb == 0:
                    DMA("sp", xin[0], x_d[i * 128:(i + 1) * 128, :], "sD_x0", w=["xin0"])
                for k in range(16):
                    MM(ps[b][:, :], mrg[:, k, i4 * 128:(i4 + 1) * 128], wv[:, k, :], start=(k == 0), stop=(k == 15),
                       r=["mrg", wres], w=["ps%d" % b])
                tq = tctr % 2
                tctr += 1
                TT("dve", t1[tq], ps[b][:, :], g1bc[:, fsl], ALU.mult, r=["ps%d" % b, "g1bc"], w=["t1_%d" % tq])
                if fb == 0:
                    TS("pool", vt[:, i4, :], xin[0], ALU_ALPHA, ALU.mult, r=["xin0"], w=["vt%d" % i4])
                TT("pool", vt[:, i4, fsl], vt[:, i4, fsl], t1[tq], ALU.add, r=["vt%d" % i4, "t1_%d" % tq], w=["vt%d" % i4])
            if fb == 3:
                for i4 in range(4):
                    i = tb * 4 + i4
                    q2 = i4 % 2
                    V = vt[:, i4, :]
                    st = st1[q2]
                    TS("dve", junk, V, 1.0, ALU.mult, 0.0, ALU.add, r=["vt%d" % i4], w=["junk", "st%d" % q2], accum=st[:, 0:1])
                    TS("dve", st[:, 1:2], st[:, 0:1], -1.0 / D, ALU.mult, r=["st%d" % q2], w=["st%d" % q2])
                    ACT(junk, V, AF.Square, r=["vt%d" % i4, "st%d" % q2], w=["junk", "st%d" % q2], bias=st[:, 1:2], accum=st[:, 2:3])
                    ACT(st[:, 3:4], st[:, 2:3], AF.Sqrt, r=["st%d" % q2], w=["st%d" % q2], scale=1.0 / D, bias=1e-5)
                    RECIP(st[:, 3:4], st[:, 3:4], r=["st%d" % q2], w=["st%d" % q2])
                    TS("dve", V, V, st[:, 1:2], ALU.add, st[:, 3:4], ALU.mult, r=["vt%d" % i4, "st%d" % q2], w=["vt%d" % i4])
                    TT("dve", V, V, lnw, ALU.mult, r=["vt%d" % i4, "lnw"], w=["vt%d" % i4])
                    TT("pool", V, V, lnb, ALU.add, r=["vt%d" % i4, "lnb"], w=["vt%d" % i4])
                    DMA("sp", x1_s[i * 128:(i + 1) * 128, :], V, "sD_x1_%d" % i4, r=["vt%d" % i4])
    BARRIER()
    if STOP_AFTER == 'D':
        return done()

    A = StageArena()
    h2T = A.bf16(128, 16, 1024)
    accT = A.f32(128, 16, 1024)
    bg = A.f32(128, 32, 16, 2)
    bd = A.f32(128, 32, 16)
    comb = A.f32(128, 8, 32)
    rw = A.f32(128, 16, 32)
    rb_bc = A.f32(128, 32)
    NSE = 3
    wE = [A.bf16(128, 16, 256) for _ in range(NSE)]
    lg = A.f32(128, 32)
    m8 = A.f32(128, 8)
    nmx = A.f32(128, 1)
    msk = A.f32(128, 32)
    ex = A.f32(128, 32)
    ssum = A.f32(128, 1)
    mark_e = A.off
    h32 = A.f32(128, 16, 128)
    x1l = [A.f32(128, D) for _ in range(2)]
    A.off = mark_e
    actT = A.bf16(128, 16, 1024)
    combs = A.f32(128, 1024)
    gsb = [A.f32(128, 512) for _ in range(2)]
    sgb = [A.f32(128, 512) for _ in range(2)]
    usb = [A.f32(128, 512) for _ in range(2)]
    tcb = [A.f32(128, 512) for _ in range(2)]
    A.off = mark_e
    lnw2 = A.f32(128, D)
    lnb2 = A.f32(128, D)
    v2 = [A.f32(128, D) for _ in range(2)]
    x1e = [A.f32(128, D)]
    junk2 = A.f32(128, D)
    st2 = [A.f32(128, 4) for _ in range(2)]
    for e4 in range(4):
        DMA("sp", bg[:, e4 * 8:(e4 + 1) * 8, :, :], b_gu_d[e4 * 8:(e4 + 1) * 8, :].rearrange("e (c p two) -> p e c two", p=128, two=2),
            "sE_bg%d" % e4, w=["bg"], slow=True)
        DMA("sp", bd[:, e4 * 8:(e4 + 1) * 8, :], b_down_d[e4 * 8:(e4 + 1) * 8, :].rearrange("e (c p) -> p e c", p=128),
            "sE_bd%d" % e4, w=["bd"], slow=True)
    DMA("sp", rw, router_w_d.rearrange("(k p) e -> p k e", p=128), "sE_rw", w=["rw"])
    DMA("sp", rb_bc, router_b_d.partition_broadcast(128), "sE_rb", w=["rb"])
    w_gu_v = w_gu_d.rearrange("e (k p) n -> e p k n", p=128)
    w_dn_v = w_down_d.rearrange("e (k p) n -> e p k n", p=128)
    egroups = []
    for blk in range(2):
        for e in range(32):
            for m in range(16):
                egroups.append(("gu", blk, e, m))
            for j2 in range(8):
                egroups.append(("dn", blk, e, j2))

    def issue_e(n):
        if n < len(egroups):
            s = n % NSE
            kind, _, e, m = egroups[n]
            if kind == "gu":
                DMA("pool", wE[s], w_gu_v[e][:, :, m * 256:(m + 1) * 256], "sE_w%d" % s, w=["wE%d" % s])
            else:
                DMA("pool", wE[s], w_dn_v[e][:, :, m * 256:(m + 1) * 256], "sE_w%d" % s, w=["wE%d" % s])

    n = 0
    for blk in range(2):
        for i8 in range(8):
            i = blk * 8 + i8
            xs_ = i % 2
            DMA("sp", x1l[xs_], x1_s[i * 128:(i + 1) * 128, :], "sE_x1_%d" % xs_, w=["x1l%d" % xs_])
            for cq in range(4):
                b = 4 + cq % 2
                for cc in range(4):
                    c = cq * 4 + cc
                    TR(ps[b][:, cc * 128:(cc + 1) * 128], x1l[xs_][:, c * 128:(c + 1) * 128], ident, r=["x1l%d" % xs_, "ident"], w=["ps%d" % b])
                for cc in range(4):
                    c = cq * 4 + cc
                    if cc % 2 == 0:
                        ACT(h32[:, c, :], ps[b][:, cc * 128:(cc + 1) * 128], AF.Identity, r=["ps%d" % b, "sc2p", "mod"], w=["h32_%d" % c],
                            scale=sc2p[:, c:c + 1], bias=sh2[:, c:c + 1])
                    else:
                        TS("dve", h32[:, c, :], ps[b][:, cc * 128:(cc + 1) * 128], sc2p[:, c:c + 1], ALU.mult, sh2[:, c:c + 1], ALU.add,
                           r=["ps%d" % b, "sc2p", "mod"], w=["h32_%d" % c])
            CP("pool", h2T[:, :, i8 * 128:(i8 + 1) * 128], h32, r=["h32_%d" % c for c in range(16)], w=["h2T"])
            for k in range(16):
                MM(ps[6][:, 0:32], h32[:, k, :], rw[:, k, :], start=(k == 0), stop=(k == 15), r=["h32_%d" % k, "rw"], w=["ps6"])
            TT("dve", lg, ps[6][:, 0:32], rb_bc, ALU.add, r=["ps6", "rb"], w=["lg"])
            S.add("dve", lambda e: e.max(out=m8, in_=lg), ["lg"], ["m8"])
            TS("dve", msk, lg, m8[:, 3:4], ALU.is_ge, r=["lg", "m8"], w=["msk"])
            TS("dve", nmx, m8[:, 0:1], -1.0, ALU.mult, r=["m8"], w=["nmx"])
            ACT(ex, lg, AF.Exp, r=["lg", "nmx"], w=["ex"], bias=nmx[:, 0:1])
            STT("dve", ex, ex, 1.0, msk, ALU.mult, ALU.mult, r=["ex", "msk"], w=["ex", "ssum"], accum=ssum[:, 0:1])
            RECIP(ssum, ssum, r=["ssum"], w=["ssum"])
            TS("dve", comb[:, i8, :], ex, ssum[:, 0:1], ALU.mult, r=["ex", "ssum"], w=["comb"])
        BARRIER()
        if blk == 0:
            for q in range(NSE - 1):
                issue_e(q)
        for e in range(32):
            for i8 in range(8):
                b = 6 + (i8 // 4)
                MM(ps[b][:, (i8 % 4) * 128:(i8 % 4 + 1) * 128], comb[:, i8, e:e + 1].to_broadcast([128, 128]), ident,
                   r=["comb", "ident"], w=["ps%d" % b])
            CP("act", combs[:, 0:512], ps[6][:, :], r=["ps6"], w=["combs"])
            CP("act", combs[:, 512:1024], ps[7][:, :], r=["ps7"], w=["combs"])
            for m in range(16):
                issue_e(n + NSE - 1)
                s = n % NSE
                n += 1
                wres = "wE%d" % s
                for t2_ in range(2):
                    tsl = slice(t2_ * 512, (t2_ + 1) * 512)
                    q2 = (m * 2 + t2_) % 2
                    for k in range(16):
                        MM(ps[0 + t2_][:, :], wE[s][:, k, 0:256:2], h2T[:, k, tsl], start=(k == 0), stop=(k == 15), r=[wres, "h2T"], w=["ps%d" % t2_])
                    for k in range(16):
                        MM(ps[2 + t2_][:, :], wE[s][:, k, 1:256:2], h2T[:, k, tsl], start=(k == 0), stop=(k == 15), r=[wres, "h2T"], w=["ps%d" % (2 + t2_)])
                    TS("dve", gsb[q2], ps[0 + t2_][:, :], bg[:, e, m, 0:1], ALU.add, 7.0, ALU.min, r=["ps%d" % t2_, "bg"], w=["gsb%d" % q2])
                    ACT(sgb[q2], gsb[q2], AF.Sigmoid, r=["gsb%d" % q2], w=["sgb%d" % q2], scale=1.702)
                    TS("dve", usb[q2], ps[2 + t2_][:, :], bg[:, e, m, 1:2], ALU.add, 7.0, ALU.min, r=["ps%d" % (2 + t2_), "bg"], w=["usb%d" % q2])
                    TS("dve", usb[q2], usb[q2], -7.0, ALU.max, 1.0, ALU.add, r=["usb%d" % q2], w=["usb%d" % q2])
                    TT("dve", gsb[q2], gsb[q2], sgb[q2], ALU.mult, r=["gsb%d" % q2, "sgb%d" % q2], w=["gsb%d" % q2])
                    TT("dve", actT[:, m, tsl], gsb[q2], usb[q2], ALU.mult, r=["gsb%d" % q2, "usb%d" % q2], w=["actT%d" % m])
            ar = ["actT%d" % m for m in range(16)]
            for j2 in range(8):
                issue_e(n + NSE - 1)
                s = n % NSE
                n += 1
                wres = "wE%d" % s
                for jj in range(2):
                    j = j2 * 2 + jj
                    for t2_ in range(2):
                        tsl = slice(t2_ * 512, (t2_ + 1) * 512)
                        b = 4 + (jj * 2 + t2_) % 2
                        q2 = (jj * 2 + t2_) % 2
                        for k in range(16):
                            MM(ps[b][:, :], wE[s][:, k, jj * 128:(jj + 1) * 128], actT[:, k, tsl], start=(k == 0), stop=(k == 15),
                               r=[wres] + ar, w=["ps%d" % b])
                        if e == 0:
                            STT("dve", accT[:, j, tsl], ps[b][:, :], bd[:, e, j:j + 1], combs[:, tsl], ALU.add, ALU.mult,
                                r=["ps%d" % b, "bd", "combs"], w=["accT%d" % j])
                        else:
                            STT("dve", tcb[q2], ps[b][:, :], bd[:, e, j:j + 1], combs[:, tsl], ALU.add, ALU.mult,
                                r=["ps%d" % b, "bd", "combs"], w=["tcb%d" % q2])
                            TT("pool", accT[:, j, tsl], accT[:, j, tsl], tcb[q2], ALU.add, r=["accT%d" % j, "tcb%d" % q2], w=["accT%d" % j])
        accr = ["accT%d" % j for j in range(16)]
        for j in range(16):
            TS("dve", accT[:, j, :], accT[:, j, :], g2c[:, j:j + 1], ALU.mult, r=["accT%d" % j, "mod"], w=["accT%d" % j])
        BARRIER()
        alias_r = []
        DMA("sp", lnw2, ln2_w_d.partition_broadcast(128), "sE_lnw", r=[], w=["lnw2"])
        DMA("sp", lnb2, ln2_b_d.partition_broadcast(128), "sE_lnb", r=[], w=["lnb2"])
        for i8 in range(8):
            i = blk * 8 + i8
            q2 = i8 % 2
            DMA("sp", x1e[0], x1_s[i * 128:(i + 1) * 128, :], "sE_x1e0", w=["x1e0"])
            for cq in range(4):
                b = cq % 2
                for cc in range(4):
                    c = cq * 4 + cc
                    TR(ps[b][:, cc * 128:(cc + 1) * 128], accT[:, c, i8 * 128:(i8 + 1) * 128], ident, r=["accT%d" % c, "ident"], w=["ps%d" % b])
                STT("dve", v2[q2][:, cq * 512:(cq + 1) * 512], x1e[0][:, cq * 512:(cq + 1) * 512], ALU_ALPHA, ps[b][:, :], ALU.mult, ALU.add,
                    r=["x1e0", "ps%d" % b], w=["v2_%d" % q2])
            V = v2[q2]
            st = st2[q2]
            vr = "v2_%d" % q2
            sr = "st2_%d" % q2
            TS("dve", junk2, V, 1.0, ALU.mult, 0.0, ALU.add, r=[vr], w=["junk2", sr], accum=st[:, 0:1])
            TS("dve", st[:, 1:2], st[:, 0:1], -1.0 / D, ALU.mult, r=[sr], w=[sr])
            ACT(junk2, V, AF.Square, r=[vr, sr], w=["junk2", sr], bias=st[:, 1:2], accum=st[:, 2:3])
            ACT(st[:, 3:4], st[:, 2:3], AF.Sqrt, r=[sr], w=[sr], scale=1.0 / D, bias=1e-5)
            RECIP(st[:, 3:4], st[:, 3:4], r=[sr], w=[sr])
            TS("dve", V, V, st[:, 1:2], ALU.add, st[:, 3:4], ALU.mult, r=[vr, sr], w=[vr])
            TT("dve", V, V, lnw2, ALU.mult, r=[vr, "lnw2"], w=[vr])
            TT("pool", V, V, lnb2, ALU.add, r=[vr, "lnb2"], w=[vr])
            DMA("sp", y_d[i * 128:(i + 1) * 128, :], V, "out_y%d" % q2, r=[vr])
        BARRIER()

    return done()


ALU_ALPHA = ALPHA

_CACHE = {}


def _rope_tables():
    n_tok = 2048
    rows = n_tok // 64
    row = np.repeat(np.arange(rows, dtype=np.float32), 64)
    col = np.tile(np.arange(64, dtype=np.float32), rows)
    n_freq = 16
    inv = (np.float32(10000.0) ** (-np.arange(n_freq, dtype=np.float32) / n_freq)).astype(np.float32)
    ang = np.concatenate([row[:, None] * inv, col[:, None] * inv], -1).astype(np.float32)
    cos = np.cos(ang).astype(np.float32)
    sin = np.sin(ang).astype(np.float32)
    cos2 = np.concatenate([cos, cos], 1).T.copy()
    sin2 = np.concatenate([-sin, sin], 1).T.copy()
    return cos2, sin2


def kernel(x_prompt, x_sample, cache_mla_ckv, cache_mla_krope, state_ssm, c, c_ctx,
           w_ada, b_ada, w_in, conv_w, conv_b, a_log, dt_bias, d_skip, ssm_norm_w, w_ssm_out,
           q_a_norm_w, w_q_b, kv_a_norm_w, w_kv_b, w_mla_out, w_o, ln1_w, ln1_b,
           router_w, router_b, w_gu, b_gu, w_down, b_down, ln2_w, ln2_b):
    f = lambda a: np.ascontiguousarray(np.asarray(a, dtype=np.float32))
    if "nc" not in _CACHE:
        _CACHE["nc"] = build_program()
    nc, _ = _CACHE["nc"]
    shared = {
        "w_ada": f(w_ada[0]), "b_ada": f(b_ada[0]), "w_in": f(w_in[0]), "conv_w": f(conv_w[0]), "conv_b": f(conv_b[0]),
        "a_log": f(a_log[0]).reshape(128), "dt_bias": f(dt_bias[0]).reshape(128), "d_skip": f(d_skip[0]),
        "ssm_norm_w": f(ssm_norm_w[0]), "w_ssm_out": f(w_ssm_out[0]), "q_a_norm_w": f(q_a_norm_w[0]), "w_q_b": f(w_q_b[0]),
        "kv_a_norm_w": f(kv_a_norm_w[0]), "w_kv_b": f(w_kv_b[0]), "w_mla_out": f(w_mla_out[0]), "w_o": f(w_o[0]),
        "ln1_w": f(ln1_w[0]), "ln1_b": f(ln1_b[0]), "router_w": f(router_w[0]), "router_b": f(router_b[0]),
        "w_gu": f(w_gu[0]) if STOP_AFTER is None else f(w_gu[0][:1]), "b_gu": f(b_gu[0]),
        "w_down": f(w_down[0]) if STOP_AFTER is None else f(w_down[0][:1]), "b_down": f(b_down[0]),
        "ln2_w": f(ln2_w[0]), "ln2_b": f(ln2_b[0]),
        "ident": np.eye(128, dtype=np.float32),
        "uf": np.triu(np.ones((128, 128), np.float32)),
        "ub": np.tril(np.ones((128, 128), np.float32)),
        "ones": np.ones((128, 128), np.float32),
    }
    khot = np.zeros((9, NKEY), np.float32)
    for t in range(T):
        khot[t // 256, t] = 1.0
    khot[8, T:] = 1.0
    shared["khot"] = khot
    cos2, sin2 = _rope_tables()
    pen_prompt = np.full((9, T), -16384.0, np.float32)
    for t in range(T):
        pen_prompt[t // 256, t] = 0.0
    in_maps = []
    for core in range(8):
        m = dict(shared)
        if core < 4:
            b = core
            m["x"] = f(x_sample[b])
            m["cond"] = f(c[b])
            m["ctx_ckv"] = f(cache_mla_ckv[b, 0])
            m["ctx_kr"] = f(cache_mla_krope[b, 0])
            m["h0"] = f(state_ssm[b, 0])
            m["carry"] = np.ones(128, np.float32)
            m["cos2"] = cos2
            m["sin2"] = sin2
            m["qpen"] = np.zeros((9, T), np.float32)
        else:
            s0 = (core - 4) * 8
            m["x"] = f(x_prompt[s0:s0 + 8]).reshape(T, D)
            m["cond"] = f(c_ctx)
            m["ctx_ckv"] = np.zeros((512, 512), np.float32)
            m["ctx_kr"] = np.zeros((512, 64), np.float32)
            m["h0"] = np.zeros((2, 64, 64, 128), np.float32)
            m["carry"] = np.zeros(128, np.float32)
            m["cos2"] = np.ones((64, T), np.float32)
            m["sin2"] = np.zeros((64, T), np.float32)
            m["qpen"] = pen_prompt
        in_maps.append(m)
    if DEBUG_CORES is not None:
        sub = [in_maps[i] for i in DEBUG_CORES]
        res = run_bass_kernel_spmd(nc, sub, core_ids=list(range(len(sub))))
        _CACHE["last"] = {c: res.results[j] for j, c in enumerate(DEBUG_CORES)}
        return None
    res = run_bass_kernel_spmd(nc, in_maps, core_ids=list(range(8)))
    R = res.results
    _CACHE["last"] = R
    y_s = np.stack([R[i]["y"] for i in range(4)], 0)
    y_p = np.concatenate([R[i]["y"].reshape(8, 256, D) for i in range(4, 8)], 0)
    ckv = np.concatenate([R[i]["ckv_out"].reshape(8, 1, 256, 512) for i in range(4, 8)], 0)
    kr = np.concatenate([R[i]["kr_out"].reshape(8, 1, 256, 64) for i in range(4, 8)], 0)
    st = np.concatenate([R[i]["st_out"].reshape(8, 1, 2, 64, 64, 128) for i in range(4, 8)], 0)
    return (y_p.astype(np.float32), y_s.astype(np.float32), ckv.astype(np.float32), kr.astype(np.float32), st.astype(np.float32))
```

```python
import contextlib
import numpy as np
import concourse.bass as bass
import concourse.mybir as mybir
from concourse.bass_utils import run_bass_kernel_spmd

F32 = mybir.dt.float32
BF16 = mybir.dt.bfloat16
AF = mybir.ActivationFunctionType
ALU = mybir.AluOpType

DEBUG = False
STOP_AFTER = None
DEBUG_CORES = None
DEBUG_KEEP = None

T = 2048
D = 2048
NT = 16
KC = 16
ALPHA = 2.0 ** 0.25
SCALE = 192.0 ** -0.5
NKEY = 2560
COMPUTE = ("pe", "act", "dve", "pool")


class Op:
    __slots__ = ("eng", "fn", "deps", "dom", "seq", "waits", "vc", "idx")


class Sched:
    def __init__(self, nc):
        self.nc = nc
        self.ops = []
        self.res_w = {}
        self.res_r = {}
        self.dom_count = {}
        self.dom_ops = {}
        self.bar_idx = None
        self.bar_start = 0
        self.barriers = []

    def add(self, eng, fn, reads=(), writes=(), key=None):
        o = Op()
        o.idx = len(self.ops)
        o.eng = eng
        o.fn = fn
        o.dom = key if key is not None else eng
        assert key is not None or eng in COMPUTE, (eng, key)
        extra = [r for r in reads if r[:2] == "ps" and r[2:].isdigit()]
        if extra:
            writes = list(writes) + extra
        deps = set()
        if self.bar_idx is not None:
            deps.add(self.bar_idx)
        for r in reads:
            w = self.res_w.get(r)
            if w is not None:
                deps.add(w)
        for w_ in writes:
            w = self.res_w.get(w_)
            if w is not None:
                deps.add(w)
            for r in self.res_r.get(w_, ()):
                deps.add(r)
        for r in reads:
            self.res_r.setdefault(r, []).append(o.idx)
        for w_ in writes:
            self.res_w[w_] = o.idx
            self.res_r[w_] = []
        deps.discard(o.idx)
        o.deps = deps
        c = self.dom_count.get(o.dom, 0) + 1
        self.dom_count[o.dom] = c
        o.seq = c
        self.dom_ops.setdefault(o.dom, []).append(o.idx)
        self.ops.append(o)
        return o

    def barrier(self, fn):
        o = self.add("dve", fn)
        last = {}
        for i in range(self.bar_start, o.idx):
            p = self.ops[i]
            last[p.dom] = i
        o.deps = set(last.values())
        if self.bar_idx is not None:
            o.deps.add(self.bar_idx)
        self.bar_idx = o.idx
        self.bar_start = o.idx
        self.barriers.append(o.idx)
        self.res_w = {}
        self.res_r = {}

    def finalize(self, final_wait_prefix="out_"):
        ops = self.ops
        know = {e: {} for e in ("pe", "act", "dve", "pool", "sp")}
        needed = set()
        for o in ops:
            k = know[o.eng]
            waits = {}
            for d in o.deps:
                dop = ops[d]
                if dop.dom == "pe" and o.eng == "pe":
                    continue
                if k.get(dop.dom, 0) >= dop.seq:
                    continue
                if waits.get(dop.dom, 0) < dop.seq:
                    waits[dop.dom] = dop.seq
            real = {}
            for dom, seq in waits.items():
                if k.get(dom, 0) >= seq:
                    continue
                k = dict(k)
                dop = ops[self.dom_ops[dom][seq - 1]]
                for kd, kv in dop.vc.items():
                    if k.get(kd, 0) < kv:
                        k[kd] = kv
                k[dom] = max(k.get(dom, 0), seq)
                needed.add((dom, seq))
                real[dom] = seq
            know[o.eng] = k
            o.waits = real
            o.vc = k
        self.final_waits = []
        for dom, c in self.dom_count.items():
            if isinstance(dom, str) and dom.startswith(final_wait_prefix):
                needed.add((dom, c))
                self.final_waits.append((dom, c))
        self.sig_val = {}
        for dom, lst in self.dom_ops.items():
            n = 0
            for s in range(1, len(lst) + 1):
                if (dom, s) in needed or dom not in COMPUTE:
                    n += 1
                    self.sig_val[(dom, s)] = n

    def emit(self):
        nc = self.nc
        doms = sorted({d for (d, _) in self.sig_val})
        import bisect
        first = {}
        last = {}
        for o in self.ops:
            if o.dom not in COMPUTE:
                first.setdefault(o.dom, o.idx)
                last[o.dom] = o.idx
        phys = {}
        base = {}
        pool_ = []
        nphys = 0
        for d in sorted(first, key=lambda k: first[k]):
            chosen = None
            for ent in pool_:
                bi = bisect.bisect_right(self.barriers, ent[0])
                if bi < len(self.barriers) and self.barriers[bi] <= first[d]:
                    chosen = ent
                    break
            if chosen is None:
                chosen = [0, 0, nphys]
                nphys += 1
                pool_.append(chosen)
            phys[d] = chosen[2]
            base[d] = chosen[1]
            chosen[0] = last[d]
            chosen[1] += self.dom_count[d]
        self.nphys = nphys
        with contextlib.ExitStack() as st:
            psem = [st.enter_context(nc.semaphore("sd%d" % i)) for i in range(nphys)]
            sems = {}
            for d in doms:
                if d in COMPUTE:
                    sems[d] = st.enter_context(nc.semaphore("s_" + str(d)))
                else:
                    sems[d] = psem[phys[d]]
            block = st.enter_context(nc.Block())
            by_eng = {e: [] for e in ("pe", "act", "dve", "pool", "sp")}
            for o in self.ops:
                by_eng[o.eng].append(o)

            def is_dma(dom):
                return dom not in COMPUTE

            def run(engine, lst, final=False):
                for o in lst:
                    for dom, seq in o.waits.items():
                        v = self.sig_val[(dom, seq)]
                        engine.wait_ge(sems[dom], (v + base[dom]) * 16 if is_dma(dom) else v)
                    ins = o.fn(engine)
                    if (o.dom, o.seq) in self.sig_val:
                        ins.then_inc(sems[o.dom], 16 if is_dma(o.dom) else 1)
                if final:
                    for dom, c in self.final_waits:
                        v = self.sig_val[(dom, c)]
                        engine.wait_ge(sems[dom], (v + base[dom]) * 16 if is_dma(dom) else v)

            @block.tensor
            def _(e):
                run(e, by_eng["pe"])

            @block.scalar
            def _(e):
                run(e, by_eng["act"])

            @block.vector
            def _(e):
                run(e, by_eng["dve"])

            @block.gpsimd
            def _(e):
                run(e, by_eng["pool"])

            @block.sync
            def _(e):
                run(e, by_eng["sp"], final=True)


class Arena:
    def __init__(self, t, nwords):
        self.t = t
        self.n = nwords
        self.off = 0

    def f32(self, *shape):
        n = int(np.prod(shape[1:]))
        n = (n + 7) // 8 * 8
        assert self.off + n <= self.n, ("SBUF arena overflow", self.off, n, self.n)
        ap = self.t[0:shape[0], self.off:self.off + int(np.prod(shape[1:]))]
        self.off += n
        return self._shape(ap, shape)

    def bf16(self, *shape):
        ne = int(np.prod(shape[1:]))
        n = (ne + 1) // 2
        n = (n + 7) // 8 * 8
        assert self.off + n <= self.n, ("SBUF arena overflow", self.off, n, self.n)
        ap = self.t[0:shape[0], self.off:self.off + (ne + 1) // 2].bitcast(BF16)
        if ne % 2:
            ap = ap[:, 0:ne]
        self.off += n
        return self._shape(ap, shape)

    @staticmethod
    def _shape(ap, shape):
        if len(shape) == 2:
            return ap
        if len(shape) == 3:
            return ap.rearrange("p (a b) -> p a b", b=shape[2])
        if len(shape) == 4:
            return ap.rearrange("p (a b c) -> p a b c", b=shape[2], c=shape[3])
        raise ValueError(shape)


def build_program():
    nc = bass.Bass("TRN2", target_bir_lowering=False)
    S = Sched(nc)

    def done():
        S.finalize()
        S.emit()
        return nc, S

    def din(name, shape, dt=F32):
        return nc.dram_tensor(name, list(shape), dt, kind="ExternalInput").ap()

    def dout(name, shape, dt=F32):
        return nc.dram_tensor(name, list(shape), dt, kind="ExternalOutput").ap()

    def dscr(name, shape, dt=F32):
        kind = "ExternalOutput" if (DEBUG and (DEBUG_KEEP is None or name in DEBUG_KEEP)) else "Internal"
        return nc.dram_tensor(name, list(shape), dt, kind=kind).ap()

    x_d = din("x", [T, D])
    cond_d = din("cond", [D])
    w_ada_d = din("w_ada", [D, 6 * D])
    b_ada_d = din("b_ada", [6 * D])
    w_in_d = din("w_in", [D, 15552])
    conv_w_d = din("conv_w", [3, 6144])
    conv_b_d = din("conv_b", [6144])
    a_log_d = din("a_log", [128])
    dt_bias_d = din("dt_bias", [128])
    d_skip_d = din("d_skip", [64])
    ssm_norm_w_d = din("ssm_norm_w", [4096])
    w_ssm_out_d = din("w_ssm_out", [4096, D])
    q_a_norm_w_d = din("q_a_norm_w", [512])
    w_q_b_d = din("w_q_b", [512, 3072])
    kv_a_norm_w_d = din("kv_a_norm_w", [512])
    w_kv_b_d = din("w_kv_b", [512, 4096])
    w_mla_out_d = din("w_mla_out", [D, D])
    w_o_d = din("w_o", [D, D])
    ln1_w_d = din("ln1_w", [D])
    ln1_b_d = din("ln1_b", [D])
    router_w_d = din("router_w", [D, 32])
    router_b_d = din("router_b", [32])
    NEW = 32 if STOP_AFTER is None else 1
    w_gu_d = din("w_gu", [NEW, D, 4096])
    b_gu_d = din("b_gu", [32, 4096])
    w_down_d = din("w_down", [NEW, D, D])
    b_down_d = din("b_down", [32, D])
    ln2_w_d = din("ln2_w", [D])
    ln2_b_d = din("ln2_b", [D])
    ctx_ckv_d = din("ctx_ckv", [512, 512])
    ctx_kr_d = din("ctx_kr", [512, 64])
    h0_d = din("h0", [2, 64, 64, 128])
    carry_d = din("carry", [128])
    cos2_d = din("cos2", [64, T])
    sin2_d = din("sin2", [64, T])
    khot_d = din("khot", [9, NKEY])
    qpen_d = din("qpen", [9, T])
    ident_d = din("ident", [128, 128])
    uf_d = din("uf", [128, 128])
    ub_d = din("ub", [128, 128])
    ones_d = din("ones", [128, 128])

    y_d = dout("y", [T, D])
    ckv_out_d = dout("ckv_out", [T, 512])
    kr_out_d = dout("kr_out", [T, 64])
    st_out_d = dout("st_out", [8, 2, 64, 64, 128])

    g1_s = dscr("g1_s", [D])
    xs_tm_s = dscr("xs_tm_s", [T, 4096], BF16)
    b_tm_s = dscr("b_tm_s", [T, 1024], BF16)
    bct_s = dscr("bct_s", [2048, T], BF16)
    zs_s = dscr("zs_s", [T, 4096], BF16)
    dtq_s = dscr("dtq_s", [5, T, 128])
    qnT_s = dscr("qnT_s", [512, T], BF16)
    ckvT_s = dscr("ckvT_s", [512, T], BF16)
    krT_s = dscr("krT_s", [64, T], BF16)
    gT_s = dscr("gT_s", [4096, T], BF16)
    yT_s = dscr("yT_s", [4096, T], BF16)
    oT_s = dscr("oT_s", [2048, T], BF16)
    x1_s = dscr("x1_s", [T, D])

    NW = (nc.sbuf_bytes_remaining - 6144) // 4
    NW = NW // 8 * 8
    arena_t = nc.alloc_sbuf_tensor("arena", [128, NW], F32)
    PERS_W = 1024
    pers = Arena(arena_t, PERS_W)
    ps = [nc.alloc_psum_tensor("ps%d" % i, [128, 512], F32) for i in range(8)]
    psb = [p[:].bitcast(BF16) for p in ps]

    class StageArena(Arena):
        def __init__(self):
            self.t = arena_t
            self.n = NW
            self.off = PERS_W

    def MM(out, lhsT, rhs, start=True, stop=True, r=(), w=()):
        S.add("pe", lambda e: e.matmul(out, lhsT=lhsT, rhs=rhs, start=start, stop=stop), r, w)

    def TR(out, in_, idn, r=(), w=()):
        S.add("pe", lambda e: e.transpose(out, in_, idn), r, w)

    def ACT(out, in_, func, r=(), w=(), bias=None, scale=None, accum=None):
        kw = {}
        if bias is not None:
            kw["bias"] = bias
        if scale is not None:
            kw["scale"] = scale
        if accum is not None:
            kw["accum_out"] = accum
        S.add("act", lambda e: e.activation(out=out, in_=in_, func=func, **kw), r, w)

    def TT(eng, out, in0, in1, op, r=(), w=()):
        S.add(eng, lambda e: e.tensor_tensor(out=out, in0=in0, in1=in1, op=op), r, w)

    def TS(eng, out, in0, s1, op0, s2=None, op1=None, r=(), w=(), accum=None):
        kw = {}
        if op1 is not None:
            kw["op1"] = op1
        if accum is not None:
            kw["accum_out"] = accum
        S.add(eng, lambda e: e.tensor_scalar(out=out, in0=in0, scalar1=s1, scalar2=s2, op0=op0, **kw), r, w)

    def STT(eng, out, in0, scalar, in1, op0, op1, r=(), w=(), accum=None):
        kw = {}
        if accum is not None:
            kw["accum_out"] = accum
        S.add(eng, lambda e: e.scalar_tensor_tensor(out=out, in0=in0, scalar=scalar, in1=in1, op0=op0, op1=op1, **kw), r, w)

    def CP(eng, out, in_, r=(), w=()):
        if eng == "act":
            S.add("act", lambda e: e.copy(out=out, in_=in_), r, w)
        else:
            S.add(eng, lambda e: e.tensor_copy(out=out, in_=in_), r, w)

    def RECIP(out, in_, r=(), w=()):
        S.add("dve", lambda e: e.reciprocal(out=out, in_=in_), r, w)

    def DMA(q, out, in_, key, r=(), w=(), slow=False):
        if slow:
            S.add(q, lambda e: e.dma_start(out=out, in_=in_, allow_slow_non_contiguous=True), r, w, key=key)
        else:
            S.add(q, lambda e: e.dma_start(out=out, in_=in_), r, w, key=key)

    def MEMSET(eng, ap, val, r=(), w=()):
        S.add(eng, lambda e: e.memset(ap, val), r, w)

    bar_t = pers.f32(128, 8)

    def BARRIER():
        S.barrier(lambda e: e.memset(bar_t, 0.0))

    ident = pers.f32(128, 128)
    identb = pers.bf16(128, 128)
    onesf = pers.f32(128, 128)
    onesb = pers.bf16(128, 128)
    uf = pers.f32(128, 128)
    ub = pers.f32(128, 128)
    mod = pers.f32(128, 96)
    sc1p = pers.f32(128, 16)
    sc2p = pers.f32(128, 16)
    carry = pers.f32(128, 1)
    cm1 = pers.f32(128, 1)
    DMA("sp", ident, ident_d, "c_ident", w=["ident"])
    DMA("pool", identb, ident_d, "c_identb", w=["identb"])
    DMA("sp", onesf, ones_d, "c_ones", w=["onesf"])
    DMA("pool", onesb, ones_d, "c_onesb", w=["onesb"])
    DMA("sp", uf, uf_d, "c_uf", w=["uf"])
    DMA("sp", ub, ub_d, "c_ub", w=["ub"])
    DMA("sp", carry, carry_d.rearrange("(p o) -> p o", o=1), "c_carry", w=["carry"], slow=True)
    TS("dve", cm1, carry, -1.0, ALU.add, r=["carry"], w=["cm1"])

    A = StageArena()
    condc = A.f32(128, 16)
    silc = A.f32(128, 16)
    bada = A.f32(128, 96)
    wsl = [A.f32(128, 16, 1024) for _ in range(2)]
    DMA("sp", condc, cond_d.rearrange("(c p) -> p c", p=128), "s0_cond", w=["condc"], slow=True)
    DMA("sp", bada, b_ada_d.rearrange("(j p) -> p j", p=128), "s0_bada", w=["bada"], slow=True)
    ACT(silc, condc, AF.Silu, r=["condc"], w=["silc"])
    w_ada_v = w_ada_d.rearrange("(k p) n -> p k n", p=128)
    for blk in range(12):
        s = blk % 2
        DMA("sp" if blk % 2 == 0 else "act", wsl[s], w_ada_v[:, :, blk * 1024:(blk + 1) * 1024], "s0_w%d" % s, w=["wada%d" % s])
        for mm in range(8):
            col = blk * 8 + mm
            for k in range(KC):
                MM(ps[0][:, col:col + 1], wsl[s][:, k, mm * 128:(mm + 1) * 128], silc[:, k:k + 1],
                   start=(k == 0), stop=(k == KC - 1), r=["wada%d" % s, "silc"], w=["ps0"])
    TT("dve", mod, ps[0][:, 0:96], bada, ALU.add, r=["ps0", "bada"], w=["mod"])
    TS("dve", sc1p, mod[:, 16:32], 1.0, ALU.add, r=["mod"], w=["sc1p"])
    TS("dve", sc2p, mod[:, 64:80], 1.0, ALU.add, r=["mod"], w=["sc2p"])
    DMA("sp", g1_s.rearrange("(c p) -> p c", p=128), mod[:, 32:48], "s0_g1", r=["mod"], slow=True)
    sh1 = mod[:, 0:16]
    sh2 = mod[:, 48:64]
    g2c = mod[:, 80:96]
    BARRIER()
    if STOP_AFTER == '0':
        return done()

    A = StageArena()
    hT = A.bf16(128, 16, T)
    mark_h = A.off
    xsl = [A.f32(128, D) for _ in range(2)]
    for i in range(NT):
        s = i % 2
        DMA("sp", xsl[s], x_d[i * 128:(i + 1) * 128, :], "s1_x%d" % s, w=["xs%d" % s])
        for cq in range(4):
            b = cq
            for cc in range(4):
                c = cq * 4 + cc
                TR(ps[b][:, cc * 128:(cc + 1) * 128], xsl[s][:, c * 128:(c + 1) * 128], ident,
                   r=["xs%d" % s, "ident"], w=["ps%d" % b])
            for cc in range(4):
                c = cq * 4 + cc
                o_ = hT[:, c, i * 128:(i + 1) * 128]
                i_ = ps[b][:, cc * 128:(cc + 1) * 128]
                if cc % 2 == 0:
                    ACT(o_, i_, AF.Identity, r=["ps%d" % b, "sc1p", "mod"], w=["hT"], scale=sc1p[:, c:c + 1], bias=sh1[:, c:c + 1])
                else:
                    TS("dve", o_, i_, sc1p[:, c:c + 1], ALU.mult, sh1[:, c:c + 1], ALU.add, r=["ps%d" % b, "sc1p", "mod"], w=["hT"])
    BARRIER()
    if STOP_AFTER == '1':
        return done()

    A.off = mark_h
    w_in_v = w_in_d.rearrange("(k p) n -> p k n", p=128)
    NSL = 3
    wsl = [A.bf16(128, 16, 512) for _ in range(NSL)]
    cw = A.f32(128, 48, 3)
    cb = A.f32(128, 48)
    w0c = A.f32(128, 48)
    w2c = A.f32(128, 48)
    qnw = A.f32(128, 4)
    kvw = A.f32(128, 4)
    mark_u = A.off
    cos2 = A.f32(64, T)
    sin2 = A.f32(64, T)
    for j in range(3):
        DMA("sp", cw[:, :, j], conv_w_d[j].rearrange("(c p) -> p c", p=128), "sA_cw%d" % j, w=["cw%d" % j], slow=True)
    DMA("sp", cb, conv_b_d.rearrange("(c p) -> p c", p=128), "sA_cb", w=["cb"], slow=True)
    DMA("sp", qnw, q_a_norm_w_d.rearrange("(c p) -> p c", p=128), "sA_qnw", w=["qnw"], slow=True)
    DMA("sp", kvw, kv_a_norm_w_d.rearrange("(c p) -> p c", p=128), "sA_kvw", w=["kvw"], slow=True)
    DMA("sp", cos2, cos2_d, "sA_cos", w=["cos2"])
    DMA("sp", sin2, sin2_d, "sA_sin", w=["sin2"])
    TS("dve", w0c, cw[:, :, 0], cm1[:, 0:1], ALU.mult, r=["cw0", "cm1"], w=["w0c"])
    TS("dve", w2c, cw[:, :, 2], cm1[:, 0:1], ALU.mult, r=["cw2", "cm1"], w=["w2c"])

    groups = []
    groups.append(("qa", 10368, 0))
    groups.append(("kva", 10880, 0))
    for g in range(12):
        groups.append(("xbc", 4096 + g * 512, g))
    for g in range(8):
        groups.append(("gate", 11456 + g * 512, g))
    for g in range(8):
        groups.append(("z", g * 512, g))

    def issue_w(n):
        if n < len(groups):
            s = n % NSL
            c0 = groups[n][1]
            DMA("pool", wsl[s], w_in_v[:, :, c0:c0 + 512], "sA_w%d" % s, w=["wA%d" % s])

    sq = A.f32(128, 4, 512)
    rt = A.f32(128, 512)
    rstd = A.f32(128, 512)
    nT = A.bf16(128, 4, T)
    ckn32 = A.f32(128, 4, 512)
    otile = [A.f32(128, 512) for _ in range(2)]
    wkr = A.bf16(128, 16, 64)
    wkrs = A.bf16(128, 16, 64)
    kr32 = A.f32(64, 512)
    krt1 = A.f32(64, 512)
    krt2 = A.f32(64, 512)
    krT = A.bf16(64, T)
    kro = [A.f32(128, 4, 64) for _ in range(2)]
    DMA("pool", wkr, w_in_v[:, :, 11392:11456], "sA_wkr", w=["wkr"])
    CP("dve", wkrs[:, :, 0:32], wkr[:, :, 32:64], r=["wkr"], w=["wkrs"])
    CP("dve", wkrs[:, :, 32:64], wkr[:, :, 0:32], r=["wkr"], w=["wkrs"])

    for n in range(NSL - 1):
        issue_w(n)
    bankrr = [0]

    def next_bank():
        b = bankrr[0] % 4
        bankrr[0] += 1
        return b

    chunk_ctr = [0]
    for n, (kind, c0, g) in enumerate(groups):
        issue_w(n + NSL - 1)
        s = n % NSL
        wres = "wA%d" % s
        if n == 2:
            BARRIER()
            if STOP_AFTER in ("Aqa", "Aqa_a", "Aqa_b", "Aqa_c", "Aqa_d"):
                return done()
            A.off = mark_u
            pre = [A.f32(128, T + 2) for _ in range(2)]
            acc = A.f32(128, T)
            post = [A.bf16(128, T) for _ in range(2)]
            tm = [A.bf16(128, 16, 128) for _ in range(2)]
            zt = [A.bf16(128, 512) for _ in range(4)]
            for s_ in range(2):
                MEMSET("dve", pre[s_][:, 0:1], 0.0, w=["pre%d" % s_])
                MEMSET("dve", pre[s_][:, T + 1:T + 2], 0.0, w=["pre%d" % s_])
        if (STOP_AFTER == "Ap" and n == 0) or (STOP_AFTER == "Aqa1" and n == 1):
            BARRIER()
            return done()
        if STOP_AFTER == "Ax1" and n == 3:
            BARRIER()
            return done()
        if kind == "xbc":
            for j in range(4):
                cc = g * 4 + j
                pslot = chunk_ctr[0] % 2
                chunk_ctr[0] += 1
                P_ = pre[pslot]
                for tb in range(4):
                    b = next_bank()
                    for k in range(KC):
                        MM(ps[b][:, :], wsl[s][:, k, j * 128:(j + 1) * 128], hT[:, k, tb * 512:(tb + 1) * 512],
                           start=(k == 0), stop=(k == KC - 1), r=[wres, "hT"], w=["ps%d" % b])
                    CP("act", P_[:, 1 + tb * 512:1 + (tb + 1) * 512], ps[b][:, :], r=["ps%d" % b], w=["pre%d" % pslot])
                pr = ["pre%d" % pslot]
                TS("dve", acc, P_[:, 0:T], cw[:, cc, 0:1], ALU.mult, r=pr + ["cw0"], w=["acc"])
                STT("dve", acc, P_[:, 1:T + 1], cw[:, cc, 1:2], acc, ALU.mult, ALU.add, r=pr + ["cw1", "acc"], w=["acc"])
                STT("dve", acc, P_[:, 2:T + 2], cw[:, cc, 2:3], acc, ALU.mult, ALU.add, r=pr + ["cw2", "acc"], w=["acc"])
                accv = acc.rearrange("p (s t) -> p s t", t=256)
                xv = P_[:, 1:T + 1].rearrange("p (s t) -> p s t", t=256)
                STT("dve", accv[:, 1:8, 0:1], xv[:, 0:7, 255:256], w0c[:, cc:cc + 1], accv[:, 1:8, 0:1], ALU.mult, ALU.add,
                    r=pr + ["w0c", "acc"], w=["acc"])
                STT("dve", accv[:, 0:7, 255:256], xv[:, 1:8, 0:1], w2c[:, cc:cc + 1], accv[:, 0:7, 255:256], ALU.mult, ALU.add,
                    r=pr + ["w2c", "acc"], w=["acc"])
                ACT(post[pslot], acc, AF.Silu, r=["acc", "cb"], w=["post%d" % pslot], bias=cb[:, cc:cc + 1])
                if cc < 40:
                    for half in range(2):
                        b = 4 + half
                        for i in range(8):
                            tix = half * 8 + i
                            TR(psb[b][:, i * 128:(i + 1) * 128], post[pslot][:, tix * 128:(tix + 1) * 128], identb,
                               r=["post%d" % pslot, "identb"], w=["ps%d" % b])
                        CP("dve", tm[pslot][:, half * 8:(half + 1) * 8, :], psb[b][:, :].rearrange("p (a b) -> p a b", b=128),
                           r=["ps%d" % b], w=["tm%d" % pslot])
                    if cc < 32:
                        dst = xs_tm_s.rearrange("(i p) c -> p i c", p=128)[:, :, cc * 128:(cc + 1) * 128]
                    else:
                        dst = b_tm_s.rearrange("(i p) c -> p i c", p=128)[:, :, (cc - 32) * 128:(cc - 31) * 128]
                    DMA("sp", dst, tm[pslot], "sA_tm%d" % pslot, r=["tm%d" % pslot])
                if cc >= 32:
                    DMA("sp", bct_s[(cc - 32) * 128:(cc - 31) * 128, :], post[pslot], "sA_post%d" % pslot, r=["post%d" % pslot])
        elif kind in ("qa", "kva"):
            nw = qnw if kind == "qa" else kvw
            for tb in range(4):
                tsl = slice(tb * 512, (tb + 1) * 512)
                for c in range(4):
                    for k in range(KC):
                        MM(ps[c][:, :], wsl[s][:, k, c * 128:(c + 1) * 128], hT[:, k, tsl],
                           start=(k == 0), stop=(k == KC - 1), r=[wres, "hT"], w=["ps%d" % c])
                for c in range(4):
                    ACT(sq[:, c, :], ps[c][:, :], AF.Square, r=["ps%d" % c], w=["sq"])
                for c in range(4):
                    MM(ps[6][:, :], onesf, sq[:, c, :], start=(c == 0), stop=(c == 3), r=["sq", "onesf"], w=["ps6"])
                ACT(rt, ps[6][:, :], AF.Sqrt, r=["ps6"], w=["rt"], scale=1.0 / 512.0, bias=1e-6)
                RECIP(rstd, rt, r=["rt"], w=["rstd"])
                if kind == "qa":
                    for c in range(4):
                        STT("dve", nT[:, c, tsl], ps[c][:, :], nw[:, c:c + 1], rstd, ALU.mult, ALU.mult,
                            r=["ps%d" % c, "qnw", "rstd"], w=["nT"])
                else:
                    for c in range(4):
                        STT("dve", ckn32[:, c, :], ps[c][:, :], nw[:, c:c + 1], rstd, ALU.mult, ALU.mult,
                            r=["ps%d" % c, "kvw", "rstd"], w=["ckn32"])
                    CP("pool", nT[:, :, tsl], ckn32, r=["ckn32"], w=["nT"])
                    for i4 in range(4 if STOP_AFTER != "Aqa_b" else 0):
                        os_ = (tb * 4 + i4) % 2
                        for c in range(4):
                            TR(ps[4][:, c * 128:(c + 1) * 128], ckn32[:, c, i4 * 128:(i4 + 1) * 128], ident,
                               r=["ckn32", "ident"], w=["ps4"])
                        CP("act", otile[os_], ps[4][:, :], r=["ps4"], w=["otile%d" % os_])
                        row = (tb * 4 + i4) * 128
                        DMA("sp", ckv_out_d[row:row + 128, :], otile[os_], "out_ckv%d" % os_, r=["otile%d" % os_])
                    if STOP_AFTER == "Aqa_a":
                        continue
                    for k in range(KC):
                        MM(ps[5][0:64, :], wkr[:, k, :], hT[:, k, tsl], start=(k == 0), stop=(k == KC - 1), r=["wkr", "hT"], w=["ps5"])
                    for k in range(KC):
                        MM(ps[7][0:64, :], wkrs[:, k, :], hT[:, k, tsl], start=(k == 0), stop=(k == KC - 1), r=["wkrs", "hT"], w=["ps7"])
                    if STOP_AFTER == "Aqa_d":
                        continue
                    CP("act", kr32, ps[5][0:64, :], r=["ps5"], w=["kr32"])
                    TT("dve", krt1, ps[5][0:64, :], cos2[:, tsl], ALU.mult, r=["ps5", "cos2"], w=["krt1"])
                    TT("dve", krt2, ps[7][0:64, :], sin2[:, tsl], ALU.mult, r=["ps7", "sin2"], w=["krt2"])
                    TT("dve", krT[:, tsl], krt1, krt2, ALU.add, r=["krt1", "krt2"], w=["krT"])
                    ks_ = tb % 2
                    if STOP_AFTER == "Aqa_c":
                        continue
                    for i4 in range(4):
                        TR(ps[6][:, i4 * 64:(i4 + 1) * 64], kr32[:, i4 * 128:(i4 + 1) * 128], ident[0:64, 0:64],
                           r=["kr32", "ident"], w=["ps6"])
                    CP("act", kro[ks_], ps[6][:, 0:256].rearrange("p (a b) -> p a b", b=64), r=["ps6"], w=["kro%d" % ks_])
                    DMA("sp", kr_out_d[tb * 512:(tb + 1) * 512, :].rearrange("(a p) c -> p a c", p=128), kro[ks_],
                        "out_kr%d" % ks_, r=["kro%d" % ks_])
            if kind == "qa":
                DMA("sp", qnT_s.rearrange("(c p) t -> p c t", p=128), nT, "sA_nT", r=["nT"])
            else:
                DMA("sp", ckvT_s.rearrange("(c p) t -> p c t", p=128), nT, "sA_nT", r=["nT"])
                DMA("sp", krT_s, krT, "sA_krT", r=["krT"])
        elif kind == "gate":
            for j in range(4):
                cc = g * 4 + j
                pslot = chunk_ctr[0] % 2
                chunk_ctr[0] += 1
                for tb in range(4):
                    b = next_bank()
                    for k in range(KC):
                        MM(ps[b][:, :], wsl[s][:, k, j * 128:(j + 1) * 128], hT[:, k, tb * 512:(tb + 1) * 512],
                           start=(k == 0), stop=(k == KC - 1), r=[wres, "hT"], w=["ps%d" % b])
                    ACT(post[pslot][:, tb * 512:(tb + 1) * 512], ps[b][:, :], AF.Sigmoid, r=["ps%d" % b], w=["post%d" % pslot])
                DMA("sp", gT_s[cc * 128:(cc + 1) * 128, :], post[pslot], "sA_post%d" % pslot, r=["post%d" % pslot])
        else:
            for i in range(NT):
                b = next_bank()
                zs_ = i % 4
                for k in range(KC):
                    MM(ps[b][:, :], hT[:, k, i * 128:(i + 1) * 128], wsl[s][:, k, :],
                       start=(k == 0), stop=(k == KC - 1), r=[wres, "hT"], w=["ps%d" % b])
                ACT(zt[zs_], ps[b][:, :], AF.Silu, r=["ps%d" % b], w=["zt%d" % zs_])
                DMA("sp", zs_s[i * 128:(i + 1) * 128, g * 512:(g + 1) * 512], zt[zs_], "sA_zt%d" % zs_, r=["zt%d" % zs_])
    BARRIER()
    if STOP_AFTER == 'A':
        return done()

    A.off = mark_h
    wdt = A.bf16(128, 16, 128)
    dtraw = A.f32(128, 16, 128)
    dte = A.f32(128, 16, 128)
    dtv = A.f32(128, 16, 128)
    lndt = A.f32(128, 16, 128)
    qa_ = A.f32(128, 16, 128)
    qbexp = A.f32(128, 16, 128)
    qeac = A.f32(128, 16, 128)
    qwdec = A.f32(128, 16, 128)
    qcdec = A.f32(128, 16, 128)
    dtb_bc = A.f32(128, 128)
    alog_bc = A.f32(128, 128)
    Abc = A.f32(128, 128)
    acs = [A.f32(128, 128) for _ in range(2)]
    tmpd = [A.f32(128, 128) for _ in range(2)]
    DMA("pool", wdt, w_in_v[:, :, 10240:10368], "sA3_w", w=["wdt"])
    DMA("sp", dtb_bc, dt_bias_d.partition_broadcast(128), "sA3_dtb", w=["dtb"])
    DMA("sp", alog_bc, a_log_d.partition_broadcast(128), "sA3_alog", w=["alog"])
    ACT(Abc, alog_bc, AF.Exp, r=["alog"], w=["Abc"])
    TS("dve", Abc, Abc, -1.0, ALU.mult, r=["Abc"], w=["Abc"])
    for i in range(NT):
        b = i // 4 % 2
        for k in range(KC):
            MM(ps[b][:, (i % 4) * 128:(i % 4 + 1) * 128], hT[:, k, i * 128:(i + 1) * 128], wdt[:, k, :],
               start=(k == 0), stop=(k == KC - 1), r=["wdt", "hT"], w=["ps%d" % b])
        if i % 4 == 3:
            i0 = i - 3
            TT("dve", dtraw[:, i0:i0 + 4, :], ps[b][:, :].rearrange("p (a b) -> p a b", b=128),
               dtb_bc.unsqueeze(1).to_broadcast([128, 4, 128]), ALU.add, r=["ps%d" % b, "dtb"], w=["dtraw"])
    ACT(dte, dtraw, AF.Exp, r=["dtraw"], w=["dte"])
    ACT(dtv, dte, AF.Ln, r=["dte"], w=["dtv"], bias=1.0, scale=1.0)
    ACT(lndt, dtv, AF.Ln, r=["dtv"], w=["lndt"])
    TT("dve", qa_, dtv, Abc.unsqueeze(1).to_broadcast([128, 16, 128]), ALU.mult, r=["dtv", "Abc"], w=["qa"])
    for c in range(NT):
        s = c % 2
        MM(ps[2 + s][:, 0:64], uf, qa_[:, c, 0:64], r=["uf", "qa"], w=["ps%d" % (2 + s)])
        MM(ps[2 + s][:, 64:128], ub, qa_[:, c, 64:128], r=["ub", "qa"], w=["ps%d" % (2 + s)])
        MM(ps[4 + s][:, 0:128], onesf, qa_[:, c, :], r=["onesf", "qa"], w=["ps%d" % (4 + s)])
        CP("act", acs[s], ps[2 + s][:, 0:128], r=["ps%d" % (2 + s)], w=["acs%d" % s])
        TT("dve", qbexp[:, c, :], lndt[:, c, :], acs[s], ALU.subtract, r=["lndt", "acs%d" % s], w=["qbexp"])
        ACT(qeac[:, c, :], ps[2 + s][:, 0:128], AF.Exp, r=["ps%d" % (2 + s)], w=["qeac"])
        ACT(qcdec[:, c, :], ps[4 + s][:, 0:128], AF.Exp, r=["ps%d" % (4 + s)], w=["qcdec"])
        TT("dve", tmpd[s], ps[4 + s][:, 0:128], acs[s], ALU.subtract, r=["ps%d" % (4 + s), "acs%d" % s], w=["tmpd%d" % s])
        ACT(tmpd[s], tmpd[s], AF.Exp, r=["tmpd%d" % s], w=["tmpd%d" % s])
        TT("dve", qwdec[:, c, :], tmpd[s], dtv[:, c, :], ALU.mult, r=["tmpd%d" % s, "dtv"], w=["qwdec"])
    for qi, (qt, nm) in enumerate(((qa_, "qa"), (qbexp, "qbexp"), (qeac, "qeac"), (qwdec, "qwdec"), (qcdec, "qcdec"))):
        DMA("sp", dtq_s[qi].rearrange("(c p) n -> p c n", p=128), qt, "sA3_q%d" % qi, r=[nm])
    BARRIER()
    if STOP_AFTER == 'A3':
        return done()

    A = StageArena()
    dq = [A.f32(128, 16, 128) for _ in range(5)]
    q_a, q_bexp, q_eac, q_wdec, q_cdec = dq
    for qi in range(5):
        DMA("sp", dq[qi], dtq_s[qi].rearrange("(c p) n -> p c n", p=128), "sB_q%d" % qi, w=["dq"])
    D_bc = A.f32(128, 64)
    DMA("sp", D_bc, d_skip_d.partition_broadcast(128), "sB_D", w=["D_bc"])
    xtm = [A.bf16(128, 16, 512) for _ in range(2)]
    btm = [A.bf16(128, 16, 128) for _ in range(2)]
    BTt = [A.bf16(128, T) for _ in range(2)]
    CTt = [A.bf16(128, T) for _ in range(2)]
    normw = [A.f32(128, 512) for _ in range(2)]
    yacc = A.f32(128, 16, 512)
    S32 = [A.f32(128, 512) for _ in range(2)]
    Sbf = [A.bf16(128, 512) for _ in range(2)]
    cbm = [A.bf16(128, 128) for _ in range(2)]
    Lsb = [A.bf16(128, 8, 128) for _ in range(2)]
    mT = [A.bf16(128, 8, 128) for _ in range(2)]
    yoff = [A.f32(128, 512) for _ in range(2)]
    xw = [A.bf16(128, 512) for _ in range(2)]
    ztB = [A.bf16(128, 512) for _ in range(2)]
    yg = [A.f32(128, 512) for _ in range(2)]
    ygsq = A.f32(128, 512)
    yn = [A.bf16(128, 512) for _ in range(2)]
    ssq = [A.f32(128, 1) for _ in range(2)]
    srt = [A.f32(128, 1) for _ in range(2)]
    srs = [A.f32(128, 1) for _ in range(2)]
    yTsb = A.bf16(128, 4, T)
    stout = [A.f32(128, 4, 128) for _ in range(2)]
    h0t = [A.f32(128, 4, 128) for _ in range(2)]
    xs_tm_v = xs_tm_s.rearrange("(i p) c -> p i c", p=128)
    b_tm_v = b_tm_s.rearrange("(i p) c -> p i c", p=128)

    def loadB(g):
        s = g % 2
        DMA("sp", xtm[s], xs_tm_v[:, :, g * 512:(g + 1) * 512], "sB_x%d" % s, w=["xtm%d" % s])
        DMA("sp", btm[s], b_tm_v[:, :, g * 128:(g + 1) * 128], "sB_b%d" % s, w=["btm%d" % s])
        DMA("sp", BTt[s], bct_s[g * 128:(g + 1) * 128, :], "sB_BT%d" % s, w=["BT%d" % s])
        DMA("sp", CTt[s], bct_s[1024 + g * 128:1024 + (g + 1) * 128, :], "sB_CT%d" % s, w=["CT%d" % s])
        DMA("sp", normw[s], ssm_norm_w_d[g * 512:(g + 1) * 512].partition_broadcast(128), "sB_nw%d" % s, w=["normw%d" % s])

    loadB(0)
    it = [0]
    for g in range(8):
        gs = g % 2
        if g + 1 < 8:
            loadB(g + 1)
        X = xtm[gs]
        xr = "xtm%d" % gs
        for c in range(NT):
            TT("pool", yacc[:, c, :].rearrange("p (h q) -> p h q", q=64), X[:, c, :].rearrange("p (h q) -> p h q", q=64),
               D_bc[:, g * 8:(g + 1) * 8].unsqueeze(2).to_broadcast([128, 8, 64]), ALU.mult, r=[xr, "D_bc"], w=["yacc%d" % c])
        iters = [(d, c) for d in range(2) for c in (range(NT) if d == 0 else range(NT - 1, -1, -1))]

        def front(idx):
            d, c = iters[idx]
            k2 = idx % 2
            U = uf if d == 0 else ub
            ures = "uf" if d == 0 else "ub"
            col0 = d * 64 + g * 8
            csl = slice(c * 128, (c + 1) * 128)
            MM(ps[0][:, 0:128], BTt[gs][:, csl], CTt[gs][:, csl], r=["BT%d" % gs, "CT%d" % gs], w=["ps0"])
            TT("dve", cbm[k2], ps[0][:, 0:128], U, ALU.mult, r=["ps0", ures], w=["cbm%d" % k2])
            for half in range(2):
                for jj in range(4):
                    j = half * 4 + jj
                    MM(ps[1 + half][:, jj * 128:(jj + 1) * 128], q_a[:, c, col0 + j:col0 + j + 1].to_broadcast([128, 128]), U,
                       r=["dq", ures], w=["ps%d" % (1 + half)])
                for jj in range(4):
                    j = half * 4 + jj
                    ACT(Lsb[k2][:, j, :], ps[1 + half][:, jj * 128:(jj + 1) * 128], AF.Exp,
                        r=["ps%d" % (1 + half), "dq"], w=["L%d_%d" % (k2, half)], bias=q_bexp[:, c, col0 + j:col0 + j + 1])
                STT("dve", mT[k2][:, half * 4:(half + 1) * 4, :], Lsb[k2][:, half * 4:(half + 1) * 4, :], 1e30,
                    cbm[k2].unsqueeze(1).to_broadcast([128, 4, 128]), ALU.min, ALU.mult,
                    r=["L%d_%d" % (k2, half), "cbm%d" % k2], w=["mT%d_%d" % (k2, half)])

        def back(idx):
            d, c = iters[idx]
            k2 = idx % 2
            col0 = d * 64 + g * 8
            first = (c == 0) if d == 0 else (c == NT - 1)
            seg_start = (c % 2 == 0) if d == 0 else (c % 2 == 1)
            seg_end = (c % 2 == 1) if d == 0 else (c % 2 == 0)
            sres = "S32_%d" % d
            bres = "Sbf_%d" % d
            csl = slice(c * 128, (c + 1) * 128)
            if first:
                hs = (g * 2 + d) % 2
                DMA("sp", h0t[hs], h0_d[d, g * 8:(g + 1) * 8].rearrange("(jj h2) q n -> (h2 q) jj n", h2=2),
                    "sB_h0%d" % hs, w=["h0t%d" % hs])
                for jj in range(4):
                    TR(ps[6][:, jj * 128:(jj + 1) * 128], h0t[hs][:, jj, :], ident, r=["h0t%d" % hs, "ident"], w=["ps6"])
                CP("dve", S32[d], ps[6][:, :], r=["ps6"], w=[sres])
                CP("act", Sbf[d], ps[6][:, :], r=["ps6"], w=[bres])
            elif seg_start:
                TS("dve", S32[d], S32[d], carry[:, 0:1], ALU.mult, r=[sres, "carry"], w=[sres])
                CP("act", Sbf[d], S32[d], r=[sres], w=[bres])
            MM(ps[4][:, :], CTt[gs][:, csl], Sbf[d], r=["CT%d" % gs, bres], w=["ps4"])
            for j in range(8):
                MM(ps[3][:, j * 64:(j + 1) * 64], mT[k2][:, j, :], X[:, c, j * 64:(j + 1) * 64],
                   r=["mT%d_%d" % (k2, j // 4), xr], w=["ps3"])
            TT("pool", xw[k2].rearrange("p (h q) -> p h q", q=64), X[:, c, :].rearrange("p (h q) -> p h q", q=64),
               q_wdec[:, c, col0:col0 + 8].unsqueeze(2).to_broadcast([128, 8, 64]), ALU.mult, r=[xr, "dq"], w=["xw%d" % k2])
            MM(ps[5][:, :], btm[gs][:, c, :], xw[k2], r=["btm%d" % gs, "xw%d" % k2], w=["ps5"])
            TT("dve", yoff[k2].rearrange("p (h q) -> p h q", q=64), ps[4][:, :].rearrange("p (h q) -> p h q", q=64),
               q_eac[:, c, col0:col0 + 8].unsqueeze(2).to_broadcast([128, 8, 64]), ALU.mult, r=["ps4", "dq"], w=["yoff%d" % k2])
            TT("pool", S32[d].rearrange("p (h q) -> p h q", q=64), S32[d].rearrange("p (h q) -> p h q", q=64),
               q_cdec[:, c, col0:col0 + 8].unsqueeze(2).to_broadcast([128, 8, 64]), ALU.mult, r=[sres, "dq"], w=[sres])
            TT("dve", S32[d], S32[d], ps[5][:, :], ALU.add, r=[sres, "ps5"], w=[sres])
            CP("act", Sbf[d], S32[d], r=[sres], w=[bres])
            TT("dve", yoff[k2], yoff[k2], ps[3][:, :], ALU.add, r=["yoff%d" % k2, "ps3"], w=["yoff%d" % k2])
            TT("pool", yacc[:, c, :], yacc[:, c, :], yoff[k2], ALU.add, r=["yacc%d" % c, "yoff%d" % k2], w=["yacc%d" % c])
            if seg_end:
                so = (c // 2 + d) % 2
                for jj in range(4):
                    TR(ps[6][:, jj * 128:(jj + 1) * 128], S32[d][:, jj * 128:(jj + 1) * 128], ident, r=[sres, "ident"], w=["ps6"])
                CP("act", stout[so], ps[6][:, :].rearrange("p (a b) -> p a b", b=128), r=["ps6"], w=["stout%d" % so])
                DMA("sp", st_out_d[c // 2, d, g * 8:(g + 1) * 8].rearrange("(jj h2) q n -> (h2 q) jj n", h2=2), stout[so],
                    "out_st%d" % so, r=["stout%d" % so])

        front(0)
        for idx in range(len(iters)):
            if idx + 1 < len(iters):
                front(idx + 1)
            back(idx)
        for c in range(NT):
            k2 = c % 2
            DMA("sp", ztB[k2], zs_s[c * 128:(c + 1) * 128, g * 512:(g + 1) * 512], "sB_zt%d" % k2, w=["ztB%d" % k2])
            TT("dve", yg[k2], yacc[:, c, :], ztB[k2], ALU.mult, r=["yacc%d" % c, "ztB%d" % k2], w=["yg%d" % k2])
            ACT(ygsq, yg[k2], AF.Square, r=["yg%d" % k2], w=["ygsq", "ssq%d" % k2], accum=ssq[k2])
            ACT(srt[k2], ssq[k2], AF.Sqrt, r=["ssq%d" % k2], w=["srt%d" % k2], scale=1.0 / 512.0, bias=1e-6)
            RECIP(srs[k2], srt[k2], r=["srt%d" % k2], w=["srs%d" % k2])
            STT("dve", yn[k2], yg[k2], srs[k2][:, 0:1], normw[gs], ALU.mult, ALU.mult, r=["yg%d" % k2, "srs%d" % k2, "normw%d" % gs], w=["yn%d" % k2])
            for cc in range(4):
                TR(psb[7][:, cc * 128:(cc + 1) * 128], yn[k2][:, cc * 128:(cc + 1) * 128], identb, r=["yn%d" % k2, "identb"], w=["ps7"])
            CP("act", yTsb[:, :, c * 128:(c + 1) * 128], psb[7][:, 0:512].rearrange("p (a b) -> p a b", b=128), r=["ps7"], w=["yTsb"])
        DMA("sp", yT_s.rearrange("(cc p) t -> p cc t", p=128)[:, g * 4:(g + 1) * 4, :], yTsb, "sB_yT", r=["yTsb"])
    BARRIER()
    if STOP_AFTER == 'B':
        return done()

    A = StageArena()
    qnT = A.bf16(128, 4, T)
    ckvT = A.bf16(128, 4, NKEY)
    kra = A.bf16(73, NKEY)
    wq = A.bf16(128, 4, 3072)
    wkv = A.bf16(128, 4, 4096)
    cos2 = A.f32(64, T)
    sin2 = A.f32(64, T)
    cxl = [A.f32(128, 512) for _ in range(2)]
    cxk = A.f32(128, 4, 64)
    wqs = [A.bf16(128, 4, 64) for _ in range(2)]
    qn_h = [A.bf16(128, T) for _ in range(2)]
    qra = [A.bf16(73, T) for _ in range(2)]
    kn_h = [A.bf16(128, NKEY) for _ in range(2)]
    v_h = [A.bf16(128, 20, 128) for _ in range(2)]
    PT = [A.bf16(128, 512) for _ in range(4)]
    rden = A.f32(128, 512)
    oTh = [A.bf16(128, T) for _ in range(2)]
    rp1 = A.f32(64, 512)
    rp2 = A.f32(64, 512)
    DMA("sp", qnT, qnT_s.rearrange("(c p) t -> p c t", p=128), "sC_qnT", w=["qnT"])
    DMA("sp", ckvT[:, :, 0:T], ckvT_s.rearrange("(c p) t -> p c t", p=128), "sC_ckvT", w=["ckvT_own"])
    DMA("sp", kra[0:64, 0:T], krT_s, "sC_kr", w=["kra_own"])
    DMA("pool", kra[64:73, :], khot_d, "sC_khot", w=["kra_hot"])
    for s in range(2):
        DMA("pool", qra[s][64:73, :], qpen_d, "sC_qpen%d" % s, w=["qra_pen%d" % s])
    DMA("pool", wq[:, :, 0:1536], w_q_b_d.rearrange("(k p) n -> p k n", p=128)[:, :, 0:1536], "sC_wq0", w=["wq0"])
    DMA("pool", wq[:, :, 1536:3072], w_q_b_d.rearrange("(k p) n -> p k n", p=128)[:, :, 1536:3072], "sC_wq1", w=["wq1"])
    DMA("pool", wkv[:, :, 0:2048], w_kv_b_d.rearrange("(k p) n -> p k n", p=128)[:, :, 0:2048], "sC_wkv0", w=["wkv0"])
    DMA("pool", wkv[:, :, 2048:4096], w_kv_b_d.rearrange("(k p) n -> p k n", p=128)[:, :, 2048:4096], "sC_wkv1", w=["wkv1"])
    DMA("sp", cos2, cos2_d, "sC_cos", w=["cos2"])
    DMA("sp", sin2, sin2_d, "sC_sin", w=["sin2"])
    for kt in range(4):
        s = kt % 2
        DMA("sp", cxl[s], ctx_ckv_d[kt * 128:(kt + 1) * 128, :], "sC_cx%d" % s, w=["cxl%d" % s])
        for c in range(4):
            TR(ps[4][:, c * 128:(c + 1) * 128], cxl[s][:, c * 128:(c + 1) * 128], ident, r=["cxl%d" % s, "ident"], w=["ps4"])
        CP("dve", ckvT[:, :, T + kt * 128:T + (kt + 1) * 128], ps[4][:, :].rearrange("p (a b) -> p a b", b=128), r=["ps4"], w=["ckvT_ctx"])
    DMA("sp", cxk, ctx_kr_d.rearrange("(a p) c -> p a c", p=128), "sC_cxk", w=["cxk"])
    for kt in range(4):
        TR(ps[5][0:64, kt * 128:(kt + 1) * 128], cxk[:, kt, :], ident, r=["cxk", "ident"], w=["ps5"])
    CP("dve", kra[0:64, T:NKEY], ps[5][0:64, :], r=["ps5"], w=["kra_ctx"])
    ckr = ["ckvT_own", "ckvT_ctx"]
    krr = ["kra_own", "kra_hot", "kra_ctx"]
    for h in range(16):
        hs = h % 2
        wqr = "wq%d" % (h // 8)
        wkr_ = "wkv%d" % (h // 8)
        qc0 = h * 192
        kc0 = h * 256
        CP("pool", wqs[hs][:, :, 0:32], wq[:, :, qc0 + 160:qc0 + 192], r=[wqr], w=["wqs%d" % hs])
        CP("pool", wqs[hs][:, :, 32:64], wq[:, :, qc0 + 128:qc0 + 160], r=[wqr], w=["wqs%d" % hs])
        for tb in range(4):
            tsl = slice(tb * 512, (tb + 1) * 512)
            for k in range(4):
                MM(ps[4][:, :], wq[:, k, qc0:qc0 + 128], qnT[:, k, tsl], start=(k == 0), stop=(k == 3), r=[wqr, "qnT"], w=["ps4"])
            CP("act", qn_h[hs][:, tsl], ps[4][:, :], r=["ps4"], w=["qn_h%d" % hs])
            for k in range(4):
                MM(ps[5][0:64, :], wq[:, k, qc0 + 128:qc0 + 192], qnT[:, k, tsl], start=(k == 0), stop=(k == 3), r=[wqr, "qnT"], w=["ps5"])
            for k in range(4):
                MM(ps[6][0:64, :], wqs[hs][:, k, :], qnT[:, k, tsl], start=(k == 0), stop=(k == 3), r=["wqs%d" % hs, "qnT"], w=["ps6"])
            TT("dve", rp1, ps[5][0:64, :], cos2[:, tsl], ALU.mult, r=["ps5", "cos2"], w=["rp1"])
            TT("dve", rp2, ps[6][0:64, :], sin2[:, tsl], ALU.mult, r=["ps6", "sin2"], w=["rp2"])
            TT("dve", qra[hs][0:64, tsl], rp1, rp2, ALU.add, r=["rp1", "rp2"], w=["qra%d" % hs])
        for kb in range(5):
            ksl = slice(kb * 512, (kb + 1) * 512)
            for k in range(4):
                MM(ps[7][:, :], wkv[:, k, kc0:kc0 + 128], ckvT[:, k, ksl], start=(k == 0), stop=(k == 3), r=[wkr_] + ckr, w=["ps7"])
            CP("act", kn_h[hs][:, ksl], ps[7][:, :], r=["ps7"], w=["kn_h%d" % hs])
        for kq in range(5):
            for kk in range(4):
                kt = kq * 4 + kk
                for k in range(4):
                    MM(ps[4][:, kk * 128:(kk + 1) * 128], ckvT[:, k, kt * 128:(kt + 1) * 128], wkv[:, k, kc0 + 128:kc0 + 256],
                       start=(k == 0), stop=(k == 3), r=[wkr_] + ckr, w=["ps4"])
            CP("dve", v_h[hs][:, kq * 4:(kq + 1) * 4, :], ps[4][:, :].rearrange("p (a b) -> p a b", b=128), r=["ps4"], w=["v_h%d" % hs])
        pti = 0
        for qb in range(4):
            qsl = slice(qb * 512, (qb + 1) * 512)
            prev = None
            for kt in range(21):
                if kt < 20:
                    sb = kt % 2
                    p_ = pti % 4
                    pti += 1
                    MM(ps[sb][:, :], kn_h[hs][:, kt * 128:(kt + 1) * 128], qn_h[hs][:, qsl], start=True, stop=False,
                       r=["kn_h%d" % hs, "qn_h%d" % hs], w=["ps%d" % sb])
                    MM(ps[sb][:, :], kra[0:73, kt * 128:(kt + 1) * 128], qra[hs][0:73, qsl], start=False, stop=True,
                       r=krr + ["qra%d" % hs, "qra_pen%d" % hs], w=["ps%d" % sb])
                    ACT(PT[p_], ps[sb][:, :], AF.Exp, r=["ps%d" % sb], w=["PT%d" % p_], scale=SCALE)
                if prev is not None:
                    pk, pp = prev
                    MM(ps[2][:, :], v_h[hs][:, pk, :], PT[pp], start=(pk == 0), stop=(pk == 19), r=["v_h%d" % hs, "PT%d" % pp], w=["ps2"])
                    MM(ps[3][:, :], onesb, PT[pp], start=(pk == 0), stop=(pk == 19), r=["onesb", "PT%d" % pp], w=["ps3"])
                prev = (kt, p_) if kt < 20 else None
            RECIP(rden, ps[3][:, :], r=["ps3"], w=["rden"])
            TT("dve", oTh[hs][:, qsl], ps[2][:, :], rden, ALU.mult, r=["ps2", "rden"], w=["oTh%d" % hs])
        DMA("sp", oT_s[h * 128:(h + 1) * 128, :], oTh[hs], "sC_oT%d" % hs, r=["oTh%d" % hs])
    BARRIER()
    if STOP_AFTER == 'C':
        return done()

    A = StageArena()
    g1bc = A.f32(128, D)
    lnw = A.f32(128, D)
    lnb = A.f32(128, D)
    DMA("sp", g1bc, g1_s.partition_broadcast(128), "sD_g1", w=["g1bc"])
    DMA("sp", lnw, ln1_w_d.partition_broadcast(128), "sD_lnw", w=["lnw"])
    DMA("sp", lnb, ln1_b_d.partition_broadcast(128), "sD_lnb", w=["lnb"])
    yTb = A.bf16(128, 32, 512)
    oTb = A.bf16(128, 16, 512)
    gsl = [A.bf16(128, 2, 2, 512) for _ in range(2)]
    mrg = A.bf16(128, 16, 512)
    NSD = 3
    wD = [A.bf16(128, 32, 256) for _ in range(NSD)]
    t1 = [A.f32(128, 512) for _ in range(2)]
    t2 = [A.f32(128, 512) for _ in range(2)]
    vt = A.f32(128, 4, D)
    xin = [A.f32(128, D)]
    junk = xin[0]
    st1 = [A.f32(128, 4) for _ in range(2)]
    w_ssm_v = w_ssm_out_d.rearrange("(k p) n -> p k n", p=128)
    w_mla_v = w_mla_out_d.rearrange("(k p) n -> p k n", p=128)
    w_o_v = w_o_d.rearrange("(k p) n -> p k n", p=128)
    dgroups = []
    for tb in range(4):
        for m2 in range(8):
            dgroups.append(("ssm", m2, tb))
            dgroups.append(("mla", m2, tb))
        for fb in range(4):
            dgroups.append(("wo", fb, tb))

    def issue_d(n):
        if n < len(dgroups):
            s = n % NSD
            kind, m, _ = dgroups[n]
            if kind == "ssm":
                DMA("pool", wD[s], w_ssm_v[:, :, m * 256:(m + 1) * 256], "sD_w%d" % s, w=["wD%d" % s])
            elif kind == "mla":
                DMA("pool", wD[s][:, 0:16, :], w_mla_v[:, :, m * 256:(m + 1) * 256], "sD_w%d" % s, w=["wD%d" % s])
            else:
                DMA("pool", wD[s].rearrange("p a b -> p (a b)").rearrange("p (a b) -> p a b", b=512), w_o_v[:, :, m * 512:(m + 1) * 512],
                    "sD_w%d" % s, w=["wD%d" % s])

    for n in range(NSD - 1):
        issue_d(n)
    tctr = 0
    for n, (kind, m, tb) in enumerate(dgroups):
        issue_d(n + NSD - 1)
        s = n % NSD
        wres = "wD%d" % s
        tsl = slice(tb * 512, (tb + 1) * 512)
        if kind == "ssm" and m == 0:
            DMA("sp", yTb, yT_s.rearrange("(c p) t -> p c t", p=128)[:, :, tsl], "sD_yT", w=["yTb"])
            DMA("sp", oTb, oT_s.rearrange("(c p) t -> p c t", p=128)[:, :, tsl], "sD_oT", w=["oTb"])
        if kind == "ssm":
            gq = m % 2
            gT_v = gT_s.rearrange("(c p) t -> p c t", p=128)
            DMA("sp", gsl[gq][:, 0, :, :], gT_v[:, m * 2:m * 2 + 2, tsl], "sD_gs%d" % gq, w=["gsl%d" % gq])
            DMA("sp", gsl[gq][:, 1, :, :], gT_v[:, 16 + m * 2:16 + m * 2 + 2, tsl], "sD_gm%d" % gq, w=["gsl%d" % gq])
            for mm in range(2):
                mc = m * 2 + mm
                b = mm
                for k in range(32):
                    MM(ps[b][:, :], wD[s][:, k, mm * 128:(mm + 1) * 128], yTb[:, k, :], start=(k == 0), stop=(k == 31),
                       r=[wres, "yTb"], w=["ps%d" % b])
                TT("dve", t1[mm], ps[b][:, :], gsl[gq][:, 0, mm, :], ALU.mult, r=["ps%d" % b, "gsl%d" % gq], w=["t1_%d" % mm])
        elif kind == "mla":
            gq = m % 2
            for mm in range(2):
                mc = m * 2 + mm
                b = 2 + mm
                for k in range(16):
                    MM(ps[b][:, :], wD[s][:, k, mm * 128:(mm + 1) * 128], oTb[:, k, :], start=(k == 0), stop=(k == 15),
                       r=[wres, "oTb"], w=["ps%d" % b])
                TT("dve", t2[mm], ps[b][:, :], gsl[gq][:, 1, mm, :], ALU.mult, r=["ps%d" % b, "gsl%d" % gq], w=["t2_%d" % mm])
                TT("pool", mrg[:, mc, :], t1[mm], t2[mm], ALU.add, r=["t1_%d" % mm, "t2_%d" % mm], w=["mrg"])
        else:
            fb = m
            fsl = slice(fb * 512, (fb + 1) * 512)
            wv = wD[s].rearrange("p a b -> p (a b)").rearrange("p (a b) -> p a b", b=512)
            for i4 in range(4):
                i = tb * 4 + i4
                b = 4 + (i4 % 2)
                if fb == 0:
                    DMA("sp", xin[0], x_d[i * 128:(i + 1) * 128, :], "sD_x0", w=["xin0"])
                for k in range(16):
                    MM(ps[b][:, :], mrg[:, k, i4 * 128:(i4 + 1) * 128], wv[:, k, :], start=(k == 0), stop=(k == 15),
                       r=["mrg", wres], w=["ps%d" % b])
                tq = tctr % 2
                tctr += 1
                TT("dve", t1[tq], ps[b][:, :], g1bc[:, fsl], ALU.mult, r=["ps%d" % b, "g1bc"], w=["t1_%d" % tq])
                if fb == 0:
                    TS("pool", vt[:, i4, :], xin[0], ALU_ALPHA, ALU.mult, r=["xin0"], w=["vt%d" % i4])
                TT("pool", vt[:, i4, fsl], vt[:, i4, fsl], t1[tq], ALU.add, r=["vt%d" % i4, "t1_%d" % tq], w=["vt%d" % i4])
            if fb == 3:
                for i4 in range(4):
                    i = tb * 4 + i4
                    q2 = i4 % 2
                    V = vt[:, i4, :]
                    st = st1[q2]
                    TS("dve", junk, V, 1.0, ALU.mult, 0.0, ALU.add, r=["vt%d" % i4], w=["xin0", "st%d" % q2], accum=st[:, 0:1])
                    TS("dve", st[:, 1:2], st[:, 0:1], -1.0 / D, ALU.mult, r=["st%d" % q2], w=["st%d" % q2])
                    ACT(junk, V, AF.Square, r=["vt%d" % i4, "st%d" % q2], w=["xin0", "st%d" % q2], bias=st[:, 1:2], accum=st[:, 2:3])
                    ACT(st[:, 3:4], st[:, 2:3], AF.Sqrt, r=["st%d" % q2], w=["st%d" % q2], scale=1.0 / D, bias=1e-5)
                    RECIP(st[:, 3:4], st[:, 3:4], r=["st%d" % q2], w=["st%d" % q2])
                    TS("dve", V, V, st[:, 1:2], ALU.add, st[:, 3:4], ALU.mult, r=["vt%d" % i4, "st%d" % q2], w=["vt%d" % i4])
                    TT("dve", V, V, lnw, ALU.mult, r=["vt%d" % i4, "lnw"], w=["vt%d" % i4])
                    TT("pool", V, V, lnb, ALU.add, r=["vt%d" % i4, "lnb"], w=["vt%d" % i4])
                    DMA("sp", x1_s[i * 128:(i + 1) * 128, :], V, "sD_x1_%d" % i4, r=["vt%d" % i4])
    BARRIER()
    if STOP_AFTER == 'D':
        return done()

    A = StageArena()
    h2T = A.bf16(128, 16, 1024)
    accT = A.f32(128, 16, 1024)
    bg = A.f32(128, 32, 16, 2)
    bd = A.f32(128, 32, 16)
    comb = A.f32(128, 8, 32)
    rw = A.f32(128, 16, 32)
    rb_bc = A.f32(128, 32)
    NSE = 3
    wE = [A.bf16(128, 16, 256) for _ in range(NSE)]
    lg = A.f32(128, 32)
    m8 = A.f32(128, 8)
    nmx = A.f32(128, 1)
    msk = A.f32(128, 32)
    ex = A.f32(128, 32)
    ssum = A.f32(128, 1)
    mark_e = A.off
    h32 = A.f32(128, 16, 128)
    x1l = [A.f32(128, D) for _ in range(2)]
    A.off = mark_e
    actT = A.bf16(128, 16, 1024)
    combs = A.f32(128, 1024)
    gsb = [A.f32(128, 512) for _ in range(2)]
    sgb = [A.f32(128, 512) for _ in range(2)]
    usb = [A.f32(128, 512) for _ in range(2)]
    tcb = [A.f32(128, 512) for _ in range(2)]
    A.off = mark_e
    lnw2 = A.f32(128, D)
    lnb2 = A.f32(128, D)
    v2 = [A.f32(128, D) for _ in range(2)]
    x1e = [A.f32(128, D)]
    junk2 = A.f32(128, D)
    st2 = [A.f32(128, 4) for _ in range(2)]
    for e4 in range(4):
        DMA("sp", bg[:, e4 * 8:(e4 + 1) * 8, :, :], b_gu_d[e4 * 8:(e4 + 1) * 8, :].rearrange("e (c p two) -> p e c two", p=128, two=2),
            "sE_bg%d" % e4, w=["bg"], slow=True)
        DMA("sp", bd[:, e4 * 8:(e4 + 1) * 8, :], b_down_d[e4 * 8:(e4 + 1) * 8, :].rearrange("e (c p) -> p e c", p=128),
            "sE_bd%d" % e4, w=["bd"], slow=True)
    DMA("sp", rw, router_w_d.rearrange("(k p) e -> p k e", p=128), "sE_rw", w=["rw"])
    DMA("sp", rb_bc, router_b_d.partition_broadcast(128), "sE_rb", w=["rb"])
    w_gu_v = w_gu_d.rearrange("e (k p) n -> e p k n", p=128)
    w_dn_v = w_down_d.rearrange("e (k p) n -> e p k n", p=128)
    egroups = []
    for blk in range(2):
        for e in range(32):
            for m in range(16):
                egroups.append(("gu", blk, e, m))
            for j2 in range(8):
                egroups.append(("dn", blk, e, j2))

    def issue_e(n):
        if n < len(egroups):
            s = n % NSE
            kind, _, e, m = egroups[n]
            if kind == "gu":
                DMA("pool", wE[s], w_gu_v[e][:, :, m * 256:(m + 1) * 256], "sE_w%d" % s, w=["wE%d" % s])
            else:
                DMA("pool", wE[s], w_dn_v[e][:, :, m * 256:(m + 1) * 256], "sE_w%d" % s, w=["wE%d" % s])

    n = 0
    for blk in range(2):
        for i8 in range(8):
            i = blk * 8 + i8
            xs_ = i % 2
            DMA("sp", x1l[xs_], x1_s[i * 128:(i + 1) * 128, :], "sE_x1_%d" % xs_, w=["x1l%d" % xs_])
            for cq in range(4):
                b = 4 + cq % 2
                for cc in range(4):
                    c = cq * 4 + cc
                    TR(ps[b][:, cc * 128:(cc + 1) * 128], x1l[xs_][:, c * 128:(c + 1) * 128], ident, r=["x1l%d" % xs_, "ident"], w=["ps%d" % b])
                for cc in range(4):
                    c = cq * 4 + cc
                    if cc % 2 == 0:
                        ACT(h32[:, c, :], ps[b][:, cc * 128:(cc + 1) * 128], AF.Identity, r=["ps%d" % b, "sc2p", "mod"], w=["h32_%d" % c],
                            scale=sc2p[:, c:c + 1], bias=sh2[:, c:c + 1])
                    else:
                        TS("dve", h32[:, c, :], ps[b][:, cc * 128:(cc + 1) * 128], sc2p[:, c:c + 1], ALU.mult, sh2[:, c:c + 1], ALU.add,
                           r=["ps%d" % b, "sc2p", "mod"], w=["h32_%d" % c])
            CP("pool", h2T[:, :, i8 * 128:(i8 + 1) * 128], h32, r=["h32_%d" % c for c in range(16)], w=["h2T"])
            for k in range(16):
                MM(ps[6][:, 0:32], h32[:, k, :], rw[:, k, :], start=(k == 0), stop=(k == 15), r=["h32_%d" % k, "rw"], w=["ps6"])
            TT("dve", lg, ps[6][:, 0:32], rb_bc, ALU.add, r=["ps6", "rb"], w=["lg"])
            S.add("dve", lambda e: e.max(out=m8, in_=lg), ["lg"], ["m8"])
            TS("dve", msk, lg, m8[:, 3:4], ALU.is_ge, r=["lg", "m8"], w=["msk"])
            TS("dve", nmx, m8[:, 0:1], -1.0, ALU.mult, r=["m8"], w=["nmx"])
            ACT(ex, lg, AF.Exp, r=["lg", "nmx"], w=["ex"], bias=nmx[:, 0:1])
            STT("dve", ex, ex, 1.0, msk, ALU.mult, ALU.mult, r=["ex", "msk"], w=["ex", "ssum"], accum=ssum[:, 0:1])
            RECIP(ssum, ssum, r=["ssum"], w=["ssum"])
            TS("dve", comb[:, i8, :], ex, ssum[:, 0:1], ALU.mult, r=["ex", "ssum"], w=["comb"])
        BARRIER()
        if blk == 0:
            for q in range(NSE - 1):
                issue_e(q)
        for e in range(32):
            for i8 in range(8):
                b = 6 + (i8 // 4)
                MM(ps[b][:, (i8 % 4) * 128:(i8 % 4 + 1) * 128], comb[:, i8, e:e + 1].to_broadcast([128, 128]), ident,
                   r=["comb", "ident"], w=["ps%d" % b])
            CP("act", combs[:, 0:512], ps[6][:, :], r=["ps6"], w=["combs"])
            CP("act", combs[:, 512:1024], ps[7][:, :], r=["ps7"], w=["combs"])
            for m in range(16):
                issue_e(n + NSE - 1)
                s = n % NSE
                n += 1
                wres = "wE%d" % s
                for t2_ in range(2):
                    tsl = slice(t2_ * 512, (t2_ + 1) * 512)
                    q2 = (m * 2 + t2_) % 2
                    for k in range(16):
                        MM(ps[0 + t2_][:, :], wE[s][:, k, 0:256:2], h2T[:, k, tsl], start=(k == 0), stop=(k == 15), r=[wres, "h2T"], w=["ps%d" % t2_])
                    for k in range(16):
                        MM(ps[2 + t2_][:, :], wE[s][:, k, 1:256:2], h2T[:, k, tsl], start=(k == 0), stop=(k == 15), r=[wres, "h2T"], w=["ps%d" % (2 + t2_)])
                    TS("dve", gsb[q2], ps[0 + t2_][:, :], bg[:, e, m, 0:1], ALU.add, 7.0, ALU.min, r=["ps%d" % t2_, "bg"], w=["gsb%d" % q2])
                    ACT(sgb[q2], gsb[q2], AF.Sigmoid, r=["gsb%d" % q2], w=["sgb%d" % q2], scale=1.702)
                    TS("dve", usb[q2], ps[2 + t2_][:, :], bg[:, e, m, 1:2], ALU.add, 7.0, ALU.min, r=["ps%d" % (2 + t2_), "bg"], w=["usb%d" % q2])
                    TS("dve", usb[q2], usb[q2], -7.0, ALU.max, 1.0, ALU.add, r=["usb%d" % q2], w=["usb%d" % q2])
                    TT("dve", gsb[q2], gsb[q2], sgb[q2], ALU.mult, r=["gsb%d" % q2, "sgb%d" % q2], w=["gsb%d" % q2])
                    TT("dve", actT[:, m, tsl], gsb[q2], usb[q2], ALU.mult, r=["gsb%d" % q2, "usb%d" % q2], w=["actT%d" % m])
            ar = ["actT%d" % m for m in range(16)]
            for j2 in range(8):
                issue_e(n + NSE - 1)
                s = n % NSE
                n += 1
                wres = "wE%d" % s
                for jj in range(2):
                    j = j2 * 2 + jj
                    for t2_ in range(2):
                        tsl = slice(t2_ * 512, (t2_ + 1) * 512)
                        b = 4 + (jj * 2 + t2_) % 2
                        q2 = (jj * 2 + t2_) % 2
                        for k in range(16):
                            MM(ps[b][:, :], wE[s][:, k, jj * 128:(jj + 1) * 128], actT[:, k, tsl], start=(k == 0), stop=(k == 15),
                               r=[wres] + ar, w=["ps%d" % b])
                        if e == 0:
                            STT("dve", accT[:, j, tsl], ps[b][:, :], bd[:, e, j:j + 1], combs[:, tsl], ALU.add, ALU.mult,
                                r=["ps%d" % b, "bd", "combs"], w=["accT%d" % j])
                        else:
                            STT("dve", tcb[q2], ps[b][:, :], bd[:, e, j:j + 1], combs[:, tsl], ALU.add, ALU.mult,
                                r=["ps%d" % b, "bd", "combs"], w=["tcb%d" % q2])
                            TT("pool", accT[:, j, tsl], accT[:, j, tsl], tcb[q2], ALU.add, r=["accT%d" % j, "tcb%d" % q2], w=["accT%d" % j])
        accr = ["accT%d" % j for j in range(16)]
        for j in range(16):
            TS("dve", accT[:, j, :], accT[:, j, :], g2c[:, j:j + 1], ALU.mult, r=["accT%d" % j, "mod"], w=["accT%d" % j])
        BARRIER()
        alias_r = []
        DMA("sp", lnw2, ln2_w_d.partition_broadcast(128), "sE_lnw", r=[], w=["lnw2"])
        DMA("sp", lnb2, ln2_b_d.partition_broadcast(128), "sE_lnb", r=[], w=["lnb2"])
        for i8 in range(8):
            i = blk * 8 + i8
            q2 = i8 % 2
            DMA("sp", x1e[0], x1_s[i * 128:(i + 1) * 128, :], "sE_x1e0", w=["x1e0"])
            for cq in range(4):
                b = cq % 2
                for cc in range(4):
                    c = cq * 4 + cc
                    TR(ps[b][:, cc * 128:(cc + 1) * 128], accT[:, c, i8 * 128:(i8 + 1) * 128], ident, r=["accT%d" % c, "ident"], w=["ps%d" % b])
                STT("dve", v2[q2][:, cq * 512:(cq + 1) * 512], x1e[0][:, cq * 512:(cq + 1) * 512], ALU_ALPHA, ps[b][:, :], ALU.mult, ALU.add,
                    r=["x1e0", "ps%d" % b], w=["v2_%d" % q2])
            V = v2[q2]
            st = st2[q2]
            vr = "v2_%d" % q2
            sr = "st2_%d" % q2
            TS("dve", junk2, V, 1.0, ALU.mult, 0.0, ALU.add, r=[vr], w=["junk2", sr], accum=st[:, 0:1])
            TS("dve", st[:, 1:2], st[:, 0:1], -1.0 / D, ALU.mult, r=[sr], w=[sr])
            ACT(junk2, V, AF.Square, r=[vr, sr], w=["junk2", sr], bias=st[:, 1:2], accum=st[:, 2:3])
            ACT(st[:, 3:4], st[:, 2:3], AF.Sqrt, r=[sr], w=[sr], scale=1.0 / D, bias=1e-5)
            RECIP(st[:, 3:4], st[:, 3:4], r=[sr], w=[sr])
            TS("dve", V, V, st[:, 1:2], ALU.add, st[:, 3:4], ALU.mult, r=[vr, sr], w=[vr])
            TT("dve", V, V, lnw2, ALU.mult, r=[vr, "lnw2"], w=[vr])
            TT("pool", V, V, lnb2, ALU.add, r=[vr, "lnb2"], w=[vr])
            DMA("sp", y_d[i * 128:(i + 1) * 128, :], V, "out_y%d" % q2, r=[vr])
        BARRIER()

    return done()


ALU_ALPHA = ALPHA

_CACHE = {}


def _rope_tables():
    n_tok = 2048
    rows = n_tok // 64
    row = np.repeat(np.arange(rows, dtype=np.float32), 64)
    col = np.tile(np.arange(64, dtype=np.float32), rows)
    n_freq = 16
    inv = (np.float32(10000.0) ** (-np.arange(n_freq, dtype=np.float32) / n_freq)).astype(np.float32)
    ang = np.concatenate([row[:, None] * inv, col[:, None] * inv], -1).astype(np.float32)
    cos = np.cos(ang).astype(np.float32)
    sin = np.sin(ang).astype(np.float32)
    cos2 = np.concatenate([cos, cos], 1).T.copy()
    sin2 = np.concatenate([-sin, sin], 1).T.copy()
    return cos2, sin2


def kernel(x_prompt, x_sample, cache_mla_ckv, cache_mla_krope, state_ssm, c, c_ctx,
           w_ada, b_ada, w_in, conv_w, conv_b, a_log, dt_bias, d_skip, ssm_norm_w, w_ssm_out,
           q_a_norm_w, w_q_b, kv_a_norm_w, w_kv_b, w_mla_out, w_o, ln1_w, ln1_b,
           router_w, router_b, w_gu, b_gu, w_down, b_down, ln2_w, ln2_b):
    f = lambda a: np.ascontiguousarray(np.asarray(a, dtype=np.float32))
    if "nc" not in _CACHE:
        _CACHE["nc"] = build_program()
    nc, _ = _CACHE["nc"]
    shared = {
        "w_ada": f(w_ada[0]), "b_ada": f(b_ada[0]), "w_in": f(w_in[0]), "conv_w": f(conv_w[0]), "conv_b": f(conv_b[0]),
        "a_log": f(a_log[0]).reshape(128), "dt_bias": f(dt_bias[0]).reshape(128), "d_skip": f(d_skip[0]),
        "ssm_norm_w": f(ssm_norm_w[0]), "w_ssm_out": f(w_ssm_out[0]), "q_a_norm_w": f(q_a_norm_w[0]), "w_q_b": f(w_q_b[0]),
        "kv_a_norm_w": f(kv_a_norm_w[0]), "w_kv_b": f(w_kv_b[0]), "w_mla_out": f(w_mla_out[0]), "w_o": f(w_o[0]),
        "ln1_w": f(ln1_w[0]), "ln1_b": f(ln1_b[0]), "router_w": f(router_w[0]), "router_b": f(router_b[0]),
        "w_gu": f(w_gu[0]) if STOP_AFTER is None else f(w_gu[0][:1]), "b_gu": f(b_gu[0]),
        "w_down": f(w_down[0]) if STOP_AFTER is None else f(w_down[0][:1]), "b_down": f(b_down[0]),
        "ln2_w": f(ln2_w[0]), "ln2_b": f(ln2_b[0]),
        "ident": np.eye(128, dtype=np.float32),
        "uf": np.triu(np.ones((128, 128), np.float32)),
        "ub": np.tril(np.ones((128, 128), np.float32)),
        "ones": np.ones((128, 128), np.float32),
    }
    khot = np.zeros((9, NKEY), np.float32)
    for t in range(T):
        khot[t // 256, t] = 1.0
    khot[8, T:] = 1.0
    shared["khot"] = khot
    cos2, sin2 = _rope_tables()
    pen_prompt = np.full((9, T), -16384.0, np.float32)
    for t in range(T):
        pen_prompt[t // 256, t] = 0.0
    in_maps = []
    for core in range(8):
        m = dict(shared)
        if core < 4:
            b = core
            m["x"] = f(x_sample[b])
            m["cond"] = f(c[b])
            m["ctx_ckv"] = f(cache_mla_ckv[b, 0])
            m["ctx_kr"] = f(cache_mla_krope[b, 0])
            m["h0"] = f(state_ssm[b, 0])
            m["carry"] = np.ones(128, np.float32)
            m["cos2"] = cos2
            m["sin2"] = sin2
            m["qpen"] = np.zeros((9, T), np.float32)
        else:
            s0 = (core - 4) * 8
            m["x"] = f(x_prompt[s0:s0 + 8]).reshape(T, D)
            m["cond"] = f(c_ctx)
            m["ctx_ckv"] = np.zeros((512, 512), np.float32)
            m["ctx_kr"] = np.zeros((512, 64), np.float32)
            m["h0"] = np.zeros((2, 64, 64, 128), np.float32)
            m["carry"] = np.zeros(128, np.float32)
            m["cos2"] = np.ones((64, T), np.float32)
            m["sin2"] = np.zeros((64, T), np.float32)
            m["qpen"] = pen_prompt
        in_maps.append(m)
    if DEBUG_CORES is not None:
        sub = [in_maps[i] for i in DEBUG_CORES]
        res = run_bass_kernel_spmd(nc, sub, core_ids=list(range(len(sub))))
        _CACHE["last"] = {c: res.results[j] for j, c in enumerate(DEBUG_CORES)}
        return None
    res = run_bass_kernel_spmd(nc, in_maps, core_ids=list(range(8)))
    R = res.results
    _CACHE["last"] = R
    y_s = np.stack([R[i]["y"] for i in range(4)], 0)
    y_p = np.concatenate([R[i]["y"].reshape(8, 256, D) for i in range(4, 8)], 0)
    ckv = np.concatenate([R[i]["ckv_out"].reshape(8, 1, 256, 512) for i in range(4, 8)], 0)
    kr = np.concatenate([R[i]["kr_out"].reshape(8, 1, 256, 64) for i in range(4, 8)], 0)
    st = np.concatenate([R[i]["st_out"].reshape(8, 1, 2, 64, 64, 128) for i in range(4, 8)], 0)
    return (y_p.astype(np.float32), y_s.astype(np.float32), ckv.astype(np.float32), kr.astype(np.float32), st.astype(np.float32))
```

```python
import contextlib
import numpy as np
import concourse.bass as bass
import concourse.mybir as mybir
from concourse.bass_utils import run_bass_kernel_spmd

F32 = mybir.dt.float32
BF16 = mybir.dt.bfloat16
AF = mybir.ActivationFunctionType
ALU = mybir.AluOpType

DEBUG = False
STOP_AFTER = None
DEBUG_CORES = None
DEBUG_KEEP = None

T = 2048
D = 2048
NT = 16
KC = 16
ALPHA = 2.0 ** 0.25
SCALE = 192.0 ** -0.5
NKEY = 2560
COMPUTE = ("pe", "act", "dve", "pool")


class Op:
    __slots__ = ("eng", "fn", "deps", "dom", "seq", "waits", "vc", "idx")


class Sched:
    def __init__(self, nc):
        self.nc = nc
        self.ops = []
        self.res_w = {}
        self.res_r = {}
        self.dom_count = {}
        self.dom_ops = {}
        self.bar_idx = None
        self.bar_start = 0
        self.barriers = []

    def add(self, eng, fn, reads=(), writes=(), key=None):
        o = Op()
        o.idx = len(self.ops)
        o.eng = eng
        o.fn = fn
        o.dom = key if key is not None else eng
        assert key is not None or eng in COMPUTE, (eng, key)
        extra = [r for r in reads if r[:2] == "ps" and r[2:].isdigit()]
        if extra:
            writes = list(writes) + extra
        deps = set()
        if self.bar_idx is not None:
            deps.add(self.bar_idx)
        for r in reads:
            w = self.res_w.get(r)
            if w is not None:
                deps.add(w)
        for w_ in writes:
            w = self.res_w.get(w_)
            if w is not None:
                deps.add(w)
            for r in self.res_r.get(w_, ()):
                deps.add(r)
        for r in reads:
            self.res_r.setdefault(r, []).append(o.idx)
        for w_ in writes:
            self.res_w[w_] = o.idx
            self.res_r[w_] = []
        deps.discard(o.idx)
        o.deps = deps
        c = self.dom_count.get(o.dom, 0) + 1
        self.dom_count[o.dom] = c
        o.seq = c
        self.dom_ops.setdefault(o.dom, []).append(o.idx)
        self.ops.append(o)
        return o

    def barrier(self, fn):
        o = self.add("dve", fn)
        last = {}
        for i in range(self.bar_start, o.idx):
            p = self.ops[i]
            last[p.dom] = i
        o.deps = set(last.values())
        if self.bar_idx is not None:
            o.deps.add(self.bar_idx)
        self.bar_idx = o.idx
        self.bar_start = o.idx
        self.barriers.append(o.idx)
        self.res_w = {}
        self.res_r = {}

    def finalize(self, final_wait_prefix="out_"):
        ops = self.ops
        know = {e: {} for e in ("pe", "act", "dve", "pool", "sp")}
        needed = set()
        for o in ops:
            k = know[o.eng]
            waits = {}
            for d in o.deps:
                dop = ops[d]
                if dop.dom == "pe" and o.eng == "pe":
                    continue
                if k.get(dop.dom, 0) >= dop.seq:
                    continue
                if waits.get(dop.dom, 0) < dop.seq:
                    waits[dop.dom] = dop.seq
            real = {}
            for dom, seq in waits.items():
                if k.get(dom, 0) >= seq:
                    continue
                k = dict(k)
                dop = ops[self.dom_ops[dom][seq - 1]]
                for kd, kv in dop.vc.items():
                    if k.get(kd, 0) < kv:
                        k[kd] = kv
                k[dom] = max(k.get(dom, 0), seq)
                needed.add((dom, seq))
                real[dom] = seq
            know[o.eng] = k
            o.waits = real
            o.vc = k
        self.final_waits = []
        for dom, c in self.dom_count.items():
            if isinstance(dom, str) and dom.startswith(final_wait_prefix):
                needed.add((dom, c))
                self.final_waits.append((dom, c))
        self.sig_val = {}
        for dom, lst in self.dom_ops.items():
            n = 0
            for s in range(1, len(lst) + 1):
                if (dom, s) in needed or dom not in COMPUTE:
                    n += 1
                    self.sig_val[(dom, s)] = n

    def emit(self):
        nc = self.nc
        doms = sorted({d for (d, _) in self.sig_val})
        import bisect
        first = {}
        last = {}
        for o in self.ops:
            if o.dom not in COMPUTE:
                first.setdefault(o.dom, o.idx)
                last[o.dom] = o.idx
        phys = {}
        base = {}
        pool_ = []
        nphys = 0
        for d in sorted(first, key=lambda k: first[k]):
            chosen = None
            for ent in pool_:
                bi = bisect.bisect_right(self.barriers, ent[0])
                if bi < len(self.barriers) and self.barriers[bi] <= first[d]:
                    chosen = ent
                    break
            if chosen is None:
                chosen = [0, 0, nphys]
                nphys += 1
                pool_.append(chosen)
            phys[d] = chosen[2]
            base[d] = chosen[1]
            chosen[0] = last[d]
            chosen[1] += self.dom_count[d]
        self.nphys = nphys
        with contextlib.ExitStack() as st:
            psem = [st.enter_context(nc.semaphore("sd%d" % i)) for i in range(nphys)]
            sems = {}
            for d in doms:
                if d in COMPUTE:
                    sems[d] = st.enter_context(nc.semaphore("s_" + str(d)))
                else:
                    sems[d] = psem[phys[d]]
            block = st.enter_context(nc.Block())
            by_eng = {e: [] for e in ("pe", "act", "dve", "pool", "sp")}
            for o in self.ops:
                by_eng[o.eng].append(o)

            def is_dma(dom):
                return dom not in COMPUTE

            def run(engine, lst, final=False):
                for o in lst:
                    for dom, seq in o.waits.items():
                        v = self.sig_val[(dom, seq)]
                        engine.wait_ge(sems[dom], (v + base[dom]) * 16 if is_dma(dom) else v)
                    ins = o.fn(engine)
                    if (o.dom, o.seq) in self.sig_val:
                        ins.then_inc(sems[o.dom], 16 if is_dma(o.dom) else 1)
                if final:
                    for dom, c in self.final_waits:
                        v = self.sig_val[(dom, c)]
                        engine.wait_ge(sems[dom], (v + base[dom]) * 16 if is_dma(dom) else v)

            @block.tensor
            def _(e):
                run(e, by_eng["pe"])

            @block.scalar
            def _(e):
                run(e, by_eng["act"])

            @block.vector
            def _(e):
                run(e, by_eng["dve"])

            @block.gpsimd
            def _(e):
                run(e, by_eng["pool"])

            @block.sync
            def _(e):
                run(e, by_eng["sp"], final=True)


class Arena:
    def __init__(self, t, nwords):
        self.t = t
        self.n = nwords
        self.off = 0

    def f32(self, *shape):
        n = int(np.prod(shape[1:]))
        n = (n + 7) // 8 * 8
        assert self.off + n <= self.n, ("SBUF arena overflow", self.off, n, self.n)
        ap = self.t[0:shape[0], self.off:self.off + int(np.prod(shape[1:]))]
        self.off += n
        return self._shape(ap, shape)

    def bf16(self, *shape):
        ne = int(np.prod(shape[1:]))
        n = (ne + 1) // 2
        n = (n + 7) // 8 * 8
        assert self.off + n <= self.n, ("SBUF arena overflow", self.off, n, self.n)
        ap = self.t[0:shape[0], self.off:self.off + (ne + 1) // 2].bitcast(BF16)
        if ne % 2:
            ap = ap[:, 0:ne]
        self.off += n
        return self._shape(ap, shape)

    @staticmethod
    def _shape(ap, shape):
        if len(shape) == 2:
            return ap
        if len(shape) == 3:
            return ap.rearrange("p (a b) -> p a b", b=shape[2])
        if len(shape) == 4:
            return ap.rearrange("p (a b c) -> p a b c", b=shape[2], c=shape[3])
        raise ValueError(shape)


def build_program():
    nc = bass.Bass("TRN2", target_bir_lowering=False)
    S = Sched(nc)

    def done():
        S.finalize()
        S.emit()
        return nc, S

    def din(name, shape, dt=F32):
        return nc.dram_tensor(name, list(shape), dt, kind="ExternalInput").ap()

    def dout(name, shape, dt=F32):
        return nc.dram_tensor(name, list(shape), dt, kind="ExternalOutput").ap()

    def dscr(name, shape, dt=F32):
        kind = "ExternalOutput" if (DEBUG and (DEBUG_KEEP is None or name in DEBUG_KEEP)) else "Internal"
        return nc.dram_tensor(name, list(shape), dt, kind=kind).ap()

    x_d = din("x", [T, D])
    cond_d = din("cond", [D])
    w_ada_d = din("w_ada", [D, 6 * D])
    b_ada_d = din("b_ada", [6 * D])
    w_in_d = din("w_in", [D, 15552])
    conv_w_d = din("conv_w", [3, 6144])
    conv_b_d = din("conv_b", [6144])
    a_log_d = din("a_log", [128])
    dt_bias_d = din("dt_bias", [128])
    d_skip_d = din("d_skip", [64])
    ssm_norm_w_d = din("ssm_norm_w", [4096])
    w_ssm_out_d = din("w_ssm_out", [4096, D])
    q_a_norm_w_d = din("q_a_norm_w", [512])
    w_q_b_d = din("w_q_b", [512, 3072])
    kv_a_norm_w_d = din("kv_a_norm_w", [512])
    w_kv_b_d = din("w_kv_b", [512, 4096])
    w_mla_out_d = din("w_mla_out", [D, D])
    w_o_d = din("w_o", [D, D])
    ln1_w_d = din("ln1_w", [D])
    ln1_b_d = din("ln1_b", [D])
    router_w_d = din("router_w", [D, 32])
    router_b_d = din("router_b", [32])
    NEW = 32 if STOP_AFTER is None else 1
    w_gu_d = din("w_gu", [NEW, D, 4096])
    b_gu_d = din("b_gu", [32, 4096])
    w_down_d = din("w_down", [NEW, D, D])
    b_down_d = din("b_down", [32, D])
    ln2_w_d = din("ln2_w", [D])
    ln2_b_d = din("ln2_b", [D])
    ctx_ckv_d = din("ctx_ckv", [512, 512])
    ctx_kr_d = din("ctx_kr", [512, 64])
    h0_d = din("h0", [2, 64, 64, 128])
    carry_d = din("carry", [128])
    cos2_d = din("cos2", [64, T])
    sin2_d = din("sin2", [64, T])
    khot_d = din("khot", [9, NKEY])
    qpen_d = din("qpen", [9, T])
    ident_d = din("ident", [128, 128])
    uf_d = din("uf", [128, 128])
    ub_d = din("ub", [128, 128])
    ones_d = din("ones", [128, 128])

    y_d = dout("y", [T, D])
    ckv_out_d = dout("ckv_out", [T, 512])
    kr_out_d = dout("kr_out", [T, 64])
    st_out_d = dout("st_out", [8, 2, 64, 64, 128])

    g1_s = dscr("g1_s", [D])
    xs_tm_s = dscr("xs_tm_s", [T, 4096], BF16)
    b_tm_s = dscr("b_tm_s", [T, 1024], BF16)
    bct_s = dscr("bct_s", [2048, T], BF16)
    zs_s = dscr("zs_s", [T, 4096], BF16)
    dtq_s = dscr("dtq_s", [5, T, 128])
    qnT_s = dscr("qnT_s", [512, T], BF16)
    ckvT_s = dscr("ckvT_s", [512, T], BF16)
    krT_s = dscr("krT_s", [64, T], BF16)
    gT_s = dscr("gT_s", [4096, T], BF16)
    yT_s = dscr("yT_s", [4096, T], BF16)
    oT_s = dscr("oT_s", [2048, T], BF16)
    x1_s = dscr("x1_s", [T, D])

    NW = (nc.sbuf_bytes_remaining - 6144) // 4
    NW = NW // 8 * 8
    arena_t = nc.alloc_sbuf_tensor("arena", [128, NW], F32)
    PERS_W = 1024
    pers = Arena(arena_t, PERS_W)
    ps = [nc.alloc_psum_tensor("ps%d" % i, [128, 512], F32) for i in range(8)]
    psb = [p[:].bitcast(BF16) for p in ps]

    class StageArena(Arena):
        def __init__(self):
            self.t = arena_t
            self.n = NW
            self.off = PERS_W

    def MM(out, lhsT, rhs, start=True, stop=True, r=(), w=()):
        S.add("pe", lambda e: e.matmul(out, lhsT=lhsT, rhs=rhs, start=start, stop=stop), r, w)

    def TR(out, in_, idn, r=(), w=()):
        S.add("pe", lambda e: e.transpose(out, in_, idn), r, w)

    def ACT(out, in_, func, r=(), w=(), bias=None, scale=None, accum=None):
        kw = {}
        if bias is not None:
            kw["bias"] = bias
        if scale is not None:
            kw["scale"] = scale
        if accum is not None:
            kw["accum_out"] = accum
        S.add("act", lambda e: e.activation(out=out, in_=in_, func=func, **kw), r, w)

    def TT(eng, out, in0, in1, op, r=(), w=()):
        S.add(eng, lambda e: e.tensor_tensor(out=out, in0=in0, in1=in1, op=op), r, w)

    def TS(eng, out, in0, s1, op0, s2=None, op1=None, r=(), w=(), accum=None):
        kw = {}
        if op1 is not None:
            kw["op1"] = op1
        if accum is not None:
            kw["accum_out"] = accum
        S.add(eng, lambda e: e.tensor_scalar(out=out, in0=in0, scalar1=s1, scalar2=s2, op0=op0, **kw), r, w)

    def STT(eng, out, in0, scalar, in1, op0, op1, r=(), w=(), accum=None):
        kw = {}
        if accum is not None:
            kw["accum_out"] = accum
        S.add(eng, lambda e: e.scalar_tensor_tensor(out=out, in0=in0, scalar=scalar, in1=in1, op0=op0, op1=op1, **kw), r, w)

    def CP(eng, out, in_, r=(), w=()):
        if eng == "act":
            S.add("act", lambda e: e.copy(out=out, in_=in_), r, w)
        else:
            S.add(eng, lambda e: e.tensor_copy(out=out, in_=in_), r, w)

    def RECIP(out, in_, r=(), w=()):
        S.add("dve", lambda e: e.reciprocal(out=out, in_=in_), r, w)

    def DMA(q, out, in_, key, r=(), w=(), slow=False):
        if slow:
            S.add(q, lambda e: e.dma_start(out=out, in_=in_, allow_slow_non_contiguous=True), r, w, key=key)
        else:
            S.add(q, lambda e: e.dma_start(out=out, in_=in_), r, w, key=key)

    def MEMSET(eng, ap, val, r=(), w=()):
        S.add(eng, lambda e: e.memset(ap, val), r, w)

    bar_t = pers.f32(128, 8)

    def BARRIER():
        S.barrier(lambda e: e.memset(bar_t, 0.0))

    ident = pers.f32(128, 128)
    identb = pers.bf16(128, 128)
    onesf = pers.f32(128, 128)
    onesb = pers.bf16(128, 128)
    uf = pers.f32(128, 128)
    ub = pers.f32(128, 128)
    mod = pers.f32(128, 96)
    sc1p = pers.f32(128, 16)
    sc2p = pers.f32(128, 16)
    carry = pers.f32(128, 1)
    cm1 = pers.f32(128, 1)
    DMA("sp", ident, ident_d, "c_ident", w=["ident"])
    DMA("pool", identb, ident_d, "c_identb", w=["identb"])
    DMA("sp", onesf, ones_d, "c_ones", w=["onesf"])
    DMA("pool", onesb, ones_d, "c_onesb", w=["onesb"])
    DMA("sp", uf, uf_d, "c_uf", w=["uf"])
    DMA("sp", ub, ub_d, "c_ub", w=["ub"])
    DMA("sp", carry, carry_d.rearrange("(p o) -> p o", o=1), "c_carry", w=["carry"], slow=True)
    TS("dve", cm1, carry, -1.0, ALU.add, r=["carry"], w=["cm1"])

    A = StageArena()
    condc = A.f32(128, 16)
    silc = A.f32(128, 16)
    bada = A.f32(128, 96)
    wsl = [A.bf16(128, 16, 1024) for _ in range(2)]
    silb = A.bf16(128, 16)
    DMA("sp", condc, cond_d.rearrange("(c p) -> p c", p=128), "s0_cond", w=["condc"], slow=True)
    DMA("sp", bada, b_ada_d.rearrange("(j p) -> p j", p=128), "s0_bada", w=["bada"], slow=True)
    ACT(silc, condc, AF.Silu, r=["condc"], w=["silc"])
    CP("dve", silb, silc, r=["silc"], w=["silb"])
    w_ada_v = w_ada_d.rearrange("(k p) n -> p k n", p=128)
    for blk in range(12):
        s = blk % 2
        DMA("pool", wsl[s], w_ada_v[:, :, blk * 1024:(blk + 1) * 1024], "s0_w%d" % s, w=["wada%d" % s])
        for mm in range(8):
            col = blk * 8 + mm
            for k in range(KC):
                MM(ps[0][:, col:col + 1], wsl[s][:, k, mm * 128:(mm + 1) * 128], silb[:, k:k + 1],
                   start=(k == 0), stop=(k == KC - 1), r=["wada%d" % s, "silb"], w=["ps0"])
    TT("dve", mod, ps[0][:, 0:96], bada, ALU.add, r=["ps0", "bada"], w=["mod"])
    TS("dve", sc1p, mod[:, 16:32], 1.0, ALU.add, r=["mod"], w=["sc1p"])
    TS("dve", sc2p, mod[:, 64:80], 1.0, ALU.add, r=["mod"], w=["sc2p"])
    DMA("sp", g1_s.rearrange("(c p) -> p c", p=128), mod[:, 32:48], "s0_g1", r=["mod"], slow=True)
    sh1 = mod[:, 0:16]
    sh2 = mod[:, 48:64]
    g2c = mod[:, 80:96]
    BARRIER()
    if STOP_AFTER == '0':
        return done()

    A = StageArena()
    hT = A.bf16(128, 16, T)
    mark_h = A.off
    xsl = [A.f32(128, D) for _ in range(2)]
    for i in range(NT):
        s = i % 2
        DMA("sp", xsl[s], x_d[i * 128:(i + 1) * 128, :], "s1_x%d" % s, w=["xs%d" % s])
        for cq in range(4):
            b = cq
            for cc in range(4):
                c = cq * 4 + cc
                TR(ps[b][:, cc * 128:(cc + 1) * 128], xsl[s][:, c * 128:(c + 1) * 128], ident,
                   r=["xs%d" % s, "ident"], w=["ps%d" % b])
            for cc in range(4):
                c = cq * 4 + cc
                o_ = hT[:, c, i * 128:(i + 1) * 128]
                i_ = ps[b][:, cc * 128:(cc + 1) * 128]
                if cc % 2 == 0:
                    ACT(o_, i_, AF.Identity, r=["ps%d" % b, "sc1p", "mod"], w=["hT"], scale=sc1p[:, c:c + 1], bias=sh1[:, c:c + 1])
                else:
                    TS("dve", o_, i_, sc1p[:, c:c + 1], ALU.mult, sh1[:, c:c + 1], ALU.add, r=["ps%d" % b, "sc1p", "mod"], w=["hT"])
    BARRIER()
    if STOP_AFTER == '1':
        return done()

    A.off = mark_h
    w_in_v = w_in_d.rearrange("(k p) n -> p k n", p=128)
    NSL = 3
    wsl = [A.bf16(128, 16, 512) for _ in range(NSL)]
    cw = A.f32(128, 48, 3)
    cb = A.f32(128, 48)
    w0c = A.f32(128, 48)
    w2c = A.f32(128, 48)
    qnw = A.f32(128, 4)
    kvw = A.f32(128, 4)
    mark_u = A.off
    cos2 = A.f32(64, T)
    sin2 = A.f32(64, T)
    for j in range(3):
        DMA("sp", cw[:, :, j], conv_w_d[j].rearrange("(c p) -> p c", p=128), "sA_cw%d" % j, w=["cw%d" % j], slow=True)
    DMA("sp", cb, conv_b_d.rearrange("(c p) -> p c", p=128), "sA_cb", w=["cb"], slow=True)
    DMA("sp", qnw, q_a_norm_w_d.rearrange("(c p) -> p c", p=128), "sA_qnw", w=["qnw"], slow=True)
    DMA("sp", kvw, kv_a_norm_w_d.rearrange("(c p) -> p c", p=128), "sA_kvw", w=["kvw"], slow=True)
    DMA("sp", cos2, cos2_d, "sA_cos", w=["cos2"])
    DMA("sp", sin2, sin2_d, "sA_sin", w=["sin2"])
    TS("dve", w0c, cw[:, :, 0], cm1[:, 0:1], ALU.mult, r=["cw0", "cm1"], w=["w0c"])
    TS("dve", w2c, cw[:, :, 2], cm1[:, 0:1], ALU.mult, r=["cw2", "cm1"], w=["w2c"])

    groups = []
    groups.append(("qa", 10368, 0))
    groups.append(("kva", 10880, 0))
    for g in range(12):
        groups.append(("xbc", 4096 + g * 512, g))
    for g in range(8):
        groups.append(("gate", 11456 + g * 512, g))
    for g in range(8):
        groups.append(("z", g * 512, g))

    def issue_w(n):
        if n < len(groups):
            s = n % NSL
            c0 = groups[n][1]
            DMA("pool", wsl[s], w_in_v[:, :, c0:c0 + 512], "sA_w%d" % s, w=["wA%d" % s])

    sq = A.f32(128, 4, 512)
    rt = A.f32(128, 512)
    rstd = A.f32(128, 512)
    nT = A.bf16(128, 4, T)
    ckn32 = A.f32(128, 4, 512)
    otile = [A.f32(128, 512) for _ in range(2)]
    wkr = A.bf16(128, 16, 64)
    wkrs = A.bf16(128, 16, 64)
    kr32 = A.f32(64, 512)
    krt1 = A.f32(64, 512)
    krt2 = A.f32(64, 512)
    krT = A.bf16(64, T)
    kro = [A.f32(128, 4, 64) for _ in range(2)]
    DMA("pool", wkr, w_in_v[:, :, 11392:11456], "sA_wkr", w=["wkr"])
    CP("dve", wkrs[:, :, 0:32], wkr[:, :, 32:64], r=["wkr"], w=["wkrs"])
    CP("dve", wkrs[:, :, 32:64], wkr[:, :, 0:32], r=["wkr"], w=["wkrs"])

    for n in range(NSL - 1):
        issue_w(n)
    bankrr = [0]

    def next_bank():
        b = bankrr[0] % 4
        bankrr[0] += 1
        return b

    chunk_ctr = [0]
    for n, (kind, c0, g) in enumerate(groups):
        issue_w(n + NSL - 1)
        s = n % NSL
        wres = "wA%d" % s
        if n == 2:
            BARRIER()
            if STOP_AFTER in ("Aqa", "Aqa_a", "Aqa_b", "Aqa_c", "Aqa_d"):
                return done()
            A.off = mark_u
            pre = [A.f32(128, T + 2) for _ in range(2)]
            acc = A.f32(128, T)
            post = [A.bf16(128, T) for _ in range(2)]
            tm = [A.bf16(128, 16, 128) for _ in range(2)]
            zt = [A.bf16(128, 512) for _ in range(4)]
            for s_ in range(2):
                MEMSET("dve", pre[s_][:, 0:1], 0.0, w=["pre%d" % s_])
                MEMSET("dve", pre[s_][:, T + 1:T + 2], 0.0, w=["pre%d" % s_])
        if (STOP_AFTER == "Ap" and n == 0) or (STOP_AFTER == "Aqa1" and n == 1):
            BARRIER()
            return done()
        if STOP_AFTER == "Ax1" and n == 3:
            BARRIER()
            return done()
        if kind == "xbc":
            for j in range(4):
                cc = g * 4 + j
                pslot = chunk_ctr[0] % 2
                chunk_ctr[0] += 1
                P_ = pre[pslot]
                for tb in range(4):
                    b = next_bank()
                    for k in range(KC):
                        MM(ps[b][:, :], wsl[s][:, k, j * 128:(j + 1) * 128], hT[:, k, tb * 512:(tb + 1) * 512],
                           start=(k == 0), stop=(k == KC - 1), r=[wres, "hT"], w=["ps%d" % b])
                    CP("act", P_[:, 1 + tb * 512:1 + (tb + 1) * 512], ps[b][:, :], r=["ps%d" % b], w=["pre%d" % pslot])
                pr = ["pre%d" % pslot]
                TS("dve", acc, P_[:, 0:T], cw[:, cc, 0:1], ALU.mult, r=pr + ["cw0"], w=["acc"])
                STT("dve", acc, P_[:, 1:T + 1], cw[:, cc, 1:2], acc, ALU.mult, ALU.add, r=pr + ["cw1", "acc"], w=["acc"])
                STT("dve", acc, P_[:, 2:T + 2], cw[:, cc, 2:3], acc, ALU.mult, ALU.add, r=pr + ["cw2", "acc"], w=["acc"])
                accv = acc.rearrange("p (s t) -> p s t", t=256)
                xv = P_[:, 1:T + 1].rearrange("p (s t) -> p s t", t=256)
                STT("dve", accv[:, 1:8, 0:1], xv[:, 0:7, 255:256], w0c[:, cc:cc + 1], accv[:, 1:8, 0:1], ALU.mult, ALU.add,
                    r=pr + ["w0c", "acc"], w=["acc"])
                STT("dve", accv[:, 0:7, 255:256], xv[:, 1:8, 0:1], w2c[:, cc:cc + 1], accv[:, 0:7, 255:256], ALU.mult, ALU.add,
                    r=pr + ["w2c", "acc"], w=["acc"])
                ACT(post[pslot], acc, AF.Silu, r=["acc", "cb"], w=["post%d" % pslot], bias=cb[:, cc:cc + 1])
                if cc < 40:
                    for half in range(2):
                        b = 4 + half
                        for i in range(8):
                            tix = half * 8 + i
                            TR(psb[b][:, i * 128:(i + 1) * 128], post[pslot][:, tix * 128:(tix + 1) * 128], identb,
                               r=["post%d" % pslot, "identb"], w=["ps%d" % b])
                        CP("dve", tm[pslot][:, half * 8:(half + 1) * 8, :], psb[b][:, :].rearrange("p (a b) -> p a b", b=128),
                           r=["ps%d" % b], w=["tm%d" % pslot])
                    if cc < 32:
                        dst = xs_tm_s.rearrange("(i p) c -> p i c", p=128)[:, :, cc * 128:(cc + 1) * 128]
                    else:
                        dst = b_tm_s.rearrange("(i p) c -> p i c", p=128)[:, :, (cc - 32) * 128:(cc - 31) * 128]
                    DMA("sp", dst, tm[pslot], "sA_tm%d" % pslot, r=["tm%d" % pslot])
                if cc >= 32:
                    DMA("sp", bct_s[(cc - 32) * 128:(cc - 31) * 128, :], post[pslot], "sA_post%d" % pslot, r=["post%d" % pslot])
        elif kind in ("qa", "kva"):
            nw = qnw if kind == "qa" else kvw
            for tb in range(4):
                tsl = slice(tb * 512, (tb + 1) * 512)
                for c in range(4):
                    for k in range(KC):
                        MM(ps[c][:, :], wsl[s][:, k, c * 128:(c + 1) * 128], hT[:, k, tsl],
                           start=(k == 0), stop=(k == KC - 1), r=[wres, "hT"], w=["ps%d" % c])
                for c in range(4):
                    ACT(sq[:, c, :], ps[c][:, :], AF.Square, r=["ps%d" % c], w=["sq"])
                for c in range(4):
                    MM(ps[6][:, :], onesf, sq[:, c, :], start=(c == 0), stop=(c == 3), r=["sq", "onesf"], w=["ps6"])
                ACT(rt, ps[6][:, :], AF.Sqrt, r=["ps6"], w=["rt"], scale=1.0 / 512.0, bias=1e-6)
                RECIP(rstd, rt, r=["rt"], w=["rstd"])
                if kind == "qa":
                    for c in range(4):
                        STT("dve", nT[:, c, tsl], ps[c][:, :], nw[:, c:c + 1], rstd, ALU.mult, ALU.mult,
                            r=["ps%d" % c, "qnw", "rstd"], w=["nT"])
                else:
                    for c in range(4):
                        STT("dve", ckn32[:, c, :], ps[c][:, :], nw[:, c:c + 1], rstd, ALU.mult, ALU.mult,
                            r=["ps%d" % c, "kvw", "rstd"], w=["ckn32"])
                    CP("pool", nT[:, :, tsl], ckn32, r=["ckn32"], w=["nT"])
                    for i4 in range(4 if STOP_AFTER != "Aqa_b" else 0):
                        os_ = (tb * 4 + i4) % 2
                        for c in range(4):
                            TR(ps[4][:, c * 128:(c + 1) * 128], ckn32[:, c, i4 * 128:(i4 + 1) * 128], ident,
                               r=["ckn32", "ident"], w=["ps4"])
                        CP("act", otile[os_], ps[4][:, :], r=["ps4"], w=["otile%d" % os_])
                        row = (tb * 4 + i4) * 128
                        DMA("sp", ckv_out_d[row:row + 128, :], otile[os_], "out_ckv%d" % os_, r=["otile%d" % os_])
                    if STOP_AFTER == "Aqa_a":
                        continue
                    for k in range(KC):
                        MM(ps[5][0:64, :], wkr[:, k, :], hT[:, k, tsl], start=(k == 0), stop=(k == KC - 1), r=["wkr", "hT"], w=["ps5"])
                    for k in range(KC):
                        MM(ps[7][0:64, :], wkrs[:, k, :], hT[:, k, tsl], start=(k == 0), stop=(k == KC - 1), r=["wkrs", "hT"], w=["ps7"])
                    if STOP_AFTER == "Aqa_d":
                        continue
                    CP("act", kr32, ps[5][0:64, :], r=["ps5"], w=["kr32"])
                    TT("dve", krt1, ps[5][0:64, :], cos2[:, tsl], ALU.mult, r=["ps5", "cos2"], w=["krt1"])
                    TT("dve", krt2, ps[7][0:64, :], sin2[:, tsl], ALU.mult, r=["ps7", "sin2"], w=["krt2"])
                    TT("dve", krT[:, tsl], krt1, krt2, ALU.add, r=["krt1", "krt2"], w=["krT"])
                    ks_ = tb % 2
                    if STOP_AFTER == "Aqa_c":
                        continue
                    for i4 in range(4):
                        TR(ps[6][:, i4 * 64:(i4 + 1) * 64], kr32[:, i4 * 128:(i4 + 1) * 128], ident[0:64, 0:64],
                           r=["kr32", "ident"], w=["ps6"])
                    CP("act", kro[ks_], ps[6][:, 0:256].rearrange("p (a b) -> p a b", b=64), r=["ps6"], w=["kro%d" % ks_])
                    DMA("sp", kr_out_d[tb * 512:(tb + 1) * 512, :].rearrange("(a p) c -> p a c", p=128), kro[ks_],
                        "out_kr%d" % ks_, r=["kro%d" % ks_])
            if kind == "qa":
                DMA("sp", qnT_s.rearrange("(c p) t -> p c t", p=128), nT, "sA_nT", r=["nT"])
            else:
                DMA("sp", ckvT_s.rearrange("(c p) t -> p c t", p=128), nT, "sA_nT", r=["nT"])
                DMA("sp", krT_s, krT, "sA_krT", r=["krT"])
        elif kind == "gate":
            for j in range(4):
                cc = g * 4 + j
                pslot = chunk_ctr[0] % 2
                chunk_ctr[0] += 1
                for tb in range(4):
                    b = next_bank()
                    for k in range(KC):
                        MM(ps[b][:, :], wsl[s][:, k, j * 128:(j + 1) * 128], hT[:, k, tb * 512:(tb + 1) * 512],
                           start=(k == 0), stop=(k == KC - 1), r=[wres, "hT"], w=["ps%d" % b])
                    ACT(post[pslot][:, tb * 512:(tb + 1) * 512], ps[b][:, :], AF.Sigmoid, r=["ps%d" % b], w=["post%d" % pslot])
                DMA("sp", gT_s[cc * 128:(cc + 1) * 128, :], post[pslot], "sA_post%d" % pslot, r=["post%d" % pslot])
        else:
            for i in range(NT):
                b = next_bank()
                zs_ = i % 4
                for k in range(KC):
                    MM(ps[b][:, :], hT[:, k, i * 128:(i + 1) * 128], wsl[s][:, k, :],
                       start=(k == 0), stop=(k == KC - 1), r=[wres, "hT"], w=["ps%d" % b])
                ACT(zt[zs_], ps[b][:, :], AF.Silu, r=["ps%d" % b], w=["zt%d" % zs_])
                DMA("sp", zs_s[i * 128:(i + 1) * 128, g * 512:(g + 1) * 512], zt[zs_], "sA_zt%d" % zs_, r=["zt%d" % zs_])
    BARRIER()
    if STOP_AFTER == 'A':
        return done()

    A.off = mark_h
    wdt = A.bf16(128, 16, 128)
    dtraw = A.f32(128, 16, 128)
    dte = A.f32(128, 16, 128)
    dtv = A.f32(128, 16, 128)
    lndt = A.f32(128, 16, 128)
    qa_ = A.f32(128, 16, 128)
    qbexp = A.f32(128, 16, 128)
    qeac = A.f32(128, 16, 128)
    qwdec = A.f32(128, 16, 128)
    qcdec = A.f32(128, 16, 128)
    dtb_bc = A.f32(128, 128)
    alog_bc = A.f32(128, 128)
    Abc = A.f32(128, 128)
    acs = [A.f32(128, 128) for _ in range(2)]
    tmpd = [A.f32(128, 128) for _ in range(2)]
    DMA("pool", wdt, w_in_v[:, :, 10240:10368], "sA3_w", w=["wdt"])
    DMA("sp", dtb_bc, dt_bias_d.partition_broadcast(128), "sA3_dtb", w=["dtb"])
    DMA("sp", alog_bc, a_log_d.partition_broadcast(128), "sA3_alog", w=["alog"])
    ACT(Abc, alog_bc, AF.Exp, r=["alog"], w=["Abc"])
    TS("dve", Abc, Abc, -1.0, ALU.mult, r=["Abc"], w=["Abc"])
    for i in range(NT):
        b = i // 4 % 2
        for k in range(KC):
            MM(ps[b][:, (i % 4) * 128:(i % 4 + 1) * 128], hT[:, k, i * 128:(i + 1) * 128], wdt[:, k, :],
               start=(k == 0), stop=(k == KC - 1), r=["wdt", "hT"], w=["ps%d" % b])
        if i % 4 == 3:
            i0 = i - 3
            TT("dve", dtraw[:, i0:i0 + 4, :], ps[b][:, :].rearrange("p (a b) -> p a b", b=128),
               dtb_bc.unsqueeze(1).to_broadcast([128, 4, 128]), ALU.add, r=["ps%d" % b, "dtb"], w=["dtraw"])
    ACT(dte, dtraw, AF.Exp, r=["dtraw"], w=["dte"])
    ACT(dtv, dte, AF.Ln, r=["dte"], w=["dtv"], bias=1.0, scale=1.0)
    ACT(lndt, dtv, AF.Ln, r=["dtv"], w=["lndt"])
    TT("dve", qa_, dtv, Abc.unsqueeze(1).to_broadcast([128, 16, 128]), ALU.mult, r=["dtv", "Abc"], w=["qa"])
    for c in range(NT):
        s = c % 2
        MM(ps[2 + s][:, 0:64], uf, qa_[:, c, 0:64], r=["uf", "qa"], w=["ps%d" % (2 + s)])
        MM(ps[2 + s][:, 64:128], ub, qa_[:, c, 64:128], r=["ub", "qa"], w=["ps%d" % (2 + s)])
        MM(ps[4 + s][:, 0:128], onesf, qa_[:, c, :], r=["onesf", "qa"], w=["ps%d" % (4 + s)])
        CP("act", acs[s], ps[2 + s][:, 0:128], r=["ps%d" % (2 + s)], w=["acs%d" % s])
        TT("dve", qbexp[:, c, :], lndt[:, c, :], acs[s], ALU.subtract, r=["lndt", "acs%d" % s], w=["qbexp"])
        ACT(qeac[:, c, :], ps[2 + s][:, 0:128], AF.Exp, r=["ps%d" % (2 + s)], w=["qeac"])
        ACT(qcdec[:, c, :], ps[4 + s][:, 0:128], AF.Exp, r=["ps%d" % (4 + s)], w=["qcdec"])
        TT("dve", tmpd[s], ps[4 + s][:, 0:128], acs[s], ALU.subtract, r=["ps%d" % (4 + s), "acs%d" % s], w=["tmpd%d" % s])
        ACT(tmpd[s], tmpd[s], AF.Exp, r=["tmpd%d" % s], w=["tmpd%d" % s])
        TT("dve", qwdec[:, c, :], tmpd[s], dtv[:, c, :], ALU.mult, r=["tmpd%d" % s, "dtv"], w=["qwdec"])
    for qi, (qt, nm) in enumerate(((qa_, "qa"), (qbexp, "qbexp"), (qeac, "qeac"), (qwdec, "qwdec"), (qcdec, "qcdec"))):
        DMA("sp", dtq_s[qi].rearrange("(c p) n -> p c n", p=128), qt, "sA3_q%d" % qi, r=[nm])
    BARRIER()
    if STOP_AFTER == 'A3':
        return done()

    A = StageArena()
    dq = [A.f32(128, 16, 128) for _ in range(5)]
    q_a, q_bexp, q_eac, q_wdec, q_cdec = dq
    for qi in range(5):
        DMA("sp", dq[qi], dtq_s[qi].rearrange("(c p) n -> p c n", p=128), "sB_q%d" % qi, w=["dq"])
    D_bc = A.f32(128, 64)
    DMA("sp", D_bc, d_skip_d.partition_broadcast(128), "sB_D", w=["D_bc"])
    xtm = [A.bf16(128, 16, 512) for _ in range(2)]
    btm = [A.bf16(128, 16, 128) for _ in range(2)]
    BTt = [A.bf16(128, T) for _ in range(2)]
    CTt = [A.bf16(128, T) for _ in range(2)]
    normw = [A.f32(128, 512) for _ in range(2)]
    yacc = A.f32(128, 16, 512)
    S32 = [A.f32(128, 512) for _ in range(2)]
    Sbf = [A.bf16(128, 512) for _ in range(2)]
    cbm = [A.bf16(128, 128) for _ in range(2)]
    Lsb = [A.bf16(128, 8, 128) for _ in range(2)]
    mT = [A.bf16(128, 8, 128) for _ in range(2)]
    yoff = [A.f32(128, 512) for _ in range(2)]
    xw = [A.bf16(128, 512) for _ in range(2)]
    ztB = [A.bf16(128, 512) for _ in range(2)]
    yg = [A.f32(128, 512) for _ in range(2)]
    ygsq = A.f32(128, 512)
    yn = [A.bf16(128, 512) for _ in range(2)]
    ssq = [A.f32(128, 1) for _ in range(2)]
    srt = [A.f32(128, 1) for _ in range(2)]
    srs = [A.f32(128, 1) for _ in range(2)]
    yTsb = A.bf16(128, 4, T)
    stout = [A.f32(128, 4, 128) for _ in range(2)]
    h0t = [A.f32(128, 4, 128) for _ in range(2)]
    xs_tm_v = xs_tm_s.rearrange("(i p) c -> p i c", p=128)
    b_tm_v = b_tm_s.rearrange("(i p) c -> p i c", p=128)

    def loadB(g):
        s = g % 2
        DMA("sp", xtm[s], xs_tm_v[:, :, g * 512:(g + 1) * 512], "sB_x%d" % s, w=["xtm%d" % s])
        DMA("sp", btm[s], b_tm_v[:, :, g * 128:(g + 1) * 128], "sB_b%d" % s, w=["btm%d" % s])
        DMA("sp", BTt[s], bct_s[g * 128:(g + 1) * 128, :], "sB_BT%d" % s, w=["BT%d" % s])
        DMA("sp", CTt[s], bct_s[1024 + g * 128:1024 + (g + 1) * 128, :], "sB_CT%d" % s, w=["CT%d" % s])
        DMA("sp", normw[s], ssm_norm_w_d[g * 512:(g + 1) * 512].partition_broadcast(128), "sB_nw%d" % s, w=["normw%d" % s])

    loadB(0)
    it = [0]
    for g in range(8):
        gs = g % 2
        if g + 1 < 8:
            loadB(g + 1)
        X = xtm[gs]
        xr = "xtm%d" % gs
        for c in range(NT):
            TT("pool", yacc[:, c, :].rearrange("p (h q) -> p h q", q=64), X[:, c, :].rearrange("p (h q) -> p h q", q=64),
               D_bc[:, g * 8:(g + 1) * 8].unsqueeze(2).to_broadcast([128, 8, 64]), ALU.mult, r=[xr, "D_bc"], w=["yacc%d" % c])
        iters = [(d, c) for d in range(2) for c in (range(NT) if d == 0 else range(NT - 1, -1, -1))]

        def front(idx):
            d, c = iters[idx]
            k2 = idx % 2
            U = uf if d == 0 else ub
            ures = "uf" if d == 0 else "ub"
            col0 = d * 64 + g * 8
            csl = slice(c * 128, (c + 1) * 128)
            MM(ps[0][:, 0:128], BTt[gs][:, csl], CTt[gs][:, csl], r=["BT%d" % gs, "CT%d" % gs], w=["ps0"])
            TT("dve", cbm[k2], ps[0][:, 0:128], U, ALU.mult, r=["ps0", ures], w=["cbm%d" % k2])
            for half in range(2):
                for jj in range(4):
                    j = half * 4 + jj
                    MM(ps[1 + half][:, jj * 128:(jj + 1) * 128], q_a[:, c, col0 + j:col0 + j + 1].to_broadcast([128, 128]), U,
                       r=["dq", ures], w=["ps%d" % (1 + half)])
                for jj in range(4):
                    j = half * 4 + jj
                    ACT(Lsb[k2][:, j, :], ps[1 + half][:, jj * 128:(jj + 1) * 128], AF.Exp,
                        r=["ps%d" % (1 + half), "dq"], w=["L%d_%d" % (k2, half)], bias=q_bexp[:, c, col0 + j:col0 + j + 1])
                STT("dve", mT[k2][:, half * 4:(half + 1) * 4, :], Lsb[k2][:, half * 4:(half + 1) * 4, :], 1e30,
                    cbm[k2].unsqueeze(1).to_broadcast([128, 4, 128]), ALU.min, ALU.mult,
                    r=["L%d_%d" % (k2, half), "cbm%d" % k2], w=["mT%d_%d" % (k2, half)])

        def back(idx):
            d, c = iters[idx]
            k2 = idx % 2
            col0 = d * 64 + g * 8
            first = (c == 0) if d == 0 else (c == NT - 1)
            seg_start = (c % 2 == 0) if d == 0 else (c % 2 == 1)
            seg_end = (c % 2 == 1) if d == 0 else (c % 2 == 0)
            sres = "S32_%d" % d
            bres = "Sbf_%d" % d
            csl = slice(c * 128, (c + 1) * 128)
            if first:
                hs = (g * 2 + d) % 2
                DMA("sp", h0t[hs], h0_d[d, g * 8:(g + 1) * 8].rearrange("(jj h2) q n -> (h2 q) jj n", h2=2),
                    "sB_h0%d" % hs, w=["h0t%d" % hs])
                for jj in range(4):
                    TR(ps[6][:, jj * 128:(jj + 1) * 128], h0t[hs][:, jj, :], ident, r=["h0t%d" % hs, "ident"], w=["ps6"])
                CP("dve", S32[d], ps[6][:, :], r=["ps6"], w=[sres])
                CP("act", Sbf[d], ps[6][:, :], r=["ps6"], w=[bres])
            elif seg_start:
                TS("dve", S32[d], S32[d], carry[:, 0:1], ALU.mult, r=[sres, "carry"], w=[sres])
                CP("act", Sbf[d], S32[d], r=[sres], w=[bres])
            MM(ps[4][:, :], CTt[gs][:, csl], Sbf[d], r=["CT%d" % gs, bres], w=["ps4"])
            for j in range(8):
                MM(ps[3][:, j * 64:(j + 1) * 64], mT[k2][:, j, :], X[:, c, j * 64:(j + 1) * 64],
                   r=["mT%d_%d" % (k2, j // 4), xr], w=["ps3"])
            TT("pool", xw[k2].rearrange("p (h q) -> p h q", q=64), X[:, c, :].rearrange("p (h q) -> p h q", q=64),
               q_wdec[:, c, col0:col0 + 8].unsqueeze(2).to_broadcast([128, 8, 64]), ALU.mult, r=[xr, "dq"], w=["xw%d" % k2])
            MM(ps[5][:, :], btm[gs][:, c, :], xw[k2], r=["btm%d" % gs, "xw%d" % k2], w=["ps5"])
            TT("dve", yoff[k2].rearrange("p (h q) -> p h q", q=64), ps[4][:, :].rearrange("p (h q) -> p h q", q=64),
               q_eac[:, c, col0:col0 + 8].unsqueeze(2).to_broadcast([128, 8, 64]), ALU.mult, r=["ps4", "dq"], w=["yoff%d" % k2])
            TT("pool", S32[d].rearrange("p (h q) -> p h q", q=64), S32[d].rearrange("p (h q) -> p h q", q=64),
               q_cdec[:, c, col0:col0 + 8].unsqueeze(2).to_broadcast([128, 8, 64]), ALU.mult, r=[sres, "dq"], w=[sres])
            TT("dve", S32[d], S32[d], ps[5][:, :], ALU.add, r=[sres, "ps5"], w=[sres])
            CP("act", Sbf[d], S32[d], r=[sres], w=[bres])
            TT("dve", yoff[k2], yoff[k2], ps[3][:, :], ALU.add, r=["yoff%d" % k2, "ps3"], w=["yoff%d" % k2])
            TT("pool", yacc[:, c, :], yacc[:, c, :], yoff[k2], ALU.add, r=["yacc%d" % c, "yoff%d" % k2], w=["yacc%d" % c])
            if seg_end:
                so = (c // 2 + d) % 2
                for jj in range(4):
                    TR(ps[6][:, jj * 128:(jj + 1) * 128], S32[d][:, jj * 128:(jj + 1) * 128], ident, r=[sres, "ident"], w=["ps6"])
                CP("act", stout[so], ps[6][:, :].rearrange("p (a b) -> p a b", b=128), r=["ps6"], w=["stout%d" % so])
                DMA("sp", st_out_d[c // 2, d, g * 8:(g + 1) * 8].rearrange("(jj h2) q n -> (h2 q) jj n", h2=2), stout[so],
                    "out_st%d" % so, r=["stout%d" % so])

        front(0)
        for idx in range(len(iters)):
            if idx + 1 < len(iters):
                front(idx + 1)
            back(idx)
        for c in range(NT):
            k2 = c % 2
            DMA("sp", ztB[k2], zs_s[c * 128:(c + 1) * 128, g * 512:(g + 1) * 512], "sB_zt%d" % k2, w=["ztB%d" % k2])
            TT("dve", yg[k2], yacc[:, c, :], ztB[k2], ALU.mult, r=["yacc%d" % c, "ztB%d" % k2], w=["yg%d" % k2])
            ACT(ygsq, yg[k2], AF.Square, r=["yg%d" % k2], w=["ygsq", "ssq%d" % k2], accum=ssq[k2])
            ACT(srt[k2], ssq[k2], AF.Sqrt, r=["ssq%d" % k2], w=["srt%d" % k2], scale=1.0 / 512.0, bias=1e-6)
            RECIP(srs[k2], srt[k2], r=["srt%d" % k2], w=["srs%d" % k2])
            STT("dve", yn[k2], yg[k2], srs[k2][:, 0:1], normw[gs], ALU.mult, ALU.mult, r=["yg%d" % k2, "srs%d" % k2, "normw%d" % gs], w=["yn%d" % k2])
            for cc in range(4):
                TR(psb[7][:, cc * 128:(cc + 1) * 128], yn[k2][:, cc * 128:(cc + 1) * 128], identb, r=["yn%d" % k2, "identb"], w=["ps7"])
            CP("act", yTsb[:, :, c * 128:(c + 1) * 128], psb[7][:, 0:512].rearrange("p (a b) -> p a b", b=128), r=["ps7"], w=["yTsb"])
        DMA("sp", yT_s.rearrange("(cc p) t -> p cc t", p=128)[:, g * 4:(g + 1) * 4, :], yTsb, "sB_yT", r=["yTsb"])
    BARRIER()
    if STOP_AFTER == 'B':
        return done()

    A = StageArena()
    qnT = A.bf16(128, 4, T)
    ckvT = A.bf16(128, 4, NKEY)
    kra = A.bf16(73, NKEY)
    wq = A.bf16(128, 4, 3072)
    wkv = A.bf16(128, 4, 4096)
    cos2 = A.f32(64, T)
    sin2 = A.f32(64, T)
    cxl = [A.f32(128, 512) for _ in range(2)]
    cxk = A.f32(128, 4, 64)
    wqs = [A.bf16(128, 4, 64) for _ in range(2)]
    qn_h = [A.bf16(128, T) for _ in range(2)]
    qra = [A.bf16(73, T) for _ in range(2)]
    kn_h = [A.bf16(128, NKEY) for _ in range(2)]
    v_h = [A.bf16(128, 20, 128) for _ in range(2)]
    PT = [A.bf16(128, 512) for _ in range(4)]
    rden = A.f32(128, 512)
    oTh = [A.bf16(128, T) for _ in range(2)]
    rp1 = A.f32(64, 512)
    rp2 = A.f32(64, 512)
    DMA("sp", qnT, qnT_s.rearrange("(c p) t -> p c t", p=128), "sC_qnT", w=["qnT"])
    DMA("sp", ckvT[:, :, 0:T], ckvT_s.rearrange("(c p) t -> p c t", p=128), "sC_ckvT", w=["ckvT_own"])
    DMA("sp", kra[0:64, 0:T], krT_s, "sC_kr", w=["kra_own"])
    DMA("pool", kra[64:73, :], khot_d, "sC_khot", w=["kra_hot"])
    for s in range(2):
        DMA("pool", qra[s][64:73, :], qpen_d, "sC_qpen%d" % s, w=["qra_pen%d" % s])
    DMA("pool", wq[:, :, 0:1536], w_q_b_d.rearrange("(k p) n -> p k n", p=128)[:, :, 0:1536], "sC_wq0", w=["wq0"])
    DMA("pool", wq[:, :, 1536:3072], w_q_b_d.rearrange("(k p) n -> p k n", p=128)[:, :, 1536:3072], "sC_wq1", w=["wq1"])
    DMA("pool", wkv[:, :, 0:2048], w_kv_b_d.rearrange("(k p) n -> p k n", p=128)[:, :, 0:2048], "sC_wkv0", w=["wkv0"])
    DMA("pool", wkv[:, :, 2048:4096], w_kv_b_d.rearrange("(k p) n -> p k n", p=128)[:, :, 2048:4096], "sC_wkv1", w=["wkv1"])
    DMA("sp", cos2, cos2_d, "sC_cos", w=["cos2"])
    DMA("sp", sin2, sin2_d, "sC_sin", w=["sin2"])
    for kt in range(4):
        s = kt % 2
        DMA("sp", cxl[s], ctx_ckv_d[kt * 128:(kt + 1) * 128, :], "sC_cx%d" % s, w=["cxl%d" % s])
        for c in range(4):
            TR(ps[4][:, c * 128:(c + 1) * 128], cxl[s][:, c * 128:(c + 1) * 128], ident, r=["cxl%d" % s, "ident"], w=["ps4"])
        CP("dve", ckvT[:, :, T + kt * 128:T + (kt + 1) * 128], ps[4][:, :].rearrange("p (a b) -> p a b", b=128), r=["ps4"], w=["ckvT_ctx"])
    DMA("sp", cxk, ctx_kr_d.rearrange("(a p) c -> p a c", p=128), "sC_cxk", w=["cxk"])
    for kt in range(4):
        TR(ps[5][0:64, kt * 128:(kt + 1) * 128], cxk[:, kt, :], ident, r=["cxk", "ident"], w=["ps5"])
    CP("dve", kra[0:64, T:NKEY], ps[5][0:64, :], r=["ps5"], w=["kra_ctx"])
    ckr = ["ckvT_own", "ckvT_ctx"]
    krr = ["kra_own", "kra_hot", "kra_ctx"]
    for h in range(16):
        hs = h % 2
        wqr = "wq%d" % (h // 8)
        wkr_ = "wkv%d" % (h // 8)
        qc0 = h * 192
        kc0 = h * 256
        CP("pool", wqs[hs][:, :, 0:32], wq[:, :, qc0 + 160:qc0 + 192], r=[wqr], w=["wqs%d" % hs])
        CP("pool", wqs[hs][:, :, 32:64], wq[:, :, qc0 + 128:qc0 + 160], r=[wqr], w=["wqs%d" % hs])
        for tb in range(4):
            tsl = slice(tb * 512, (tb + 1) * 512)
            for k in range(4):
                MM(ps[4][:, :], wq[:, k, qc0:qc0 + 128], qnT[:, k, tsl], start=(k == 0), stop=(k == 3), r=[wqr, "qnT"], w=["ps4"])
            CP("act", qn_h[hs][:, tsl], ps[4][:, :], r=["ps4"], w=["qn_h%d" % hs])
            for k in range(4):
                MM(ps[5][0:64, :], wq[:, k, qc0 + 128:qc0 + 192], qnT[:, k, tsl], start=(k == 0), stop=(k == 3), r=[wqr, "qnT"], w=["ps5"])
            for k in range(4):
                MM(ps[6][0:64, :], wqs[hs][:, k, :], qnT[:, k, tsl], start=(k == 0), stop=(k == 3), r=["wqs%d" % hs, "qnT"], w=["ps6"])
            TT("dve", rp1, ps[5][0:64, :], cos2[:, tsl], ALU.mult, r=["ps5", "cos2"], w=["rp1"])
            TT("dve", rp2, ps[6][0:64, :], sin2[:, tsl], ALU.mult, r=["ps6", "sin2"], w=["rp2"])
            TT("dve", qra[hs][0:64, tsl], rp1, rp2, ALU.add, r=["rp1", "rp2"], w=["qra%d" % hs])
        for kb in range(5):
            ksl = slice(kb * 512, (kb + 1) * 512)
            for k in range(4):
                MM(ps[7][:, :], wkv[:, k, kc0:kc0 + 128], ckvT[:, k, ksl], start=(k == 0), stop=(k == 3), r=[wkr_] + ckr, w=["ps7"])
            CP("act", kn_h[hs][:, ksl], ps[7][:, :], r=["ps7"], w=["kn_h%d" % hs])
        for kq in range(5):
            for kk in range(4):
                kt = kq * 4 + kk
                for k in range(4):
                    MM(ps[4][:, kk * 128:(kk + 1) * 128], ckvT[:, k, kt * 128:(kt + 1) * 128], wkv[:, k, kc0 + 128:kc0 + 256],
                       start=(k == 0), stop=(k == 3), r=[wkr_] + ckr, w=["ps4"])
            CP("dve", v_h[hs][:, kq * 4:(kq + 1) * 4, :], ps[4][:, :].rearrange("p (a b) -> p a b", b=128), r=["ps4"], w=["v_h%d" % hs])
        pti = 0
        for qb in range(4):
            qsl = slice(qb * 512, (qb + 1) * 512)
            prev = None
            for kt in range(21):
                if kt < 20:
                    sb = kt % 2
                    p_ = pti % 4
                    pti += 1
                    MM(ps[sb][:, :], kn_h[hs][:, kt * 128:(kt + 1) * 128], qn_h[hs][:, qsl], start=True, stop=False,
                       r=["kn_h%d" % hs, "qn_h%d" % hs], w=["ps%d" % sb])
                    MM(ps[sb][:, :], kra[0:73, kt * 128:(kt + 1) * 128], qra[hs][0:73, qsl], start=False, stop=True,
                       r=krr + ["qra%d" % hs, "qra_pen%d" % hs], w=["ps%d" % sb])
                    ACT(PT[p_], ps[sb][:, :], AF.Exp, r=["ps%d" % sb], w=["PT%d" % p_], scale=SCALE)
                if prev is not None:
                    pk, pp = prev
                    MM(ps[2][:, :], v_h[hs][:, pk, :], PT[pp], start=(pk == 0), stop=(pk == 19), r=["v_h%d" % hs, "PT%d" % pp], w=["ps2"])
                    MM(ps[3][:, :], onesb, PT[pp], start=(pk == 0), stop=(pk == 19), r=["onesb", "PT%d" % pp], w=["ps3"])
                prev = (kt, p_) if kt < 20 else None
            RECIP(rden, ps[3][:, :], r=["ps3"], w=["rden"])
            TT("dve", oTh[hs][:, qsl], ps[2][:, :], rden, ALU.mult, r=["ps2", "rden"], w=["oTh%d" % hs])
        DMA("sp", oT_s[h * 128:(h + 1) * 128, :], oTh[hs], "sC_oT%d" % hs, r=["oTh%d" % hs])
    BARRIER()
    if STOP_AFTER == 'C':
        return done()

    A = StageArena()
    g1bc = A.f32(128, D)
    lnw = A.f32(128, D)
    lnb = A.f32(128, D)
    DMA("sp", g1bc, g1_s.partition_broadcast(128), "sD_g1", w=["g1bc"])
    DMA("sp", lnw, ln1_w_d.partition_broadcast(128), "sD_lnw", w=["lnw"])
    DMA("sp", lnb, ln1_b_d.partition_broadcast(128), "sD_lnb", w=["lnb"])
    yTb = A.bf16(128, 32, 512)
    oTb = A.bf16(128, 16, 512)
    gsl = [A.bf16(128, 2, 2, 512) for _ in range(2)]
    mrg = A.bf16(128, 16, 512)
    NSD = 3
    wD = [A.bf16(128, 32, 256) for _ in range(NSD)]
    t1 = [A.f32(128, 512) for _ in range(2)]
    t2 = [A.f32(128, 512) for _ in range(2)]
    vt = A.f32(128, 4, D)
    xin = [A.f32(128, D)]
    junk = xin[0]
    st1 = [A.f32(128, 4) for _ in range(2)]
    w_ssm_v = w_ssm_out_d.rearrange("(k p) n -> p k n", p=128)
    w_mla_v = w_mla_out_d.rearrange("(k p) n -> p k n", p=128)
    w_o_v = w_o_d.rearrange("(k p) n -> p k n", p=128)
    dgroups = []
    for tb in range(4):
        for m2 in range(8):
            dgroups.append(("ssm", m2, tb))
            dgroups.append(("mla", m2, tb))
        for fb in range(4):
            dgroups.append(("wo", fb, tb))

    def issue_d(n):
        if n < len(dgroups):
            s = n % NSD
            kind, m, _ = dgroups[n]
            if kind == "ssm":
                DMA("pool", wD[s], w_ssm_v[:, :, m * 256:(m + 1) * 256], "sD_w%d" % s, w=["wD%d" % s])
            elif kind == "mla":
                DMA("pool", wD[s][:, 0:16, :], w_mla_v[:, :, m * 256:(m + 1) * 256], "sD_w%d" % s, w=["wD%d" % s])
            else:
                DMA("pool", wD[s].rearrange("p a b -> p (a b)").rearrange("p (a b) -> p a b", b=512), w_o_v[:, :, m * 512:(m + 1) * 512],
                    "sD_w%d" % s, w=["wD%d" % s])

    for n in range(NSD - 1):
        issue_d(n)
    tctr = 0
    for n, (kind, m, tb) in enumerate(dgroups):
        issue_d(n + NSD - 1)
        s = n % NSD
        wres = "wD%d" % s
        tsl = slice(tb * 512, (tb + 1) * 512)
        if kind == "ssm" and m == 0:
            DMA("sp", yTb, yT_s.rearrange("(c p) t -> p c t", p=128)[:, :, tsl], "sD_yT", w=["yTb"])
            DMA("sp", oTb, oT_s.rearrange("(c p) t -> p c t", p=128)[:, :, tsl], "sD_oT", w=["oTb"])
        if kind == "ssm":
            gq = m % 2
            gT_v = gT_s.rearrange("(c p) t -> p c t", p=128)
            DMA("sp", gsl[gq][:, 0, :, :], gT_v[:, m * 2:m * 2 + 2, tsl], "sD_gs%d" % gq, w=["gsl%d" % gq])
            DMA("sp", gsl[gq][:, 1, :, :], gT_v[:, 16 + m * 2:16 + m * 2 + 2, tsl], "sD_gm%d" % gq, w=["gsl%d" % gq])
            for mm in range(2):
                mc = m * 2 + mm
                b = mm
                for k in range(32):
                    MM(ps[b][:, :], wD[s][:, k, mm * 128:(mm + 1) * 128], yTb[:, k, :], start=(k == 0), stop=(k == 31),
                       r=[wres, "yTb"], w=["ps%d" % b])
                TT("dve", t1[mm], ps[b][:, :], gsl[gq][:, 0, mm, :], ALU.mult, r=["ps%d" % b, "gsl%d" % gq], w=["t1_%d" % mm])
        elif kind == "mla":
            gq = m % 2
            for mm in range(2):
                mc = m * 2 + mm
                b = 2 + mm
                for k in range(16):
                    MM(ps[b][:, :], wD[s][:, k, mm * 128:(mm + 1) * 128], oTb[:, k, :], start=(k == 0), stop=(k == 15),
                       r=[wres, "oTb"], w=["ps%d" % b])
                TT("dve", t2[mm], ps[b][:, :], gsl[gq][:, 1, mm, :], ALU.mult, r=["ps%d" % b, "gsl%d" % gq], w=["t2_%d" % mm])
                TT("pool", mrg[:, mc, :], t1[mm], t2[mm], ALU.add, r=["t1_%d" % mm, "t2_%d" % mm], w=["mrg"])
        else:
            fb = m
            fsl = slice(fb * 512, (fb + 1) * 512)
            wv = wD[s].rearrange("p a b -> p (a b)").rearrange("p (a b) -> p a b", b=512)
            for i4 in range(4):
                i = tb * 4 + i4
                b = 4 + (i4 % 2)
                if fb == 0:
                    DMA("sp", xin[0], x_d[i * 128:(i + 1) * 128, :], "sD_x0", w=["xin0"])
                for k in range(16):
                    MM(ps[b][:, :], mrg[:, k, i4 * 128:(i4 + 1) * 128], wv[:, k, :], start=(k == 0), stop=(k == 15),
                       r=["mrg", wres], w=["ps%d" % b])
                tq = tctr % 2
                tctr += 1
                TT("dve", t1[tq], ps[b][:, :], g1bc[:, fsl], ALU.mult, r=["ps%d" % b, "g1bc"], w=["t1_%d" % tq])
                if fb == 0:
                    TS("pool", vt[:, i4, :], xin[0], ALU_ALPHA, ALU.mult, r=["xin0"], w=["vt%d" % i4])
                TT("pool", vt[:, i4, fsl], vt[:, i4, fsl], t1[tq], ALU.add, r=["vt%d" % i4, "t1_%d" % tq], w=["vt%d" % i4])
            if fb == 3:
                for i4 in range(4):
                    i = tb * 4 + i4
                    q2 = i4 % 2
                    V = vt[:, i4, :]
                    st = st1[q2]
                    TS("dve", junk, V, 1.0, ALU.mult, 0.0, ALU.add, r=["vt%d" % i4], w=["xin0", "st%d" % q2], accum=st[:, 0:1])
                    TS("dve", st[:, 1:2], st[:, 0:1], -1.0 / D, ALU.mult, r=["st%d" % q2], w=["st%d" % q2])
                    ACT(junk, V, AF.Square, r=["vt%d" % i4, "st%d" % q2], w=["xin0", "st%d" % q2], bias=st[:, 1:2], accum=st[:, 2:3])
                    ACT(st[:, 3:4], st[:, 2:3], AF.Sqrt, r=["st%d" % q2], w=["st%d" % q2], scale=1.0 / D, bias=1e-5)
                    RECIP(st[:, 3:4], st[:, 3:4], r=["st%d" % q2], w=["st%d" % q2])
                    TS("dve", V, V, st[:, 1:2], ALU.add, st[:, 3:4], ALU.mult, r=["vt%d" % i4, "st%d" % q2], w=["vt%d" % i4])
                    TT("dve", V, V, lnw, ALU.mult, r=["vt%d" % i4, "lnw"], w=["vt%d" % i4])
                    TT("pool", V, V, lnb, ALU.add, r=["vt%d" % i4, "lnb"], w=["vt%d" % i4])
                    DMA("sp", x1_s[i * 128:(i + 1) * 128, :], V, "sD_x1_%d" % i4, r=["vt%d" % i4])
    BARRIER()
    if STOP_AFTER == 'D':
        return done()

    A = StageArena()
    h2T = A.bf16(128, 16, 1024)
    accT = A.f32(128, 16, 1024)
    bg = A.f32(128, 32, 16, 2)
    bd = A.f32(128, 32, 16)
    comb = A.f32(128, 8, 32)
    rw = A.f32(128, 16, 32)
    rb_bc = A.f32(128, 32)
    NSE = 3
    wE = [A.bf16(128, 16, 256) for _ in range(NSE)]
    lg = A.f32(128, 32)
    m8 = A.f32(128, 8)
    nmx = A.f32(128, 1)
    msk = A.f32(128, 32)
    ex = A.f32(128, 32)
    ssum = A.f32(128, 1)
    mark_e = A.off
    h32 = A.f32(128, 16, 128)
    x1l = [A.f32(128, D) for _ in range(2)]
    A.off = mark_e
    actT = A.bf16(128, 16, 1024)
    combs = A.f32(128, 1024)
    gsb = [A.f32(128, 512) for _ in range(2)]
    sgb = [A.f32(128, 512) for _ in range(2)]
    usb = [A.f32(128, 512) for _ in range(2)]
    tcb = [A.f32(128, 512) for _ in range(2)]
    A.off = mark_e
    lnw2 = A.f32(128, D)
    lnb2 = A.f32(128, D)
    v2 = [A.f32(128, D) for _ in range(2)]
    x1e = [A.f32(128, D)]
    junk2 = A.f32(128, D)
    st2 = [A.f32(128, 4) for _ in range(2)]
    for e4 in range(4):
        DMA("sp", bg[:, e4 * 8:(e4 + 1) * 8, :, :], b_gu_d[e4 * 8:(e4 + 1) * 8, :].rearrange("e (c p two) -> p e c two", p=128, two=2),
            "sE_bg%d" % e4, w=["bg"], slow=True)
        DMA("sp", bd[:, e4 * 8:(e4 + 1) * 8, :], b_down_d[e4 * 8:(e4 + 1) * 8, :].rearrange("e (c p) -> p e c", p=128),
            "sE_bd%d" % e4, w=["bd"], slow=True)
    DMA("sp", rw, router_w_d.rearrange("(k p) e -> p k e", p=128), "sE_rw", w=["rw"])
    DMA("sp", rb_bc, router_b_d.partition_broadcast(128), "sE_rb", w=["rb"])
    w_gu_v = w_gu_d.rearrange("e (k p) n -> e p k n", p=128)
    w_dn_v = w_down_d.rearrange("e (k p) n -> e p k n", p=128)
    egroups = []
    for blk in range(2):
        for e in range(32):
            for m in range(16):
                egroups.append(("gu", blk, e, m))
            for j2 in range(8):
                egroups.append(("dn", blk, e, j2))

    def issue_e(n):
        if n < len(egroups):
            s = n % NSE
            kind, _, e, m = egroups[n]
            if kind == "gu":
                DMA("pool", wE[s], w_gu_v[e][:, :, m * 256:(m + 1) * 256], "sE_w%d" % s, w=["wE%d" % s])
            else:
                DMA("pool", wE[s], w_dn_v[e][:, :, m * 256:(m + 1) * 256], "sE_w%d" % s, w=["wE%d" % s])

    n = 0
    for blk in range(2):
        for i8 in range(8):
            i = blk * 8 + i8
            xs_ = i % 2
            DMA("sp", x1l[xs_], x1_s[i * 128:(i + 1) * 128, :], "sE_x1_%d" % xs_, w=["x1l%d" % xs_])
            for cq in range(4):
                b = 4 + cq % 2
                for cc in range(4):
                    c = cq * 4 + cc
                    TR(ps[b][:, cc * 128:(cc + 1) * 128], x1l[xs_][:, c * 128:(c + 1) * 128], ident, r=["x1l%d" % xs_, "ident"], w=["ps%d" % b])
                for cc in range(4):
                    c = cq * 4 + cc
                    if cc % 2 == 0:
                        ACT(h32[:, c, :], ps[b][:, cc * 128:(cc + 1) * 128], AF.Identity, r=["ps%d" % b, "sc2p", "mod"], w=["h32_%d" % c],
                            scale=sc2p[:, c:c + 1], bias=sh2[:, c:c + 1])
                    else:
                        TS("dve", h32[:, c, :], ps[b][:, cc * 128:(cc + 1) * 128], sc2p[:, c:c + 1], ALU.mult, sh2[:, c:c + 1], ALU.add,
                           r=["ps%d" % b, "sc2p", "mod"], w=["h32_%d" % c])
            CP("pool", h2T[:, :, i8 * 128:(i8 + 1) * 128], h32, r=["h32_%d" % c for c in range(16)], w=["h2T"])
            for k in range(16):
                MM(ps[6][:, 0:32], h32[:, k, :], rw[:, k, :], start=(k == 0), stop=(k == 15), r=["h32_%d" % k, "rw"], w=["ps6"])
            TT("dve", lg, ps[6][:, 0:32], rb_bc, ALU.add, r=["ps6", "rb"], w=["lg"])
            S.add("dve", lambda e: e.max(out=m8, in_=lg), ["lg"], ["m8"])
            TS("dve", msk, lg, m8[:, 3:4], ALU.is_ge, r=["lg", "m8"], w=["msk"])
            TS("dve", nmx, m8[:, 0:1], -1.0, ALU.mult, r=["m8"], w=["nmx"])
            ACT(ex, lg, AF.Exp, r=["lg", "nmx"], w=["ex"], bias=nmx[:, 0:1])
            STT("dve", ex, ex, 1.0, msk, ALU.mult, ALU.mult, r=["ex", "msk"], w=["ex", "ssum"], accum=ssum[:, 0:1])
            RECIP(ssum, ssum, r=["ssum"], w=["ssum"])
            TS("dve", comb[:, i8, :], ex, ssum[:, 0:1], ALU.mult, r=["ex", "ssum"], w=["comb"])
        BARRIER()
        if blk == 0:
            for q in range(NSE - 1):
                issue_e(q)
        for e in range(32):
            for i8 in range(8):
                b = 6 + (i8 // 4)
                MM(ps[b][:, (i8 % 4) * 128:(i8 % 4 + 1) * 128], comb[:, i8, e:e + 1].to_broadcast([128, 128]), ident,
                   r=["comb", "ident"], w=["ps%d" % b])
            CP("act", combs[:, 0:512], ps[6][:, :], r=["ps6"], w=["combs"])
            CP("act", combs[:, 512:1024], ps[7][:, :], r=["ps7"], w=["combs"])
            for m in range(16):
                issue_e(n + NSE - 1)
                s = n % NSE
                n += 1
                wres = "wE%d" % s
                for t2_ in range(2):
                    tsl = slice(t2_ * 512, (t2_ + 1) * 512)
                    q2 = (m * 2 + t2_) % 2
                    for k in range(16):
                        MM(ps[0 + t2_][:, :], wE[s][:, k, 0:256:2], h2T[:, k, tsl], start=(k == 0), stop=(k == 15), r=[wres, "h2T"], w=["ps%d" % t2_])
                    for k in range(16):
                        MM(ps[2 + t2_][:, :], wE[s][:, k, 1:256:2], h2T[:, k, tsl], start=(k == 0), stop=(k == 15), r=[wres, "h2T"], w=["ps%d" % (2 + t2_)])
                    TS("dve", gsb[q2], ps[0 + t2_][:, :], bg[:, e, m, 0:1], ALU.add, 7.0, ALU.min, r=["ps%d" % t2_, "bg"], w=["gsb%d" % q2])
                    ACT(sgb[q2], gsb[q2], AF.Sigmoid, r=["gsb%d" % q2], w=["sgb%d" % q2], scale=1.702)
                    TS("dve", usb[q2], ps[2 + t2_][:, :], bg[:, e, m, 1:2], ALU.add, 7.0, ALU.min, r=["ps%d" % (2 + t2_), "bg"], w=["usb%d" % q2])
                    TS("dve", usb[q2], usb[q2], -7.0, ALU.max, 1.0, ALU.add, r=["usb%d" % q2], w=["usb%d" % q2])
                    TT("dve", gsb[q2], gsb[q2], sgb[q2], ALU.mult, r=["gsb%d" % q2, "sgb%d" % q2], w=["gsb%d" % q2])
                    TT("dve", actT[:, m, tsl], gsb[q2], usb[q2], ALU.mult, r=["gsb%d" % q2, "usb%d" % q2], w=["actT%d" % m])
            ar = ["actT%d" % m for m in range(16)]
            for j2 in range(8):
                issue_e(n + NSE - 1)
                s = n % NSE
                n += 1
                wres = "wE%d" % s
                for jj in range(2):
                    j = j2 * 2 + jj
                    for t2_ in range(2):
                        tsl = slice(t2_ * 512, (t2_ + 1) * 512)
                        b = 4 + (jj * 2 + t2_) % 2
                        q2 = (jj * 2 + t2_) % 2
                        for k in range(16):
                            MM(ps[b][:, :], wE[s][:, k, jj * 128:(jj + 1) * 128], actT[:, k, tsl], start=(k == 0), stop=(k == 15),
                               r=[wres] + ar, w=["ps%d" % b])
                        if e == 0:
                            STT("dve", accT[:, j, tsl], ps[b][:, :], bd[:, e, j:j + 1], combs[:, tsl], ALU.add, ALU.mult,
                                r=["ps%d" % b, "bd", "combs"], w=["accT%d" % j])
                        else:
                            STT("dve", tcb[q2], ps[b][:, :], bd[:, e, j:j + 1], combs[:, tsl], ALU.add, ALU.mult,
                                r=["ps%d" % b, "bd", "combs"], w=["tcb%d" % q2])
                            TT("pool", accT[:, j, tsl], accT[:, j, tsl], tcb[q2], ALU.add, r=["accT%d" % j, "tcb%d" % q2], w=["accT%d" % j])
        accr = ["accT%d" % j for j in range(16)]
        for j in range(16):
            TS("dve", accT[:, j, :], accT[:, j, :], g2c[:, j:j + 1], ALU.mult, r=["accT%d" % j, "mod"], w=["accT%d" % j])
        BARRIER()
        alias_r = []
        DMA("sp", lnw2, ln2_w_d.partition_broadcast(128), "sE_lnw", r=[], w=["lnw2"])
        DMA("sp", lnb2, ln2_b_d.partition_broadcast(128), "sE_lnb", r=[], w=["lnb2"])
        for i8 in range(8):
            i = blk * 8 + i8
            q2 = i8 % 2
            DMA("sp", x1e[0], x1_s[i * 128:(i + 1) * 128, :], "sE_x1e0", w=["x1e0"])
            for cq in range(4):
                b = cq % 2
                for cc in range(4):
                    c = cq * 4 + cc
                    TR(ps[b][:, cc * 128:(cc + 1) * 128], accT[:, c, i8 * 128:(i8 + 1) * 128], ident, r=["accT%d" % c, "ident"], w=["ps%d" % b])
                STT("dve", v2[q2][:, cq * 512:(cq + 1) * 512], x1e[0][:, cq * 512:(cq + 1) * 512], ALU_ALPHA, ps[b][:, :], ALU.mult, ALU.add,
                    r=["x1e0", "ps%d" % b], w=["v2_%d" % q2])
            V = v2[q2]
            st = st2[q2]
            vr = "v2_%d" % q2
            sr = "st2_%d" % q2
            TS("dve", junk2, V, 1.0, ALU.mult, 0.0, ALU.add, r=[vr], w=["junk2", sr], accum=st[:, 0:1])
            TS("dve", st[:, 1:2], st[:, 0:1], -1.0 / D, ALU.mult, r=[sr], w=[sr])
            ACT(junk2, V, AF.Square, r=[vr, sr], w=["junk2", sr], bias=st[:, 1:2], accum=st[:, 2:3])
            ACT(st[:, 3:4], st[:, 2:3], AF.Sqrt, r=[sr], w=[sr], scale=1.0 / D, bias=1e-5)
            RECIP(st[:, 3:4], st[:, 3:4], r=[sr], w=[sr])
            TS("dve", V, V, st[:, 1:2], ALU.add, st[:, 3:4], ALU.mult, r=[vr, sr], w=[vr])
            TT("dve", V, V, lnw2, ALU.mult, r=[vr, "lnw2"], w=[vr])
            TT("pool", V, V, lnb2, ALU.add, r=[vr, "lnb2"], w=[vr])
            DMA("sp", y_d[i * 128:(i + 1) * 128, :], V, "out_y%d" % q2, r=[vr])
        BARRIER()

    return done()


ALU_ALPHA = ALPHA

_CACHE = {}


def _rope_tables():
    n_tok = 2048
    rows = n_tok // 64
    row = np.repeat(np.arange(rows, dtype=np.float32), 64)
    col = np.tile(np.arange(64, dtype=np.float32), rows)
    n_freq = 16
    inv = (np.float32(10000.0) ** (-np.arange(n_freq, dtype=np.float32) / n_freq)).astype(np.float32)
    ang = np.concatenate([row[:, None] * inv, col[:, None] * inv], -1).astype(np.float32)
    cos = np.cos(ang).astype(np.float32)
    sin = np.sin(ang).astype(np.float32)
    cos2 = np.concatenate([cos, cos], 1).T.copy()
    sin2 = np.concatenate([-sin, sin], 1).T.copy()
    return cos2, sin2


def kernel(x_prompt, x_sample, cache_mla_ckv, cache_mla_krope, state_ssm, c, c_ctx,
           w_ada, b_ada, w_in, conv_w, conv_b, a_log, dt_bias, d_skip, ssm_norm_w, w_ssm_out,
           q_a_norm_w, w_q_b, kv_a_norm_w, w_kv_b, w_mla_out, w_o, ln1_w, ln1_b,
           router_w, router_b, w_gu, b_gu, w_down, b_down, ln2_w, ln2_b):
    f = lambda a: np.ascontiguousarray(np.asarray(a, dtype=np.float32))
    if "nc" not in _CACHE:
        _CACHE["nc"] = build_program()
    nc, _ = _CACHE["nc"]
    shared = {
        "w_ada": f(w_ada[0]), "b_ada": f(b_ada[0]), "w_in": f(w_in[0]), "conv_w": f(conv_w[0]), "conv_b": f(conv_b[0]),
        "a_log": f(a_log[0]).reshape(128), "dt_bias": f(dt_bias[0]).reshape(128), "d_skip": f(d_skip[0]),
        "ssm_norm_w": f(ssm_norm_w[0]), "w_ssm_out": f(w_ssm_out[0]), "q_a_norm_w": f(q_a_norm_w[0]), "w_q_b": f(w_q_b[0]),
        "kv_a_norm_w": f(kv_a_norm_w[0]), "w_kv_b": f(w_kv_b[0]), "w_mla_out": f(w_mla_out[0]), "w_o": f(w_o[0]),
        "ln1_w": f(ln1_w[0]), "ln1_b": f(ln1_b[0]), "router_w": f(router_w[0]), "router_b": f(router_b[0]),
        "w_gu": f(w_gu[0]) if STOP_AFTER is None else f(w_gu[0][:1]), "b_gu": f(b_gu[0]),
        "w_down": f(w_down[0]) if STOP_AFTER is None else f(w_down[0][:1]), "b_down": f(b_down[0]),
        "ln2_w": f(ln2_w[0]), "ln2_b": f(ln2_b[0]),
        "ident": np.eye(128, dtype=np.float32),
        "uf": np.triu(np.ones((128, 128), np.float32)),
        "ub": np.tril(np.ones((128, 128), np.float32)),
        "ones": np.ones((128, 128), np.float32),
    }
    khot = np.zeros((9, NKEY), np.float32)
    for t in range(T):
        khot[t // 256, t] = 1.0
    khot[8, T:] = 1.0
    shared["khot"] = khot
    cos2, sin2 = _rope_tables()
    pen_prompt = np.full((9, T), -16384.0, np.float32)
    for t in range(T):
        pen_prompt[t // 256, t] = 0.0
    in_maps = []
    for core in range(8):
        m = dict(shared)
        if core < 4:
            b = core
            m["x"] = f(x_sample[b])
            m["cond"] = f(c[b])
            m["ctx_ckv"] = f(cache_mla_ckv[b, 0])
            m["ctx_kr"] = f(cache_mla_krope[b, 0])
            m["h0"] = f(state_ssm[b, 0])
            m["carry"] = np.ones(128, np.float32)
            m["cos2"] = cos2
            m["sin2"] = sin2
            m["qpen"] = np.zeros((9, T), np.float32)
        else:
            s0 = (core - 4) * 8
            m["x"] = f(x_prompt[s0:s0 + 8]).reshape(T, D)
            m["cond"] = f(c_ctx)
            m["ctx_ckv"] = np.zeros((512, 512), np.float32)
            m["ctx_kr"] = np.zeros((512, 64), np.float32)
            m["h0"] = np.zeros((2, 64, 64, 128), np.float32)
            m["carry"] = np.zeros(128, np.float32)
            m["cos2"] = np.ones((64, T), np.float32)
            m["sin2"] = np.zeros((64, T), np.float32)
            m["qpen"] = pen_prompt
        in_maps.append(m)
    if DEBUG_CORES is not None:
        sub = [in_maps[i] for i in DEBUG_CORES]
        res = run_bass_kernel_spmd(nc, sub, core_ids=list(range(len(sub))))
        _CACHE["last"] = {c: res.results[j] for j, c in enumerate(DEBUG_CORES)}
        return None
    res = run_bass_kernel_spmd(nc, in_maps, core_ids=list(range(8)))
    R = res.results
    _CACHE["last"] = R
    y_s = np.stack([R[i]["y"] for i in range(4)], 0)
    y_p = np.concatenate([R[i]["y"].reshape(8, 256, D) for i in range(4, 8)], 0)
    ckv = np.concatenate([R[i]["ckv_out"].reshape(8, 1, 256, 512) for i in range(4, 8)], 0)
    kr = np.concatenate([R[i]["kr_out"].reshape(8, 1, 256, 64) for i in range(4, 8)], 0)
    st = np.concatenate([R[i]["st_out"].reshape(8, 1, 2, 64, 64, 128) for i in range(4, 8)], 0)
    return (y_p.astype(np.float32), y_s.astype(np.float32), ckv.astype(np.float32), kr.astype(np.float32), st.astype(np.float32))
```
